# Optimizing a Trainium2 kernel written in Bass

```python
import jax, jax.numpy as jnp
from jax import lax
import numpy as np

D_MODEL = 1024
BATCH = 16
SEQ = 2048
DEPTH = 2

CHUNK = 64
HEAD_DIM = 128
EPS = 1e-6
N_HEADS_A = 4
A_WIDTH = N_HEADS_A * HEAD_DIM
IDX_HEADS = 8
IDX_DIM = 64
IDX_TOPK_MAX = 256
N_HEADS_B = 4
GLA_DK = 64
GLA_DV = 128
GLA_GATE_RANK = 16
GLA_TAU = 16.0
B_WIDTH = N_HEADS_B * GLA_DV
N_HEADS_C = 8
C_WIDTH = N_HEADS_C * HEAD_DIM
SB_QBLOCK = 128
ROPE_THETA = 500000.0
ROPE_FRACTION = 4
D_FF = 4 * D_MODEL
N_EVEN = (DEPTH + 1) // 2
N_ODD = DEPTH // 2
AB_SIZES = (
    A_WIDTH, A_WIDTH, A_WIDTH,
    IDX_HEADS * IDX_DIM, IDX_DIM, IDX_HEADS,
    N_HEADS_B * GLA_DK, N_HEADS_B * GLA_DK,
    B_WIDTH,
    GLA_GATE_RANK,
    B_WIDTH,
)
AB_PROJ = sum(AB_SIZES)

kernel_name = "hybrid_dsa_gla_stickbreak_encoder"


def _split(a, sizes):
    out, o = [], 0
    for s in sizes:
        out.append(a[..., o:o + s])
        o += s
    return out


def rmsnorm(x, g):
    xf = x.astype(jnp.float32)
    y = xf * lax.rsqrt(jnp.mean(xf * xf, axis=-1, keepdims=True) + EPS)
    return (y * g.astype(jnp.float32)).astype(x.dtype)


def rope_partial(x, pos):
    d = x.shape[-1]
    rot = d // ROPE_FRACTION
    half = rot // 2
    inv = jnp.power(ROPE_THETA, -jnp.arange(half, dtype=jnp.float32) * 2.0 / rot)
    ang = pos.astype(jnp.float32)[:, None] * inv[None, :]
    cos = jnp.cos(ang)[None, :, None, :]
    sin = jnp.sin(ang)[None, :, None, :]
    xf = x.astype(jnp.float32)
    x1, x2, rest = xf[..., :half], xf[..., half:rot], xf[..., rot:]
    out = jnp.concatenate([x1 * cos - x2 * sin, x2 * cos + x1 * sin, rest], axis=-1)
    return out.astype(x.dtype)


def dsa_attention(q, k, v, iq, ik, iw):
    bsz, s_len, n_h, d_h = q.shape
    topk = min(IDX_TOPK_MAX, s_len // 4)
    n_blk = s_len // CHUNK
    key_pos = jnp.arange(s_len)

    def blk_fn(args):
        c, qb, iqb, iwb = args
        logits = jnp.einsum('bthd,bsd->bths', iqb, ik).astype(jnp.float32) * IDX_DIM ** -0.5
        score = jnp.einsum('bths,bth->bts', jax.nn.relu(logits),
                           iwb.astype(jnp.float32) * IDX_HEADS ** -0.5)
        limit = (c + 1) * CHUNK
        score = jnp.where((key_pos < limit)[None, None, :], score, -jnp.inf)
        _, idx = lax.top_k(score, topk)
        valid = idx < limit
        ks = jax.vmap(lambda a, i: a[i])(k, idx)
        vs = jax.vmap(lambda a, i: a[i])(v, idx)
        s = jnp.einsum('bthd,btkhd->bthk', qb, ks).astype(jnp.float32) * HEAD_DIM ** -0.5
        s = jnp.where(valid[:, :, None, :], s, -jnp.inf)
        p = jax.nn.softmax(s, axis=-1)
        return jnp.einsum('bthk,btkhd->bthd', p.astype(v.dtype), vs)

    def to_blocks(a):
        return a.reshape(bsz, n_blk, CHUNK, *a.shape[2:]).swapaxes(0, 1)

    out = lax.map(blk_fn, (jnp.arange(n_blk), to_blocks(q), to_blocks(iq), to_blocks(iw)))
    return out.swapaxes(0, 1).reshape(bsz, s_len, n_h, d_h)


def gla_chunked(q, k, v, lg):
    bsz, s_len, n_h, dk = q.shape
    dv = v.shape[-1]
    n_c = s_len // CHUNK
    causal = jnp.tril(jnp.ones((CHUNK, CHUNK), dtype=bool))

    def chunks(a):
        return a.astype(jnp.float32).reshape(bsz, n_c, CHUNK, n_h, a.shape[-1]).transpose(1, 0, 3, 2, 4)

    def step(state, inp):
        qc, kc, vc, gc = inp
        b = jnp.cumsum(gc, axis=2)
        o_inter = jnp.einsum('bhtd,bhde->bhte', qc * jnp.exp(b), state)
        diff = b[:, :, :, None, :] - b[:, :, None, :, :]
        decay = jnp.exp(jnp.where(causal[:, :, None], diff, -jnp.inf))
        att = jnp.einsum('bhtd,bhsd,bhtsd->bhts', qc, kc, decay)
        o = o_inter + jnp.einsum('bhts,bhse->bhte', att, vc)
        b_last = b[:, :, -1:, :]
        state = (jnp.exp(b_last[:, :, 0, :])[..., None] * state
                 + jnp.einsum('bhsd,bhse->bhde', kc * jnp.exp(b_last - b), vc))
        return state, o

    s0 = jnp.zeros((bsz, n_h, dk, dv), jnp.float32)
    _, o = lax.scan(step, s0, (chunks(q), chunks(k), chunks(v), chunks(lg)))
    return o.transpose(1, 0, 3, 2, 4).reshape(bsz, s_len, n_h, dv)


def stick_breaking(q, k, v):
    bsz, s_len, n_h, d_h = q.shape
    n_blk = s_len // SB_QBLOCK
    key_pos = jnp.arange(s_len)

    def blk_fn(args):
        i, qb = args
        z = jnp.einsum('bthd,bshd->bhts', qb, k).astype(jnp.float32) * d_h ** -0.5
        t_pos = i * SB_QBLOCK + jnp.arange(SB_QBLOCK)
        mask = key_pos[None, :] < t_pos[:, None]
        lo = jnp.where(mask, jax.nn.log_sigmoid(-z), 0.0)
        rc = lax.cumsum(lo, axis=3, reverse=True)
        log_a = jax.nn.log_sigmoid(z) + (rc - lo)
        a = jnp.where(mask, jnp.exp(log_a), 0.0)
        return jnp.einsum('bhts,bshd->bthd', a.astype(v.dtype), v)

    qb = q.reshape(bsz, n_blk, SB_QBLOCK, n_h, d_h).swapaxes(0, 1)
    out = lax.map(blk_fn, (jnp.arange(n_blk), qb))
    return out.swapaxes(0, 1).reshape(bsz, s_len, n_h, d_h)


def mixer_ab(n, pos, w_in, gq, gk, w_gate_up, b_gate, g_gla, w_out):
    bsz, s_len, _ = n.shape
    proj = n @ w_in
    (qa, ka, va, iq, ik, iw, qb, kb, vb, g_low, og) = _split(proj, AB_SIZES)
    qa = rope_partial(rmsnorm(qa.reshape(bsz, s_len, N_HEADS_A, HEAD_DIM), gq), pos)
    ka = rope_partial(rmsnorm(ka.reshape(bsz, s_len, N_HEADS_A, HEAD_DIM), gk), pos)
    va = va.reshape(bsz, s_len, N_HEADS_A, HEAD_DIM)
    iq = rope_partial(iq.reshape(bsz, s_len, IDX_HEADS, IDX_DIM), pos)
    ik = rope_partial(ik[:, :, None, :], pos)[:, :, 0, :]
    oa = dsa_attention(qa, ka, va, iq, ik, iw).reshape(bsz, s_len, A_WIDTH)
    qb = qb.reshape(bsz, s_len, N_HEADS_B, GLA_DK) * GLA_DK ** -0.5
    kb = kb.reshape(bsz, s_len, N_HEADS_B, GLA_DK)
    vb = vb.reshape(bsz, s_len, N_HEADS_B, GLA_DV)
    gate = (g_low @ w_gate_up + b_gate).astype(jnp.float32)
    lg = (jax.nn.log_sigmoid(gate) / GLA_TAU).reshape(bsz, s_len, N_HEADS_B, GLA_DK)
    ob = gla_chunked(qb, kb, vb, lg).astype(n.dtype)
    ob = rmsnorm(ob, g_gla).reshape(bsz, s_len, B_WIDTH) * jax.nn.silu(og)
    return jnp.concatenate([oa, ob], axis=-1) @ w_out


def mixer_c(n, w_in, gq, gk, w_out):
    bsz, s_len, _ = n.shape
    q, k, v = _split(n @ w_in, (C_WIDTH, C_WIDTH, C_WIDTH))
    q = rmsnorm(q.reshape(bsz, s_len, N_HEADS_C, HEAD_DIM), gq)
    k = rmsnorm(k.reshape(bsz, s_len, N_HEADS_C, HEAD_DIM), gk)
    v = v.reshape(bsz, s_len, N_HEADS_C, HEAD_DIM)
    return stick_breaking(q, k, v).reshape(bsz, s_len, C_WIDTH) @ w_out


def squared_relu_mlp(n, w_up, w_down):
    return jnp.square(jax.nn.relu(n @ w_up)) @ w_down


def setup_inputs(seed: int = 0) -> dict:
    key = jax.random.key(seed)
    ks = jax.random.split(key, 16)
    f32 = jnp.float32

    def w(k, shape, fan_in):
        return jax.random.normal(k, shape, f32) * fan_in ** -0.5

    def gain(k, shape):
        return 1.0 + 0.02 * jax.random.normal(k, shape, f32)

    return {
        "x": jax.random.normal(ks[0], (BATCH, SEQ, D_MODEL), f32),
        "g_mix": gain(ks[1], (DEPTH, D_MODEL)),
        "g_ffn": gain(ks[2], (DEPTH, D_MODEL)),
        "w_in_ab": w(ks[3], (N_EVEN, D_MODEL, AB_PROJ), D_MODEL),
        "gq_a": gain(ks[4], (N_EVEN, HEAD_DIM)),
        "gk_a": gain(ks[5], (N_EVEN, HEAD_DIM)),
        "w_gate_up": w(ks[6], (N_EVEN, GLA_GATE_RANK, N_HEADS_B * GLA_DK), GLA_GATE_RANK),
        "b_gate": 0.1 * jax.random.normal(ks[7], (N_EVEN, N_HEADS_B * GLA_DK), f32),
        "g_gla": gain(ks[8], (N_EVEN, GLA_DV)),
        "w_out_ab": w(ks[9], (N_EVEN, A_WIDTH + B_WIDTH, D_MODEL), A_WIDTH + B_WIDTH),
        "w_in_c": w(ks[10], (N_ODD, D_MODEL, 3 * C_WIDTH), D_MODEL),
        "gq_c": gain(ks[11], (N_ODD, HEAD_DIM)),
        "gk_c": gain(ks[12], (N_ODD, HEAD_DIM)),
        "w_out_c": w(ks[13], (N_ODD, C_WIDTH, D_MODEL), C_WIDTH),
        "w_up": w(ks[14], (DEPTH, D_MODEL, D_FF), D_MODEL),
        "w_down": w(ks[15], (DEPTH, D_FF, D_MODEL), D_FF),
    }


def reference(x, g_mix, g_ffn, w_in_ab, gq_a, gk_a, w_gate_up, b_gate, g_gla, w_out_ab,
              w_in_c, gq_c, gk_c, w_out_c, w_up, w_down):
    pos = jnp.arange(x.shape[1])
    h = x
    for layer in range(DEPTH):
        n = rmsnorm(h, g_mix[layer])
        if layer % 2 == 0:
            i = layer // 2
            h = h + mixer_ab(n, pos, w_in_ab[i], gq_a[i], gk_a[i], w_gate_up[i],
                             b_gate[i], g_gla[i], w_out_ab[i])
        else:
            i = layer // 2
            h = h + mixer_c(n, w_in_c[i], gq_c[i], gk_c[i], w_out_c[i])
        h = h + squared_relu_mlp(rmsnorm(h, g_ffn[layer]), w_up[layer], w_down[layer])
    return h
```

```python
import numpy as np
import concourse.bass as bass
import concourse.mybir as mybir
from concourse.bass_utils import run_bass_kernel_spmd
from concourse.alu_op_type import AluOpType as ALU

F32 = mybir.dt.float32
BF16 = mybir.dt.bfloat16
AF = mybir.ActivationFunctionType
AX = mybir.AxisListType

SEQ = 2048
DM = 1024
NT = SEQ // 128
EPS = 1e-6
ABW = 3672
NEG = -1.0e30
BIS_ITERS = 18
ARENA_BYTES = 175 * 1024

_ENG_ATTR = {"pe": "tensor", "act": "scalar", "dve": "vector", "pool": "gpsimd", "sp": "sync"}


class Buf:
    __slots__ = ("lw", "rd")

    def __init__(self):
        self.lw = None
        self.rd = {}


class Sched:
    ENG = ("pe", "act", "dve", "pool", "sp")

    def __init__(self, nc):
        self.nc = nc
        self.sem = {e: nc.alloc_semaphore("sem_" + e) for e in self.ENG}
        self.cnt = {e: 0 for e in self.ENG}
        self.ops = {e: [] for e in self.ENG}
        self.waited = {e: {} for e in self.ENG}
        self.chan = {}

    def channel(self, name):
        if name not in self.chan:
            self.chan[name] = [self.nc.alloc_semaphore("ch_" + name), 0]
        return name

    def _deps(self, reads, writes):
        d = {}
        for b in reads:
            if b.lw is not None and d.get(b.lw[0], 0) < b.lw[1]:
                d[b.lw[0]] = b.lw[1]
        for b in writes:
            if b.lw is not None and d.get(b.lw[0], 0) < b.lw[1]:
                d[b.lw[0]] = b.lw[1]
            for k, v in b.rd.items():
                if d.get(k, 0) < v:
                    d[k] = v
        return d

    def op(self, eng, fn, reads=(), writes=()):
        d = self._deps(reads, writes)
        self.cnt[eng] += 1
        idx = self.cnt[eng]
        for b in reads:
            b.rd[eng] = idx
        for b in writes:
            b.lw = (eng, idx)
            b.rd = {}
        self.ops[eng].append((d, fn, None))

    def pe(self, fn, reads=(), writes=()):
        self.op("pe", fn, reads, writes)

    def act(self, fn, reads=(), writes=()):
        self.op("act", fn, reads, writes)

    def dve(self, fn, reads=(), writes=()):
        self.op("dve", fn, reads, writes)

    def pool(self, fn, reads=(), writes=()):
        self.op("pool", fn, reads, writes)

    def dma(self, fn, chan, reads=(), writes=(), queue="sp"):
        d = self._deps(reads, writes)
        c = self.chan[chan]
        c[1] += 16
        key = "ch:" + chan
        for b in reads:
            b.rd[key] = c[1]
        for b in writes:
            b.lw = (key, c[1])
            b.rd = {}
        self.ops[queue].append((d, fn, chan))

    def _semof(self, k):
        if k.startswith("ch:"):
            return self.chan[k[3:]][0]
        return self.sem[k]

    def flush(self, final=False):
        nc = self.nc
        if final:
            d = {}
            for name, c in self.chan.items():
                if c[1] > 0:
                    d["ch:" + name] = c[1]
            self.ops["sp"].append((d, None, None))
        with nc.Block() as blk:
            for eng in self.ENG:
                ops = self.ops[eng]

                def body(e, eng=eng, ops=ops):
                    w = self.waited[eng]
                    for d, fn, chan in ops:
                        for k, v in d.items():
                            if k == eng and eng == "pe":
                                continue
                            if w.get(k, 0) < v:
                                e.wait_ge(self._semof(k), v)
                                w[k] = v
                        if fn is None:
                            continue
                        inst = fn(e)
                        if chan is None:
                            inst.then_inc(self.sem[eng], 1)
                        else:
                            inst.then_inc(self.chan[chan][0], 16)

                getattr(blk, _ENG_ATTR[eng])(body)
        self.ops = {e: [] for e in self.ENG}


class Arena:
    def __init__(self, nc, nbytes, base=None):
        self.t = nc.alloc_sbuf_tensor("arena", [128, nbytes // 4], F32) if base is None else base
        self.top = 0
        self.nbytes = nbytes

    def mark(self):
        return self.top

    def release(self, m):
        self.top = m

    def alloc(self, shape, dtype):
        esz = 4 if dtype == F32 else 2
        n = int(np.prod(shape[1:]))
        nb = (n * esz + 63) // 64 * 64
        off = self.top
        self.top += nb
        assert self.top <= self.nbytes, ("arena overflow", self.top)
        ap = self.t[0:shape[0], off // 4:(off + nb) // 4]
        if dtype != F32:
            ap = ap.bitcast(dtype)
        ap = ap[:, 0:n]
        if len(shape) == 3:
            ap = ap.rearrange("p (a b) -> p a b", a=shape[1])
        elif len(shape) == 4:
            ap = ap.rearrange("p (a b c) -> p a b c", a=shape[1], b=shape[2])
        return ap


class Rot:
    def __init__(self, arena, n, shape, dtype):
        self.items = [(arena.alloc(shape, dtype), Buf()) for _ in range(n)]
        self.i = 0

    def next(self):
        it = self.items[self.i % len(self.items)]
        self.i += 1
        return it


def build_program(NSEQ=2, stop=None):
    nc = bass.Bass("TRN2", target_bir_lowering=False)
    S = Sched(nc)

    def dram_in(name, shape):
        return nc.dram_tensor(name, shape, F32, kind="ExternalInput").ap()

    x = dram_in("x", [NSEQ, SEQ, DM])
    w_in_ab = dram_in("w_in_ab", [DM, ABW])
    w_gate_up = dram_in("w_gate_up", [16, 256])
    b_gate = dram_in("b_gate", [1, 256])
    w_out_ab = dram_in("w_out_ab", [DM, DM])
    w_in_c = dram_in("w_in_c", [DM, 3 * DM])
    w_out_c = dram_in("w_out_c", [DM, DM])
    w_up = dram_in("w_up", [2, DM, 4 * DM])
    w_down = dram_in("w_down", [2, 4 * DM, DM])
    c_ident = dram_in("c_ident", [128, 128])
    c_rope_a = dram_in("c_rope_a", [SEQ, 32])
    c_rope_i = dram_in("c_rope_i", [SEQ, 16])
    c_m64 = dram_in("c_m64", [64, 3, 64])
    c_m128 = dram_in("c_m128", [128, 3, 128])
    c_gains = dram_in("c_gains", [128, 5, 128])
    c_gcol = dram_in("c_gcol", [128, 4, 8])
    out = nc.dram_tensor("out", [NSEQ, SEQ, DM], F32, kind="ExternalOutput").ap()
    h_scr = nc.dram_tensor("h_scr", [NSEQ, SEQ, DM], F32).ap()
    hb = [[Buf() for _ in range(NT)] for _ in range(NSEQ)]

    A = Arena(nc, ARENA_BYTES)
    PS = [nc.alloc_psum_tensor(f"psb{i}", [128, 512], F32) for i in range(8)]
    PSB = [Buf() for _ in range(8)]

    def psf(i):
        return PS[i][:, :]

    def psh(i):
        return PS[i][:, :].bitcast(BF16)

    for nm in ("c0", "c1", "c2", "c3", "xin0", "xin1", "st0", "st1", "stg0", "stg1", "hres"):
        S.channel(nm)

    nT_raw = A.alloc([128, 8192], F32)
    nT = nT_raw.bitcast(BF16).rearrange("p (a b) -> p a b", a=8)
    nTb = [Buf() for _ in range(NT)]
    oT_raw = A.alloc([128, 8192], F32)
    oT = oT_raw.bitcast(BF16).rearrange("p (a b) -> p a b", a=8)
    oTb = [Buf() for _ in range(NT)]
    identf = A.alloc([128, 128], F32)
    ident = A.alloc([128, 128], BF16)
    ropeA = A.alloc([128, NT, 32], F32)
    ropeI = A.alloc([128, NT, 16], F32)
    m64 = A.alloc([64, 3, 64], F32)
    m128f = A.alloc([128, 3, 128], F32)
    m128 = A.alloc([128, 3, 128], BF16)
    gains = A.alloc([128, 5, 128], F32)
    gcol = A.alloc([128, 4, 8], F32)
    wg = A.alloc([32, 256], F32)
    thr_all = A.alloc([128, 1], F32)
    constb = Buf()

    def bc_row(ap):
        r = ap.partition_broadcast(128)
        if len(r.shape) == 3:
            r = r.rearrange("p a b -> p (a b)")
        return r

    def dma_simple(out_ap, in_ap, chan, writes, reads=(), ncdma=True):
        S.dma(lambda e: nc.sync.dma_start(out=out_ap, in_=in_ap), chan, reads=reads, writes=writes)

    dma_simple(identf, c_ident, "c0", [constb])
    dma_simple(ropeA, c_rope_a.rearrange("(i p) c -> p i c", p=128), "c0", [constb])
    dma_simple(ropeI, c_rope_i.rearrange("(i p) c -> p i c", p=128), "c0", [constb])
    dma_simple(m64, c_m64, "c0", [constb])
    dma_simple(m128f, c_m128, "c0", [constb])
    dma_simple(gains, c_gains, "c0", [constb])
    dma_simple(wg[0:16, :], w_gate_up, "c0", [constb])
    dma_simple(wg[16:17, :], b_gate, "c0", [constb])
    dma_simple(gcol, c_gcol, "c0", [constb])
    S.dve(lambda e: nc.vector.tensor_copy(out=ident, in_=identf), reads=[constb], writes=[constb])
    S.dve(lambda e: nc.vector.tensor_copy(out=m128, in_=m128f), reads=[constb], writes=[constb])
    S.dve(lambda e: nc.vector.memset(thr_all, -1.0e29), reads=[], writes=[constb])
    S.flush()
    base_mark = A.mark()

    triU = m128[:, 0, :]
    ones128 = m128[:, 1, :]
    strictT = m128[:, 2, :]
    tri_incl = m64[:, 0, :]
    tri_rev = m64[:, 1, :]
    causT = m64[:, 2, :]

    def mk_stage(ar=None):
        ar = A if ar is None else ar
        return [(ar.alloc([128, 2048], F32), Buf(), S.channel(f"stg{i}")) for i in range(2)]

    stage_state = {"pool": None, "i": 0}

    def load_w(src2d, KC, ncols, dst, dstb, gc=None):
        srcv = src2d.rearrange("(k p) c -> p k c", p=128)
        kstep = max(1, 2048 // ncols)
        for k0 in range(0, KC, kstep):
            kn = min(kstep, KC - k0)
            st, stb, ch = stage_state["pool"][stage_state["i"] % 2]
            stage_state["i"] += 1
            stv = st[:, 0:kn * ncols].rearrange("p (k c) -> p k c", k=kn)
            S.dma(lambda e, stv=stv, k0=k0, kn=kn: nc.sync.dma_start(out=stv, in_=srcv[:, k0:k0 + kn, :]),
                  ch, writes=[stb])
            if gc is None:
                S.pool(lambda e, stv=stv, k0=k0, kn=kn: nc.gpsimd.tensor_copy(out=dst[:, k0:k0 + kn, :], in_=stv),
                       reads=[stb], writes=[dstb])
            else:
                S.pool(lambda e, stv=stv, k0=k0, kn=kn: nc.gpsimd.tensor_tensor(
                    out=dst[:, k0:k0 + kn, :], in0=stv,
                    in1=gc[:, k0:k0 + kn].unsqueeze(2).to_broadcast([128, kn, ncols]), op=ALU.mult),
                    reads=[stb, constb], writes=[dstb])

    def mm_group(out_ap, pairs, rd, wr):
        def f(e):
            n = len(pairs)
            inst = None
            for q, (l, r) in enumerate(pairs):
                inst = nc.tensor.matmul(out_ap, lhsT=l, rhs=r, start=(q == 0), stop=(q == n - 1))
            return inst
        S.pe(f, reads=rd, writes=wr)

    def transposes(outs_ins, idn, rd, wr):
        def f(e):
            inst = None
            for o, i_ in outs_ins:
                inst = nc.tensor.transpose(o, i_, idn)
            return inst
        S.pe(f, reads=rd, writes=wr)

    def rstd_from_ss(ssv, ssb, n, inv_n, add_eps):
        P = ssv.shape[0]
        if add_eps:
            S.dve(lambda e: nc.vector.tensor_scalar(out=ssv[:, 0, :], in0=ssv[:, 0, :], scalar1=inv_n, scalar2=EPS,
                                                    op0=ALU.mult, op1=ALU.add), reads=[ssb], writes=[ssb])
            S.act(lambda e: nc.scalar.activation(out=ssv[:, 1, :], in_=ssv[:, 0, :], func=AF.Ln), reads=[ssb], writes=[ssb])
        else:
            S.act(lambda e: nc.scalar.activation(out=ssv[:, 1, :], in_=ssv[:, 0, :], func=AF.Ln, scale=inv_n),
                  reads=[ssb], writes=[ssb])
        S.act(lambda e: nc.scalar.activation(out=ssv[:, 2, :], in_=ssv[:, 1, :], func=AF.Exp, scale=-0.5),
              reads=[ssb], writes=[ssb])

    def rope(xv, xb, H, half, table, i, tmp, tmpb, P=128, prow=None):
        if prow is None:
            cs = table[0:P, i, :]
        else:
            cs = prow
        cos = cs[:, 0:half].unsqueeze(1).to_broadcast([P, H, half])
        sin = cs[:, half:2 * half].unsqueeze(1).to_broadcast([P, H, half])
        x1 = xv[:, :, 0:half]
        x2 = xv[:, :, half:2 * half]

        def f1(e):
            nc.gpsimd.tensor_tensor(out=tmp[:, 0], in0=x1, in1=cos, op=ALU.mult)
            nc.gpsimd.tensor_tensor(out=tmp[:, 1], in0=x2, in1=sin, op=ALU.mult)
            nc.gpsimd.tensor_tensor(out=tmp[:, 2], in0=x2, in1=cos, op=ALU.mult)
            return nc.gpsimd.tensor_tensor(out=tmp[:, 3], in0=x1, in1=sin, op=ALU.mult)
        S.pool(f1, reads=[xb, constb], writes=[tmpb])

        def f2(e):
            nc.gpsimd.tensor_tensor(out=x1, in0=tmp[:, 0], in1=tmp[:, 1], op=ALU.subtract)
            return nc.gpsimd.tensor_tensor(out=x2, in0=tmp[:, 2], in1=tmp[:, 3], op=ALU.add)
        S.pool(f2, reads=[tmpb], writes=[xb])

    def norm_transpose(xt, xtb, i, ws):
        junk, jb = ws["junk"].next()
        ss, sb = ws["ss"].next()
        nb, nbb = ws["nb"].next()
        S.act(lambda e: nc.scalar.activation(out=junk, in_=xt, func=AF.Square, accum_out=ss[:, 0, 0:1]),
              reads=[xtb], writes=[jb, sb])
        rstd_from_ss(ss, sb, 1, 1.0 / DM, True)
        S.dve(lambda e: nc.vector.tensor_scalar(out=nb, in0=xt, scalar1=ss[:, 2, 0:1], scalar2=None, op0=ALU.mult),
              reads=[xtb, sb], writes=[nbb])
        pb = ws["psT"]
        pv = psh(pb)
        transposes([(pv[:, kc * 128:(kc + 1) * 128], nb[:, kc * 128:(kc + 1) * 128]) for kc in range(8)], ident,
                   [nbb, constb], [PSB[pb]])
        S.act(lambda e: nc.scalar.copy(out=nT[:, :, i * 128:(i + 1) * 128],
                                       in_=pv[:, 0:1024].rearrange("p (k t) -> p k t", k=8)),
              reads=[PSB[pb]], writes=[nTb[i]])

    def mk_norm_ws(psT):
        return {"junk": Rot(A, 1, [128, 1024], BF16), "ss": Rot(A, 2, [128, 3, 1], F32),
                "nb": Rot(A, 2, [128, 1024], BF16), "psT": psT}

    def phase_norm(s, src):
        m = A.mark()
        ws = mk_norm_ws(7)
        xin = [(A.alloc([128, 1024], F32), Buf(), f"xin{k}") for k in range(2)]
        for i in range(NT):
            xt, xb, ch = xin[i % 2]
            S.dma(lambda e, xt=xt, i=i: nc.sync.dma_start(out=xt, in_=src[s, i * 128:(i + 1) * 128, :]), ch,
                  reads=[hb[s][i]], writes=[xb])
            norm_transpose(xt, xb, i, ws)
        S.flush()
        A.release(m)

    def qk_post(ps_i, i, ws, gidx, dstT, dst_b, scale, do_rope):
        sq, sqb = ws["sq"].next()
        ss, ssb = ws["ss4"].next()
        qn, qnb = ws["qn"].next()
        qh, qhb = ws["qh"].next()
        pv = psf(ps_i)
        S.act(lambda e: nc.scalar.activation(out=sq, in_=pv, func=AF.Square), reads=[PSB[ps_i]], writes=[sqb])
        S.dve(lambda e: nc.vector.tensor_reduce(out=ss[:, 0, :], in_=sq.rearrange("p (h d) -> p h d", h=4), axis=AX.X,
                                                op=ALU.add), reads=[sqb], writes=[ssb])
        rstd_from_ss(ss, ssb, 4, 1.0 / 128, True)

        def f(e):
            inst = None
            for h in range(4):
                inst = nc.vector.scalar_tensor_tensor(out=qn[:, h * 128:(h + 1) * 128], in0=pv[:, h * 128:(h + 1) * 128],
                                                      scalar=ss[:, 2, h:h + 1], in1=gains[:, gidx, :], op0=ALU.mult,
                                                      op1=ALU.mult)
            return inst
        S.dve(f, reads=[PSB[ps_i], ssb, constb], writes=[qnb])
        if do_rope:
            tmp, tmpb = ws["rtmp"].next()
            rope(qn.rearrange("p (h d) -> p h d", h=4), qnb, 4, 16, ropeA, i, tmp, tmpb)
        S.act(lambda e: nc.scalar.activation(out=qh, in_=qn, func=AF.Copy, scale=scale), reads=[qnb], writes=[qhb])
        pt = ws["psT"]
        ptv = psh(pt)
        transposes([(ptv[:, h * 128:(h + 1) * 128], qh[:, h * 128:(h + 1) * 128]) for h in range(4)], ident,
                   [qhb, constb], [PSB[pt]])
        S.act(lambda e: nc.scalar.copy(out=dstT[:, :, i * 128:(i + 1) * 128],
                                       in_=ptv[:, 0:512].rearrange("p (h t) -> p h t", h=4)),
              reads=[PSB[pt]], writes=[dst_b])

    def proj_tok(wb, ncols, i, ps_i, wbb, M=128, t0=None):
        t0 = i * 128 if t0 is None else t0
        mm_group(psf(ps_i)[0:M, 0:ncols], [(nT[:, kc, t0:t0 + M], wb[:, kc, 0:ncols]) for kc in range(8)],
                 [nTb[t0 // 128], wbb], [PSB[ps_i]])

    def mk_qk_ws(psT):
        return {"sq": Rot(A, 2, [128, 512], F32), "ss4": Rot(A, 2, [128, 3, 4], F32), "qn": Rot(A, 2, [128, 512], F32),
                "qh": Rot(A, 2, [128, 512], BF16), "rtmp": Rot(A, 2, [128, 4, 4, 16], F32), "psT": psT}

    def out_proj_and_norm(s, wout_dram, src, dst):
        m = A.mark()
        stage_state["pool"] = mk_stage()
        wo = A.alloc([128, 8, DM], BF16)
        wob = Buf()
        for c0 in range(0, DM, 256):
            load_w(wout_dram[:, c0:c0 + 256], 8, 256, wo[:, :, c0:c0 + 256], wob)
        ws = mk_norm_ws(7)
        hin = [(A.alloc([128, 1024], F32), Buf(), f"xin{k}") for k in range(2)]
        hnew = [(A.alloc([128, 1024], F32), Buf(), f"st{k}") for k in range(2)]
        for i in range(NT):
            hi_, hib, ch = hin[i % 2]
            hn, hnb, sch = hnew[i % 2]
            S.dma(lambda e, hi_=hi_, i=i: nc.sync.dma_start(out=hi_, in_=src[s, i * 128:(i + 1) * 128, :]), ch,
                  reads=[hb[s][i]], writes=[hib])
            for half in range(2):
                pi = 0 + 2 * (i % 2) + half
                mm_group(psf(pi), [(oT[:, c, i * 128:(i + 1) * 128], wo[:, c, half * 512:(half + 1) * 512])
                                   for c in range(8)], [oTb[i], wob], [PSB[pi]])
                S.dve(lambda e, pi=pi, half=half, hn=hn, hi_=hi_: nc.vector.tensor_tensor(
                    out=hn[:, half * 512:(half + 1) * 512], in0=psf(pi), in1=hi_[:, half * 512:(half + 1) * 512],
                    op=ALU.add), reads=[PSB[pi], hib], writes=[hnb])
            S.dma(lambda e, hn=hn, i=i: nc.sync.dma_start(out=dst[s, i * 128:(i + 1) * 128, :], in_=hn), sch,
                  reads=[hnb], writes=[hb[s][i]])
            norm_transpose(hn, hnb, i, ws)
        S.flush()
        A.release(m)

    def ffn(s, layer, final):
        m = A.mark()
        stage_state["pool"] = mk_stage()
        hres = A.alloc([128, NT, DM], F32)
        hresb = [Buf() for _ in range(NT)]
        actT = A.alloc([128, 4, SEQ], BF16)
        actb = [Buf() for _ in range(4)]
        AO = Arena(nc, 32768, base=oT_raw)
        wub = [(AO.alloc([128, 8, 512], BF16), Buf()) for _ in range(2)]
        wdb = [(AO.alloc([128, 4, DM], BF16), Buf()) for _ in range(2)]
        rr = Rot(A, 2, [128, 512], F32)
        for i in range(NT):
            S.dma(lambda e, i=i: nc.sync.dma_start(out=hres[:, i, :], in_=h_scr[s, i * 128:(i + 1) * 128, :]), "hres",
                  reads=[hb[s][i]], writes=[hresb[i]])
        gc = gcol[:, 2 + layer, :]
        for g in range(8):
            wu, wubb = wub[g % 2]
            wd, wdbb = wdb[g % 2]
            load_w(w_up[layer, :, g * 512:(g + 1) * 512], 8, 512, wu, wubb, gc)
            load_w(w_down[layer, g * 512:(g + 1) * 512, :], 4, DM, wd, wdbb)
            for fc in range(4):
                for tb in range(4):
                    pi = (fc * 4 + tb) % 4
                    mm_group(psf(pi), [(wu[:, kc, fc * 128:(fc + 1) * 128], nT[:, kc, tb * 512:(tb + 1) * 512])
                                       for kc in range(8)], nTb[4 * tb:4 * tb + 4] + [wubb], [PSB[pi]])
                    r, rb = rr.next()
                    S.act(lambda e, r=r, pi=pi: nc.scalar.activation(out=r, in_=psf(pi), func=AF.Relu),
                          reads=[PSB[pi]], writes=[rb])
                    S.pool(lambda e, r=r, fc=fc, tb=tb: nc.gpsimd.tensor_tensor(
                        out=actT[:, fc, tb * 512:(tb + 1) * 512], in0=r, in1=r, op=ALU.mult),
                        reads=[rb], writes=[actb[tb]])
            for ti in range(NT):
                for half in range(2):
                    pi = 4 + (ti * 2 + half) % 4
                    mm_group(psf(pi), [(actT[:, fc, ti * 128:(ti + 1) * 128], wd[:, fc, half * 512:(half + 1) * 512])
                                       for fc in range(4)], [actb[ti // 4], wdbb], [PSB[pi]])
                    S.dve(lambda e, pi=pi, ti=ti, half=half: nc.vector.tensor_tensor(
                        out=hres[:, ti, half * 512:(half + 1) * 512], in0=psf(pi),
                        in1=hres[:, ti, half * 512:(half + 1) * 512], op=ALU.add),
                        reads=[PSB[pi], hresb[ti]], writes=[hresb[ti]])
        dst = out if final else h_scr
        for i in range(NT):
            S.dma(lambda e, i=i: nc.sync.dma_start(out=dst[s, i * 128:(i + 1) * 128, :], in_=hres[:, i, :]),
                  f"st{i % 2}", reads=[hresb[i]], writes=[hb[s][i]])
        S.flush()
        A.release(m)

    def dsa(s):
        m = A.mark()
        kaT = A.alloc([128, 4, SEQ], BF16)
        qaT = A.alloc([128, 4, SEQ], BF16)
        va = A.alloc([128, NT, 4, 132], BF16)
        iqT = A.alloc([128, 4, SEQ], BF16)
        ikT = A.alloc([128, SEQ], BF16)
        iws = A.alloc([128, NT, 8], F32)
        kaTb, qaTb, vab, iqTb, ikTb, iwsb = Buf(), Buf(), Buf(), Buf(), Buf(), Buf()
        m2 = A.mark()
        AO = Arena(nc, 32768, base=oT_raw)
        stage_state["pool"] = mk_stage(AO)
        wbp = [(AO.alloc([128, 8, 512], BF16), Buf()) for _ in range(2)]
        ws = mk_qk_ws(7)
        cp = Rot(A, 2, [128, 512], F32)
        cpb = Rot(A, 2, [128, 512], BF16)
        itmp = Rot(A, 2, [128, 4, 8, 8], F32)
        ikf = Rot(A, 2, [128, 72], F32)
        ikd = Rot(A, 2, [128, 128], BF16)
        gc = gcol[:, 0, :]
        S.pool(lambda e: nc.gpsimd.memset(va[:, :, :, 128:129], 1.0), writes=[vab])
        blocks = [("qa", 0), ("ka", 512), ("va", 1024), ("iq", 1536), ("ik", 2048)]
        for bi, (nm, c0) in enumerate(blocks):
            ncols = 72 if nm == "ik" else 512
            wb, wbb = wbp[bi % 2]
            load_w(w_in_ab[:, c0:c0 + ncols], 8, ncols, wb[:, :, 0:ncols], wbb, gc)
            for i in range(NT):
                pi = i % 2
                proj_tok(wb, ncols, i, pi, wbb)
                if nm == "qa":
                    qk_post(pi, i, ws, 0, qaT, qaTb, 128 ** -0.5, True)
                elif nm == "ka":
                    qk_post(pi, i, ws, 1, kaT, kaTb, 1.0, True)
                elif nm == "va":
                    S.act(lambda e, pi=pi, i=i: nc.scalar.copy(out=va[:, i, :, 0:128],
                                                              in_=psf(pi).rearrange("p (h d) -> p h d", h=4)),
                          reads=[PSB[pi]], writes=[vab])
                elif nm == "iq":
                    c, cb = cp.next()
                    S.act(lambda e, c=c, pi=pi: nc.scalar.copy(out=c, in_=psf(pi)), reads=[PSB[pi]], writes=[cb])
                    tmp, tmpb = itmp.next()
                    rope(c.rearrange("p (h d) -> p h d", h=8), cb, 8, 8, ropeI, i, tmp, tmpb)
                    ch_, chb = cpb.next()
                    S.act(lambda e, c=c, ch_=ch_: nc.scalar.copy(out=ch_, in_=c), reads=[cb], writes=[chb])
                    ptv = psh(7)
                    transposes([(ptv[:, p * 128:(p + 1) * 128], ch_[:, p * 128:(p + 1) * 128]) for p in range(4)], ident,
                               [chb, constb], [PSB[7]])
                    S.act(lambda e, i=i, ptv=ptv: nc.scalar.copy(out=iqT[:, :, i * 128:(i + 1) * 128],
                                                                 in_=ptv[:, 0:512].rearrange("p (h t) -> p h t", h=4)),
                          reads=[PSB[7]], writes=[iqTb])
                else:
                    f_, fb = ikf.next()
                    S.act(lambda e, f_=f_, pi=pi: nc.scalar.copy(out=f_, in_=psf(pi)[:, 0:72]), reads=[PSB[pi]], writes=[fb])
                    tmp, tmpb = itmp.next()
                    rope(f_[:, 0:64].rearrange("p (h d) -> p h d", h=1), fb, 1, 8, ropeI, i, tmp[:, :, 0:1, :], tmpb)
                    d_, db = ikd.next()

                    def fcp(e, f_=f_, d_=d_, i=i):
                        nc.vector.tensor_copy(out=d_[:, 0:64], in_=f_[:, 0:64])
                        nc.vector.tensor_copy(out=d_[:, 64:128], in_=f_[:, 0:64])
                        return nc.vector.tensor_scalar(out=iws[:, i, :], in0=f_[:, 64:72], scalar1=1.0 / (8.0 * 8.0 ** 0.5),
                                                       scalar2=None, op0=ALU.mult)
                    S.dve(fcp, reads=[fb], writes=[db, iwsb])
                    ptv = psh(7)
                    transposes([(ptv[:, 0:128], d_)], ident, [db, constb], [PSB[7]])
                    S.act(lambda e, i=i, ptv=ptv: nc.scalar.copy(out=ikT[:, i * 128:(i + 1) * 128], in_=ptv[:, 0:128]),
                          reads=[PSB[7]], writes=[ikTb])
        S.flush()
        A.release(m2)
        AN = Arena(nc, 32768, base=nT_raw)
        score = Rot(AN, 2, [128, SEQ], F32)
        junk = AN.alloc([128, SEQ], BF16)
        junkb = Buf()
        maskr = Rot(AN, 2, [128, SEQ], BF16)
        maskTr = Rot(A, 2, [128, NT, 128], BF16)
        tmpf = Rot(A, 3, [128, 512], F32)
        ptr = Rot(A, 2, [128, 512], BF16)
        ptmr = Rot(A, 2, [128, 512], BF16)
        bsr = Rot(A, 2, [128, 8], F32)
        rzr = Rot(A, 2, [128, 4], F32)
        oar = Rot(A, 2, [128, 512], BF16)
        for j in range(NT):
            W = (j + 1) * 128
            sc, scb = score.next()
            for h in range(8):
                p0 = (h % 2) * 64
                pair = h // 2
                for kb in range((W + 511) // 512):
                    c0 = kb * 512
                    cw = min(512, W - c0)
                    pi = (h * 4 + kb) % 2
                    mm_group(psf(pi)[:, 0:cw], [(iqT[p0:p0 + 64, pair, j * 128:(j + 1) * 128], ikT[p0:p0 + 64, c0:c0 + cw])],
                             [iqTb, ikTb], [PSB[pi]])
                    t_, tb_ = tmpf.next()
                    S.act(lambda e, t_=t_, pi=pi, cw=cw: nc.scalar.activation(out=t_[:, 0:cw], in_=psf(pi)[:, 0:cw], func=AF.Relu),
                          reads=[PSB[pi]], writes=[tb_])
                    if h == 0:
                        S.dve(lambda e, t_=t_, sc=sc, c0=c0, cw=cw, j=j: nc.vector.tensor_scalar(
                            out=sc[:, c0:c0 + cw], in0=t_[:, 0:cw], scalar1=iws[:, j, 0:1], scalar2=None, op0=ALU.mult),
                            reads=[tb_, iwsb], writes=[scb])
                    else:
                        S.dve(lambda e, t_=t_, sc=sc, c0=c0, cw=cw, j=j, h=h: nc.vector.scalar_tensor_tensor(
                            out=sc[:, c0:c0 + cw], in0=t_[:, 0:cw], scalar=iws[:, j, h:h + 1], in1=sc[:, c0:c0 + cw],
                            op0=ALU.mult, op1=ALU.add), reads=[tb_, iwsb, scb], writes=[scb])
            S.dve(lambda e, sc=sc, W=W: nc.vector.memset(sc[0:64, W - 64:W], NEG), reads=[scb], writes=[scb])
            if j >= 2:
                bs, bsb = bsr.next()

                def f0(e, sc=sc, bs=bs, W=W):
                    nc.vector.tensor_reduce(out=bs[:, 0:1], in_=sc[:, 0:W - 64], axis=AX.X, op=ALU.min)
                    return nc.vector.tensor_reduce(out=bs[:, 1:2], in_=sc[:, 0:W], axis=AX.X, op=ALU.max)
                S.dve(f0, reads=[scb], writes=[bsb])
                S.dve(lambda e, bs=bs: nc.vector.tensor_tensor(out=bs[:, 1:2], in0=bs[:, 1:2], in1=bs[:, 0:1], op=ALU.subtract),
                      reads=[bsb], writes=[bsb])
                for k in range(1, BIS_ITERS + 1):
                    f = 2.0 ** (-k)
                    S.dve(lambda e, bs=bs, f=f: nc.vector.tensor_scalar(out=bs[:, 2:3], in0=bs[:, 1:2], scalar1=f,
                                                                        scalar2=bs[:, 0:1], op0=ALU.mult, op1=ALU.add),
                          reads=[bsb], writes=[bsb])
                    S.dve(lambda e, bs=bs, sc=sc, W=W: nc.vector.tensor_scalar(
                        out=junk[:, 0:W], in0=sc[:, 0:W], scalar1=bs[:, 2:3], scalar2=None, op0=ALU.is_ge, op1=ALU.add,
                        accum_out=bs[:, 3:4]), reads=[bsb, scb], writes=[bsb, junkb])
                    S.dve(lambda e, bs=bs: nc.vector.tensor_scalar(out=bs[:, 4:5], in0=bs[:, 3:4], scalar1=255.5,
                                                                   scalar2=bs[:, 1:2], op0=ALU.is_ge, op1=ALU.mult),
                          reads=[bsb], writes=[bsb])
                    S.dve(lambda e, bs=bs, f=f: nc.vector.scalar_tensor_tensor(out=bs[:, 0:1], in0=bs[:, 4:5], scalar=f,
                                                                               in1=bs[:, 0:1], op0=ALU.mult, op1=ALU.add),
                          reads=[bsb], writes=[bsb])
                thr = bs[:, 0:1]
                thrb = bsb
            else:
                thr = thr_all[:, 0:1]
                thrb = constb
            mk, mkb = maskr.next()
            S.dve(lambda e, mk=mk, sc=sc, W=W, thr=thr: nc.vector.tensor_scalar(out=mk[:, 0:W], in0=sc[:, 0:W], scalar1=thr,
                                                                               scalar2=None, op0=ALU.is_ge),
                  reads=[scb, thrb], writes=[mkb])
            mT, mTb = maskTr.next()
            for g in range((j + 4) // 4):
                kts = list(range(4 * g, min(4 * g + 4, j + 1)))
                pv = psh(2)
                transposes([(pv[:, q * 128:(q + 1) * 128], mk[:, kt * 128:(kt + 1) * 128]) for q, kt in enumerate(kts)],
                           ident, [mkb, constb], [PSB[2]])
                n = len(kts)
                S.act(lambda e, mT=mT, g=g, n=n, pv=pv: nc.scalar.copy(
                    out=mT[:, 4 * g:4 * g + n, :], in_=pv[:, 0:n * 128].rearrange("p (k t) -> p k t", k=n)),
                    reads=[PSB[2]], writes=[mTb])
            for h in range(4):
                po = 5 + h // 2
                ov = psf(po)[:, (h % 2) * 132:(h % 2) * 132 + 129]
                for g in range((j + 4) // 4):
                    kts = list(range(4 * g, min(4 * g + 4, j + 1)))
                    n = len(kts)
                    pi = 3 + (h * 4 + g) % 2

                    def fs(e, kts=kts, pi=pi, h=h, j=j):
                        inst = None
                        for q, kt in enumerate(kts):
                            inst = nc.tensor.matmul(psf(pi)[:, q * 128:(q + 1) * 128], lhsT=kaT[:, h, kt * 128:(kt + 1) * 128],
                                                    rhs=qaT[:, h, j * 128:(j + 1) * 128], start=True, stop=True)
                        return inst
                    S.pe(fs, reads=[kaTb, qaTb], writes=[PSB[pi]])
                    pt, ptb = ptr.next()
                    S.act(lambda e, pt=pt, pi=pi, n=n: nc.scalar.activation(out=pt[:, 0:n * 128], in_=psf(pi)[:, 0:n * 128],
                                                                           func=AF.Exp), reads=[PSB[pi]], writes=[ptb])
                    pm, pmb = ptmr.next()
                    S.dve(lambda e, pm=pm, pt=pt, mT=mT, g=g, n=n: nc.vector.tensor_tensor(
                        out=pm[:, 0:n * 128], in0=pt[:, 0:n * 128], in1=mT[:, 4 * g:4 * g + n, :].rearrange("p k t -> p (k t)"),
                        op=ALU.mult), reads=[ptb, mTb], writes=[pmb])

                    def fo(e, kts=kts, pm=pm, ov=ov, h=h, j=j):
                        inst = None
                        for q, kt in enumerate(kts):
                            inst = nc.tensor.matmul(ov, lhsT=pm[:, q * 128:(q + 1) * 128], rhs=va[:, kt, h, 0:129],
                                                    start=(kt == 0), stop=(kt == j))
                        return inst
                    S.pe(fo, reads=[pmb, vab], writes=[PSB[po]])
            rz, rzb = rzr.next()

            def frz(e, rz=rz):
                inst = None
                for h in range(4):
                    zc = (h % 2) * 132 + 128
                    inst = nc.vector.reciprocal(out=rz[:, h:h + 1], in_=psf(5 + h // 2)[:, zc:zc + 1])
                return inst
            S.dve(frz, reads=[PSB[5], PSB[6]], writes=[rzb])
            oa, oab = oar.next()

            def foa(e, rz=rz, oa=oa):
                inst = None
                for h in range(4):
                    c = (h % 2) * 132
                    inst = nc.scalar.activation(out=oa[:, h * 128:(h + 1) * 128], in_=psf(5 + h // 2)[:, c:c + 128],
                                                func=AF.Identity, scale=rz[:, h:h + 1])
                return inst
            S.act(foa, reads=[PSB[5], PSB[6], rzb], writes=[oab])
            ptv = psh(7)
            transposes([(ptv[:, h * 128:(h + 1) * 128], oa[:, h * 128:(h + 1) * 128]) for h in range(4)], ident,
                       [oab, constb], [PSB[7]])
            S.act(lambda e, j=j, ptv=ptv: nc.scalar.copy(out=oT[:, 0:4, j * 128:(j + 1) * 128],
                                                         in_=ptv[:, 0:512].rearrange("p (h t) -> p h t", h=4)),
                  reads=[PSB[7]], writes=[oTb[j]])
        S.flush()
        A.release(m)

    def gla(s):
        m = A.mark()
        stage_state["pool"] = mk_stage()
        wq = A.alloc([128, 8, 256], BF16)
        wk = A.alloc([128, 8, 256], BF16)
        wv = A.alloc([128, 8, 512], BF16)
        wl = A.alloc([128, 8, 16], BF16)
        wo_ = A.alloc([128, 8, 512], BF16)
        wb_ = Buf()
        gc = gcol[:, 0, :]
        load_w(w_in_ab[:, 2120:2376], 8, 256, wq, wb_, gc)
        load_w(w_in_ab[:, 2376:2632], 8, 256, wk, wb_, gc)
        load_w(w_in_ab[:, 2632:3144], 8, 512, wv, wb_, gc)
        load_w(w_in_ab[:, 3144:3160], 8, 16, wl, wb_, gc)
        load_w(w_in_ab[:, 3160:3672], 8, 512, wo_, wb_, gc)
        glr = [(A.alloc([32, 64], F32), Buf()) for _ in range(2)]
        for gl_, glb in glr:
            S.dve(lambda e, gl_=gl_: nc.vector.memset(gl_, 1.0), writes=[glb])
        spr = Rot(A, 2, [64, 256], F32)
        ebr = Rot(A, 2, [128, 2, 2, 64], F32)
        ebvr = Rot(A, 2, [64, 256], F32)
        qer = Rot(A, 2, [128, 2, 2, 64], BF16)
        for q_, qb_ in qer.items:
            S.dve(lambda e, q_=q_: nc.vector.memset(q_, 0.0), writes=[qb_])
        kdr = Rot(A, 2, [128, 2, 64], BF16)
        klr = Rot(A, 2, [64, 256], BF16)
        vcr = Rot(A, 2, [64, 512], BF16)
        sgr = Rot(A, 2, [64, 512], F32)
        gvr = Rot(A, 2, [64, 512], F32)
        atr = Rot(A, 2, [64, 4, 64], BF16)
        sqr = Rot(A, 2, [64, 512], F32)
        ssr = Rot(A, 2, [64, 3, 4], F32)
        onr = Rot(A, 2, [64, 512], F32)
        obr = Rot(A, 2, [64, 512], BF16)
        Sf = A.alloc([128, 2, 128], F32)
        Sfb = [Buf(), Buf()]
        Sbf = [(A.alloc([128, 2, 128], BF16), [Buf(), Buf()]) for _ in range(2)]
        NCH = SEQ // 64
        for c in range(NCH):
            t0 = c * 64
            ntb = nTb[t0 // 128]
            gl_, glb = glr[c % 2]
            mm_group(psf(0)[0:16, 0:64], [(wl[:, kc, 0:16], nT[:, kc, t0:t0 + 64]) for kc in range(8)], [ntb, wb_], [PSB[0]])
            S.act(lambda e, gl_=gl_: nc.scalar.copy(out=gl_[0:16, :], in_=psf(0)[0:16, 0:64]), reads=[PSB[0]], writes=[glb])
            mm_group(psf(0)[0:64, 256:512], [(gl_[0:17, 0:64], wg[0:17, :])], [glb, constb], [PSB[0]])
            sp, spb = spr.next()
            S.act(lambda e, sp=sp: nc.scalar.activation(out=sp, in_=psf(0)[0:64, 256:512], func=AF.Exp, scale=-1.0),
                  reads=[PSB[0]], writes=[spb])
            S.act(lambda e, sp=sp: nc.scalar.activation(out=sp, in_=sp, func=AF.Ln, bias=1.0), reads=[spb], writes=[spb])
            def fb(e, sp=sp):
                nc.tensor.matmul(psf(1)[:, 0:64], lhsT=sp[:, 0:128], rhs=tri_incl, start=True, stop=True)
                nc.tensor.matmul(psf(1)[:, 64:128], lhsT=sp[:, 128:256], rhs=tri_incl, start=True, stop=True)
                return nc.tensor.matmul(psf(1)[0:64, 128:384], lhsT=tri_rev, rhs=sp, start=True, stop=True)
            S.pe(fb, reads=[spb, constb], writes=[PSB[1]])
            eb, ebb = ebr.next()
            ebv, ebvb = ebvr.next()

            def fe(e, eb=eb, ebv=ebv):
                bv = psf(1)[:, 0:128].rearrange("p (g t) -> p g t", g=2)
                nc.scalar.activation(out=eb[:, 0], in_=bv, func=AF.Exp)
                nc.scalar.activation(out=eb[:, 1], in_=bv, func=AF.Exp, scale=-1.0)
                return nc.scalar.activation(out=ebv, in_=psf(1)[0:64, 128:384], func=AF.Exp)
            S.act(fe, reads=[PSB[1]], writes=[ebb, ebvb])
            def fqk(e, t0=t0):
                inst = None
                for g in range(2):
                    for kc in range(8):
                        nc.tensor.matmul(psf(2)[:, g * 64:(g + 1) * 64], lhsT=wq[:, kc, g * 128:(g + 1) * 128],
                                         rhs=nT[:, kc, t0:t0 + 64], start=(kc == 0), stop=(kc == 7))
                for g in range(2):
                    for kc in range(8):
                        nc.tensor.matmul(psf(2)[:, 128 + g * 64:128 + (g + 1) * 64], lhsT=wk[:, kc, g * 128:(g + 1) * 128],
                                         rhs=nT[:, kc, t0:t0 + 64], start=(kc == 0), stop=(kc == 7))
                for kc in range(8):
                    inst = nc.tensor.matmul(psf(2)[0:64, 256:512], lhsT=nT[:, kc, t0:t0 + 64], rhs=wk[:, kc, :],
                                            start=(kc == 0), stop=(kc == 7))
                return inst
            S.pe(fqk, reads=[ntb, wb_], writes=[PSB[2]])
            qe, qeb = qer.next()
            kd, kdb = kdr.next()
            kl, klb = klr.next()

            def fq(e, qe=qe, kd=kd, kl=kl, eb=eb, ebv=ebv):
                for hh in range(2):
                    p0 = hh * 64
                    nc.vector.scalar_tensor_tensor(out=qe[p0:p0 + 64, :, hh, :],
                                                   in0=psf(2)[p0:p0 + 64, 0:128].rearrange("p (g t) -> p g t", g=2),
                                                   scalar=0.125, in1=eb[p0:p0 + 64, 0], op0=ALU.mult, op1=ALU.mult)
                nc.vector.tensor_tensor(out=kd, in0=psf(2)[:, 128:256].rearrange("p (g t) -> p g t", g=2), in1=eb[:, 1],
                                        op=ALU.mult)
                return nc.vector.tensor_tensor(out=kl, in0=psf(2)[0:64, 256:512], in1=ebv, op=ALU.mult)
            S.dve(fq, reads=[PSB[2], ebb, ebvb], writes=[qeb, kdb, klb])
            mm_group(psf(3)[0:64, :], [(nT[:, kc, t0:t0 + 64], wv[:, kc, :]) for kc in range(8)], [ntb, wb_], [PSB[3]])
            vc, vcb = vcr.next()
            S.act(lambda e, vc=vc: nc.scalar.copy(out=vc, in_=psf(3)[0:64, :]), reads=[PSB[3]], writes=[vcb])
            mm_group(psf(4)[0:64, :], [(nT[:, kc, t0:t0 + 64], wo_[:, kc, :]) for kc in range(8)], [ntb, wb_], [PSB[4]])
            sg, sgb = sgr.next()

            def fsg(e, sg=sg):
                nc.scalar.activation(out=sg, in_=psf(4)[0:64, :], func=AF.Exp, scale=-1.0)
                nc.scalar.activation(out=sg, in_=sg, func=AF.Ln, bias=1.0)
                return nc.scalar.activation(out=sg, in_=sg, func=AF.Exp, scale=-1.0)
            S.act(fsg, reads=[PSB[4]], writes=[sgb])
            gv, gvb = gvr.next()
            S.dve(lambda e, gv=gv, sg=sg: nc.vector.tensor_tensor(out=gv, in0=psf(4)[0:64, :], in1=sg, op=ALU.mult),
                  reads=[PSB[4], sgb], writes=[gvb])
            def fat(e, kd=kd, qe=qe):
                inst = None
                for g in range(2):
                    for hh in range(2):
                        p0 = hh * 64
                        q = g * 2 + hh
                        inst = nc.tensor.matmul(psf(5)[0:64, q * 64:(q + 1) * 64], lhsT=kd[:, g, :],
                                                rhs=qe[:, g, hh, :], start=True, stop=True)
                return inst
            S.pe(fat, reads=[kdb, qeb], writes=[PSB[5]])
            at, atb = atr.next()
            S.dve(lambda e, at=at: nc.vector.tensor_tensor(
                out=at, in0=psf(5)[0:64, 0:256].rearrange("p (q t) -> p q t", q=4),
                in1=causT.unsqueeze(1).to_broadcast([64, 4, 64]), op=ALU.mult), reads=[PSB[5], constb], writes=[atb])
            sbf_prev, sbfb_prev = Sbf[(c + 1) % 2]
            sbf_cur, sbfb_cur = Sbf[c % 2]

            def fo(e, qe=qe, at=at, vc=vc, c=c, sbf_prev=sbf_prev):
                inst = None
                for g in range(2):
                    for hh in range(2):
                        p0 = hh * 64
                        q = g * 2 + hh
                        ov = psf(6)[0:64, q * 128:(q + 1) * 128]
                        if c > 0:
                            nc.tensor.matmul(ov, lhsT=qe[:, g, hh, :], rhs=sbf_prev[:, g, :], start=True, stop=False)
                        inst = nc.tensor.matmul(ov, lhsT=at[:, q, :], rhs=vc[:, q * 128:(q + 1) * 128], start=(c == 0), stop=True)
                return inst
            S.pe(fo, reads=[qeb, atb, vcb] + (sbfb_prev if c > 0 else []), writes=[PSB[6]])
            if c < NCH - 1:
                def fu(e, kl=kl, vc=vc):
                    nc.tensor.matmul(psf(7)[:, 0:256], lhsT=kl[:, 0:128], rhs=vc[:, 0:256], start=True, stop=True)
                    return nc.tensor.matmul(psf(7)[:, 256:512], lhsT=kl[:, 128:256], rhs=vc[:, 256:512], start=True, stop=True)
                S.pe(fu, reads=[klb, vcb], writes=[PSB[7]])
                for g in range(2):
                    def fs_(e, g=g, eb=eb, c=c):
                        inst = None
                        for hh in range(2):
                            p0 = hh * 64
                            uv = psf(7)[p0:p0 + 64, g * 256 + hh * 128:g * 256 + (hh + 1) * 128]
                            if c == 0:
                                inst = nc.vector.tensor_copy(out=Sf[p0:p0 + 64, g, :], in_=uv)
                            else:
                                inst = nc.vector.scalar_tensor_tensor(out=Sf[p0:p0 + 64, g, :], in0=Sf[p0:p0 + 64, g, :],
                                                                      scalar=eb[p0:p0 + 64, 0, g, 63:64], in1=uv,
                                                                      op0=ALU.mult, op1=ALU.add)
                        return inst
                    S.dve(fs_, reads=[PSB[7], ebb, Sfb[g]], writes=[Sfb[g]])
                    S.act(lambda e, g=g, sbf_cur=sbf_cur: nc.scalar.copy(out=sbf_cur[:, g, :], in_=Sf[:, g, :]),
                          reads=[Sfb[g]], writes=[sbfb_cur[g]])
            sq, sqb = sqr.next()
            ss, ssb = ssr.next()
            S.act(lambda e, sq=sq: nc.scalar.activation(out=sq, in_=psf(6)[0:64, :], func=AF.Square), reads=[PSB[6]], writes=[sqb])
            S.dve(lambda e, sq=sq, ss=ss: nc.vector.tensor_reduce(out=ss[:, 0, :], in_=sq.rearrange("p (h d) -> p h d", h=4),
                                                                  axis=AX.X, op=ALU.add), reads=[sqb], writes=[ssb])
            rstd_from_ss(ss, ssb, 4, 1.0 / 128, True)
            on, onb = onr.next()

            def fn_(e, on=on, ss=ss):
                inst = None
                for h in range(4):
                    inst = nc.vector.scalar_tensor_tensor(out=on[:, h * 128:(h + 1) * 128], in0=psf(6)[0:64, h * 128:(h + 1) * 128],
                                                          scalar=ss[:, 2, h:h + 1], in1=gains[0:64, 4, :], op0=ALU.mult,
                                                          op1=ALU.mult)
                return inst
            S.dve(fn_, reads=[PSB[6], ssb, constb], writes=[onb])
            ob, obb = obr.next()
            S.dve(lambda e, ob=ob, on=on, gv=gv: nc.vector.tensor_tensor(out=ob, in0=on, in1=gv, op=ALU.mult),
                  reads=[onb, gvb], writes=[obb])
            ptv = psh(5)
            transposes([(ptv[:, 512 + h * 64:512 + (h + 1) * 64], ob[:, h * 128:(h + 1) * 128]) for h in range(4)],
                       ident[0:64, 0:64], [obb, constb], [PSB[5]])
            S.act(lambda e, t0=t0, ptv=ptv: nc.scalar.copy(out=oT[:, 4:8, t0:t0 + 64],
                                                           in_=ptv[:, 512:768].rearrange("p (h t) -> p h t", h=4)),
                  reads=[PSB[5]], writes=[oTb[t0 // 128]])
        S.flush()
        A.release(m)

    def sb_attn(s):
        m = A.mark()
        qT = A.alloc([128, 4, SEQ], BF16)
        kT = A.alloc([128, 4, SEQ], BF16)
        v = A.alloc([128, NT, 512], BF16)
        qTb, kTb, vb_ = Buf(), Buf(), Buf()
        gc = gcol[:, 1, :]
        for hg in range(2):
            mp = A.mark()
            stage_state["pool"] = mk_stage()
            wbp = [(A.alloc([128, 8, 512], BF16), Buf()) for _ in range(2)]
            ws = mk_qk_ws(7)
            for bi, (nm, c0) in enumerate((("q", hg * 512), ("k", 1024 + hg * 512), ("v", 2048 + hg * 512))):
                wb, wbb = wbp[bi % 2]
                load_w(w_in_c[:, c0:c0 + 512], 8, 512, wb, wbb, gc)
                for i in range(NT):
                    pi = i % 2
                    proj_tok(wb, 512, i, pi, wbb)
                    if nm == "q":
                        qk_post(pi, i, ws, 2, qT, qTb, 128 ** -0.5, False)
                    elif nm == "k":
                        qk_post(pi, i, ws, 3, kT, kTb, 1.0, False)
                    else:
                        S.act(lambda e, pi=pi, i=i: nc.scalar.copy(out=v[:, i, :], in_=psf(pi)), reads=[PSB[pi]], writes=[vb_])
            S.flush()
            A.release(mp)
            espr = [Rot(A, 2, [128, 512], F32) for _ in range(2)]
            lor = [Rot(A, 2, [128, 512], BF16) for _ in range(2)]
            ar = [Rot(A, 2, [128, 512], BF16) for _ in range(2)]
            lsf = [(A.alloc([128, 512], F32), Buf()) for _ in range(2)]
            lsb = [(A.alloc([128, 512], BF16), Buf()) for _ in range(2)]
            for hp in range(2):
                for qb in range(4):
                    kts = list(range(4 * qb + 3, -1, -1))
                    for step, kt in enumerate(kts):
                        for st_ in range(2):
                            h = hp * 2 + st_
                            cc = max(0, kt - 4 * qb) * 128
                            ncol = 512 - cc
                            diag = kt >= 4 * qb
                            pz, pc, po = 0 + st_, 2 + st_, 4 + st_
                            q0 = qb * 512 + cc
                            mm_group(psf(pz)[:, 0:ncol], [(kT[:, h, kt * 128:(kt + 1) * 128], qT[:, h, q0:q0 + ncol])],
                                     [kTb, qTb], [PSB[pz]])
                            es, esb = espr[st_].next()
                            S.act(lambda e, es=es, pz=pz, ncol=ncol: nc.scalar.activation(
                                out=es[:, 0:ncol], in_=psf(pz)[:, 0:ncol], func=AF.Exp, scale=-1.0), reads=[PSB[pz]], writes=[esb])
                            S.act(lambda e, es=es, ncol=ncol: nc.scalar.activation(
                                out=es[:, 0:ncol], in_=es[:, 0:ncol], func=AF.Ln, bias=1.0), reads=[esb], writes=[esb])
                            lo, lob = lor[st_].next()
                            S.dve(lambda e, lo=lo, es=es, pz=pz, ncol=ncol: nc.vector.scalar_tensor_tensor(
                                out=lo[:, 0:ncol], in0=psf(pz)[:, 0:ncol], scalar=-1.0, in1=es[:, 0:ncol], op0=ALU.mult,
                                op1=ALU.subtract), reads=[PSB[pz], esb], writes=[lob])
                            if diag:
                                S.pool(lambda e, lo=lo: nc.gpsimd.tensor_tensor(out=lo[:, 0:128], in0=lo[:, 0:128], in1=strictT,
                                                                                 op=ALU.mult), reads=[lob, constb], writes=[lob])
                            lf, lfb = lsf[st_]
                            lb, lbb = lsb[st_]
                            if step == 0:
                                mm_group(psf(pc)[:, 0:ncol], [(triU, lo[:, 0:ncol])], [lob, constb], [PSB[pc]])
                            else:
                                mm_group(psf(pc)[:, 0:ncol], [(triU, lo[:, 0:ncol]), (ones128, lb[:, cc:512])],
                                         [lob, lbb, constb], [PSB[pc]])
                            S.dve(lambda e, es=es, pc=pc, ncol=ncol: nc.vector.tensor_tensor(
                                out=es[:, 0:ncol], in0=psf(pc)[:, 0:ncol], in1=es[:, 0:ncol], op=ALU.subtract),
                                reads=[PSB[pc], esb], writes=[esb])
                            a_, ab_ = ar[st_].next()
                            S.act(lambda e, a_=a_, es=es, ncol=ncol: nc.scalar.activation(
                                out=a_[:, 0:ncol], in_=es[:, 0:ncol], func=AF.Exp), reads=[esb], writes=[ab_])
                            if diag:
                                S.pool(lambda e, a_=a_: nc.gpsimd.tensor_tensor(out=a_[:, 0:128], in0=a_[:, 0:128], in1=strictT,
                                                                                 op=ALU.mult), reads=[ab_, constb], writes=[ab_])
                            S.pe(lambda e, po=po, cc=cc, a_=a_, ncol=ncol, kt=kt, h=h, step=step, kts=kts: nc.tensor.matmul(
                                psf(po)[:, cc:512], lhsT=v[:, kt, h * 128:(h + 1) * 128], rhs=a_[:, 0:ncol],
                                start=(step == 0), stop=(step == len(kts) - 1)), reads=[vb_, ab_], writes=[PSB[po]])
                            if step < len(kts) - 1:
                                if step == 0:
                                    def fl0(e, lf=lf, lo=lo, cc=cc, ncol=ncol):
                                        if cc > 0:
                                            nc.gpsimd.memset(lf[:, 0:cc], 0.0)
                                        return nc.gpsimd.tensor_copy(out=lf[:, cc:512], in_=lo[:, 0:ncol])
                                    S.pool(fl0, reads=[lob], writes=[lfb])
                                else:
                                    S.pool(lambda e, lf=lf, lo=lo, cc=cc, ncol=ncol: nc.gpsimd.tensor_tensor(
                                        out=lf[:, cc:512], in0=lf[:, cc:512], in1=lo[:, 0:ncol], op=ALU.add),
                                        reads=[lob, lfb], writes=[lfb])
                                S.pool(lambda e, lf=lf, lb=lb: nc.gpsimd.tensor_copy(out=lb, in_=lf), reads=[lfb], writes=[lbb])
                    for st_ in range(2):
                        h = hp * 2 + st_
                        po = 4 + st_
                        hh = hg * 4 + h
                        S.act(lambda e, po=po, hh=hh, qb=qb: nc.scalar.copy(out=oT[:, hh, qb * 512:(qb + 1) * 512], in_=psf(po)),
                              reads=[PSB[po]], writes=oTb[4 * qb:4 * qb + 4])
            S.flush()
            A.release(mp)
        A.release(m)

    def dump_h(s):
        m = A.mark()
        t = [(A.alloc([128, 1024], F32), Buf(), f"xin{k}") for k in range(2)]
        for i in range(NT):
            tt, tb, ch = t[i % 2]
            S.dma(lambda e, tt=tt, i=i: nc.sync.dma_start(out=tt, in_=h_scr[s, i * 128:(i + 1) * 128, :]), ch,
                  reads=[hb[s][i]], writes=[tb])
            S.dma(lambda e, tt=tt, i=i: nc.sync.dma_start(out=out[s, i * 128:(i + 1) * 128, :], in_=tt), f"st{i % 2}",
                  reads=[tb], writes=[])
        S.flush()
        A.release(m)

    for s in range(NSEQ):
        if stop != "const":
            phase_norm(s, x)
        if stop == "norm":
            continue
        if stop == "const":
            continue
        dsa(s)
        if stop == "dsa":
            continue
        phase_norm(s, x)
        gla(s)
        if stop == "gla":
            continue
        out_proj_and_norm(s, w_out_ab, x, h_scr)
        if stop == "mix0":
            dump_h(s)
            continue
        ffn(s, 0, False)
        if stop == "ffn0":
            dump_h(s)
            continue
        phase_norm(s, h_scr)
        sb_attn(s)
        out_proj_and_norm(s, w_out_c, h_scr, h_scr)
        if stop == "mix1":
            dump_h(s)
            continue
        ffn(s, 1, True)
    S.flush(final=True)
    return nc


def host_constants():
    c = {}
    c["c_ident"] = np.eye(128, dtype=np.float32)
    pos = np.arange(SEQ, dtype=np.float32)
    inv_a = np.power(np.float32(500000.0), -np.arange(16, dtype=np.float32) * 2.0 / 32).astype(np.float32)
    ang = pos[:, None] * inv_a[None, :]
    c["c_rope_a"] = np.concatenate([np.cos(ang), np.sin(ang)], axis=1).astype(np.float32)
    inv_i = np.power(np.float32(500000.0), -np.arange(8, dtype=np.float32) * 2.0 / 16).astype(np.float32)
    ang = pos[:, None] * inv_i[None, :]
    c["c_rope_i"] = np.concatenate([np.cos(ang), np.sin(ang)], axis=1).astype(np.float32)
    a = np.arange(64)
    m64 = np.zeros((64, 3, 64), np.float32)
    m64[:, 0, :] = (a[:, None] <= a[None, :]) * (-1.0 / 16.0)
    m64[:, 1, :] = (a[:, None] > a[None, :]) * (-1.0 / 16.0)
    m64[:, 2, :] = (a[:, None] <= a[None, :]) * 1.0
    c["c_m64"] = m64
    b = np.arange(128)
    m128 = np.zeros((128, 3, 128), np.float32)
    m128[:, 0, :] = (b[:, None] > b[None, :]) * 1.0
    m128[:, 1, :] = 1.0
    m128[:, 2, :] = (b[:, None] < b[None, :]) * 1.0
    c["c_m128"] = m128
    return c


_CACHE = {}


def kernel(x, g_mix, g_ffn, w_in_ab, gq_a, gk_a, w_gate_up, b_gate, g_gla, w_out_ab,
           w_in_c, gq_c, gk_c, w_out_c, w_up, w_down, _ncores=8, _stop=None):
    f = lambda a: np.ascontiguousarray(np.asarray(a, dtype=np.float32))
    x = f(x)
    nseq = x.shape[0] // _ncores
    shared = {
        "w_in_ab": f(w_in_ab)[0], "w_gate_up": f(w_gate_up)[0], "b_gate": f(b_gate), "w_out_ab": f(w_out_ab)[0],
        "w_in_c": f(w_in_c)[0], "w_out_c": f(w_out_c)[0], "w_up": f(w_up), "w_down": f(w_down),
    }
    gains = np.stack([np.broadcast_to(f(g).reshape(1, 128), (128, 128)) for g in (gq_a, gk_a, gq_c, gk_c, g_gla)], axis=1)
    shared["c_gains"] = np.ascontiguousarray(gains, dtype=np.float32)
    gcols = np.stack([f(g_mix)[0].reshape(8, 128).T, f(g_mix)[1].reshape(8, 128).T,
                      f(g_ffn)[0].reshape(8, 128).T, f(g_ffn)[1].reshape(8, 128).T], axis=1)
    shared["c_gcol"] = np.ascontiguousarray(gcols, dtype=np.float32)
    shared.update(host_constants())
    key = (nseq, _stop)
    if key not in _CACHE:
        _CACHE[key] = build_program(nseq, _stop)
    nc = _CACHE[key]
    in_maps = []
    for c in range(_ncores):
        d = dict(shared)
        d["x"] = np.ascontiguousarray(x[c * nseq:(c + 1) * nseq])
        in_maps.append(d)
    res = run_bass_kernel_spmd(nc, in_maps, core_ids=list(range(_ncores)))
    return np.concatenate([np.asarray(r["out"], dtype=np.float32) for r in res.results], axis=0)
```

```python
import numpy as np
import concourse.bass as bass
import concourse.mybir as mybir
from concourse.bass_utils import run_bass_kernel_spmd
from concourse.alu_op_type import AluOpType as ALU

F32 = mybir.dt.float32
BF16 = mybir.dt.bfloat16
AF = mybir.ActivationFunctionType
AX = mybir.AxisListType

SEQ = 2048
DM = 1024
NT = SEQ // 128
EPS = 1e-6
ABW = 3672
NEG = -1.0e30
BIS_ITERS = 18
ARENA_BYTES = 175 * 1024

_ENG_ATTR = {"pe": "tensor", "act": "scalar", "dve": "vector", "pool": "gpsimd", "sp": "sync"}


class Buf:
    __slots__ = ("lw", "rd")

    def __init__(self):
        self.lw = None
        self.rd = {}


class Sched:
    ENG = ("pe", "act", "dve", "pool", "sp")

    def __init__(self, nc):
        self.nc = nc
        self.sem = {e: nc.alloc_semaphore("sem_" + e) for e in self.ENG}
        self.cnt = {e: 0 for e in self.ENG}
        self.ops = {e: [] for e in self.ENG}
        self.waited = {e: {} for e in self.ENG}
        self.chan = {}

    def channel(self, name):
        if name not in self.chan:
            self.chan[name] = [self.nc.alloc_semaphore("ch_" + name), 0]
        return name

    def _deps(self, reads, writes):
        d = {}
        for b in reads:
            if b.lw is not None and d.get(b.lw[0], 0) < b.lw[1]:
                d[b.lw[0]] = b.lw[1]
        for b in writes:
            if b.lw is not None and d.get(b.lw[0], 0) < b.lw[1]:
                d[b.lw[0]] = b.lw[1]
            for k, v in b.rd.items():
                if d.get(k, 0) < v:
                    d[k] = v
        return d

    def op(self, eng, fn, reads=(), writes=()):
        d = self._deps(reads, writes)
        self.cnt[eng] += 1
        idx = self.cnt[eng]
        for b in reads:
            b.rd[eng] = idx
        for b in writes:
            b.lw = (eng, idx)
            b.rd = {}
        self.ops[eng].append((d, fn, None))

    def pe(self, fn, reads=(), writes=()):
        self.op("pe", fn, reads, writes)

    def act(self, fn, reads=(), writes=()):
        self.op("act", fn, reads, writes)

    def dve(self, fn, reads=(), writes=()):
        self.op("dve", fn, reads, writes)

    def pool(self, fn, reads=(), writes=()):
        self.op("pool", fn, reads, writes)

    def dma(self, fn, chan, reads=(), writes=(), queue="sp"):
        d = self._deps(reads, writes)
        c = self.chan[chan]
        c[1] += 16
        key = "ch:" + chan
        for b in reads:
            b.rd[key] = c[1]
        for b in writes:
            b.lw = (key, c[1])
            b.rd = {}
        self.ops[queue].append((d, fn, chan))

    def _semof(self, k):
        if k.startswith("ch:"):
            return self.chan[k[3:]][0]
        return self.sem[k]

    def flush(self, final=False):
        nc = self.nc
        if final:
            d = {}
            for name, c in self.chan.items():
                if c[1] > 0:
                    d["ch:" + name] = c[1]
            self.ops["sp"].append((d, None, None))
        with nc.Block() as blk:
            for eng in self.ENG:
                ops = self.ops[eng]

                def body(e, eng=eng, ops=ops):
                    w = self.waited[eng]
                    for d, fn, chan in ops:
                        for k, v in d.items():
                            if k == eng and eng == "pe":
                                continue
                            if w.get(k, 0) < v:
                                e.wait_ge(self._semof(k), v)
                                w[k] = v
                        if fn is None:
                            continue
                        inst = fn(e)
                        if chan is None:
                            inst.then_inc(self.sem[eng], 1)
                        else:
                            inst.then_inc(self.chan[chan][0], 16)

                getattr(blk, _ENG_ATTR[eng])(body)
        self.ops = {e: [] for e in self.ENG}


class Arena:
    def __init__(self, nc, nbytes, base=None):
        self.t = nc.alloc_sbuf_tensor("arena", [128, nbytes // 4], F32) if base is None else base
        self.top = 0
        self.nbytes = nbytes

    def mark(self):
        return self.top

    def release(self, m):
        self.top = m

    def alloc(self, shape, dtype):
        esz = 4 if dtype == F32 else 2
        n = int(np.prod(shape[1:]))
        nb = (n * esz + 63) // 64 * 64
        off = self.top
        self.top += nb
        assert self.top <= self.nbytes, ("arena overflow", self.top)
        ap = self.t[0:shape[0], off // 4:(off + nb) // 4]
        if dtype != F32:
            ap = ap.bitcast(dtype)
        ap = ap[:, 0:n]
        if len(shape) == 3:
            ap = ap.rearrange("p (a b) -> p a b", a=shape[1])
        elif len(shape) == 4:
            ap = ap.rearrange("p (a b c) -> p a b c", a=shape[1], b=shape[2])
        return ap


class Rot:
    def __init__(self, arena, n, shape, dtype):
        self.items = [(arena.alloc(shape, dtype), Buf()) for _ in range(n)]
        self.i = 0

    def next(self):
        it = self.items[self.i % len(self.items)]
        self.i += 1
        return it


def run_streams(gens):
    gens = list(gens)
    while gens:
        for g in list(gens):
            try:
                next(g)
            except StopIteration:
                gens.remove(g)


def build_program(NSEQ=2, stop=None):
    nc = bass.Bass("TRN2", target_bir_lowering=False)
    S = Sched(nc)

    def dram_in(name, shape):
        return nc.dram_tensor(name, shape, F32, kind="ExternalInput").ap()

    x = dram_in("x", [NSEQ, SEQ, DM])
    w_in_ab = dram_in("w_in_ab", [DM, ABW])
    w_gate_up = dram_in("w_gate_up", [16, 256])
    b_gate = dram_in("b_gate", [1, 256])
    w_out_ab = dram_in("w_out_ab", [DM, DM])
    w_in_c = dram_in("w_in_c", [DM, 3 * DM])
    w_out_c = dram_in("w_out_c", [DM, DM])
    w_up = dram_in("w_up", [2, DM, 4 * DM])
    w_down = dram_in("w_down", [2, 4 * DM, DM])
    c_ident = dram_in("c_ident", [128, 128])
    c_rope_a = dram_in("c_rope_a", [SEQ, 32])
    c_rope_i = dram_in("c_rope_i", [SEQ, 16])
    c_m64 = dram_in("c_m64", [64, 3, 64])
    c_m128 = dram_in("c_m128", [128, 3, 128])
    c_gains = dram_in("c_gains", [128, 5, 128])
    c_gcol = dram_in("c_gcol", [128, 4, 8])
    out = nc.dram_tensor("out", [NSEQ, SEQ, DM], F32, kind="ExternalOutput").ap()
    h_scr = nc.dram_tensor("h_scr", [NSEQ, SEQ, DM], F32).ap()
    hb = [[Buf() for _ in range(NT)] for _ in range(NSEQ)]

    A = Arena(nc, ARENA_BYTES)
    PS = [nc.alloc_psum_tensor(f"psb{i}", [128, 512], F32) for i in range(8)]
    PSB = [Buf() for _ in range(8)]

    def psf(i):
        return PS[i][:, :]

    def psh(i):
        return PS[i][:, :].bitcast(BF16)

    for nm in ("c0", "c1", "c2", "c3", "xin0", "xin1", "st0", "st1", "stg0", "stg1", "hres"):
        S.channel(nm)

    nT_raw = A.alloc([128, 8192], F32)
    nT = nT_raw.bitcast(BF16).rearrange("p (a b) -> p a b", a=8)
    nTb = [Buf() for _ in range(NT)]
    oT_raw = A.alloc([128, 8192], F32)
    oT = oT_raw.bitcast(BF16).rearrange("p (a b) -> p a b", a=8)
    oTb = [Buf() for _ in range(NT)]
    identf = A.alloc([128, 128], F32)
    ident = A.alloc([128, 128], BF16)
    ropeA = A.alloc([128, NT, 32], F32)
    ropeI = A.alloc([128, NT, 16], F32)
    m64 = A.alloc([64, 3, 64], F32)
    m128f = A.alloc([128, 3, 128], F32)
    m128 = A.alloc([128, 3, 128], BF16)
    gains = A.alloc([128, 5, 128], F32)
    gcol = A.alloc([128, 4, 8], F32)
    wg = A.alloc([32, 256], F32)
    thr_all = A.alloc([128, 1], F32)
    constb = Buf()

    def bc_row(ap):
        r = ap.partition_broadcast(128)
        if len(r.shape) == 3:
            r = r.rearrange("p a b -> p (a b)")
        return r

    def dma_simple(out_ap, in_ap, chan, writes, reads=(), ncdma=True):
        S.dma(lambda e: nc.sync.dma_start(out=out_ap, in_=in_ap), chan, reads=reads, writes=writes)

    dma_simple(identf, c_ident, "c0", [constb])
    dma_simple(ropeA, c_rope_a.rearrange("(i p) c -> p i c", p=128), "c0", [constb])
    dma_simple(ropeI, c_rope_i.rearrange("(i p) c -> p i c", p=128), "c0", [constb])
    dma_simple(m64, c_m64, "c0", [constb])
    dma_simple(m128f, c_m128, "c0", [constb])
    dma_simple(gains, c_gains, "c0", [constb])
    dma_simple(wg[0:16, :], w_gate_up, "c0", [constb])
    dma_simple(wg[16:17, :], b_gate, "c0", [constb])
    dma_simple(gcol, c_gcol, "c0", [constb])
    S.dve(lambda e: nc.vector.tensor_copy(out=ident, in_=identf), reads=[constb], writes=[constb])
    S.dve(lambda e: nc.vector.tensor_copy(out=m128, in_=m128f), reads=[constb], writes=[constb])
    S.dve(lambda e: nc.vector.memset(thr_all, -1.0e29), reads=[], writes=[constb])
    S.flush()
    base_mark = A.mark()

    triU = m128[:, 0, :]
    ones128 = m128[:, 1, :]
    strictT = m128[:, 2, :]
    tri_incl = m64[:, 0, :]
    tri_rev = m64[:, 1, :]
    causT = m64[:, 2, :]

    def mk_stage(ar=None):
        ar = A if ar is None else ar
        return [(ar.alloc([128, 2048], F32), Buf(), S.channel(f"stg{i}")) for i in range(2)]

    stage_state = {"pool": None, "i": 0}

    def load_w(src2d, KC, ncols, dst, dstb, gc=None):
        srcv = src2d.rearrange("(k p) c -> p k c", p=128)
        kstep = max(1, 2048 // ncols)
        for k0 in range(0, KC, kstep):
            kn = min(kstep, KC - k0)
            st, stb, ch = stage_state["pool"][stage_state["i"] % 2]
            stage_state["i"] += 1
            stv = st[:, 0:kn * ncols].rearrange("p (k c) -> p k c", k=kn)
            S.dma(lambda e, stv=stv, k0=k0, kn=kn: nc.sync.dma_start(out=stv, in_=srcv[:, k0:k0 + kn, :]),
                  ch, writes=[stb])
            if gc is None:
                S.pool(lambda e, stv=stv, k0=k0, kn=kn: nc.gpsimd.tensor_copy(out=dst[:, k0:k0 + kn, :], in_=stv),
                       reads=[stb], writes=[dstb])
            else:
                S.pool(lambda e, stv=stv, k0=k0, kn=kn: nc.gpsimd.tensor_tensor(
                    out=dst[:, k0:k0 + kn, :], in0=stv,
                    in1=gc[:, k0:k0 + kn].unsqueeze(2).to_broadcast([128, kn, ncols]), op=ALU.mult),
                    reads=[stb, constb], writes=[dstb])

    def mm_group(out_ap, pairs, rd, wr):
        def f(e):
            n = len(pairs)
            inst = None
            for q, (l, r) in enumerate(pairs):
                inst = nc.tensor.matmul(out_ap, lhsT=l, rhs=r, start=(q == 0), stop=(q == n - 1))
            return inst
        S.pe(f, reads=rd, writes=wr)

    def transposes(outs_ins, idn, rd, wr):
        def f(e):
            inst = None
            for o, i_ in outs_ins:
                inst = nc.tensor.transpose(o, i_, idn)
            return inst
        S.pe(f, reads=rd, writes=wr)

    def rstd_from_ss(ssv, ssb, n, inv_n, add_eps):
        P = ssv.shape[0]
        if add_eps:
            S.dve(lambda e: nc.vector.tensor_scalar(out=ssv[:, 0, :], in0=ssv[:, 0, :], scalar1=inv_n, scalar2=EPS,
                                                    op0=ALU.mult, op1=ALU.add), reads=[ssb], writes=[ssb])
            S.act(lambda e: nc.scalar.activation(out=ssv[:, 1, :], in_=ssv[:, 0, :], func=AF.Ln), reads=[ssb], writes=[ssb])
        else:
            S.act(lambda e: nc.scalar.activation(out=ssv[:, 1, :], in_=ssv[:, 0, :], func=AF.Ln, scale=inv_n),
                  reads=[ssb], writes=[ssb])
        S.act(lambda e: nc.scalar.activation(out=ssv[:, 2, :], in_=ssv[:, 1, :], func=AF.Exp, scale=-0.5),
              reads=[ssb], writes=[ssb])

    def rope(xv, xb, H, half, table, i, tmp, tmpb, P=128, prow=None):
        if prow is None:
            cs = table[0:P, i, :]
        else:
            cs = prow
        cos = cs[:, 0:half].unsqueeze(1).to_broadcast([P, H, half])
        sin = cs[:, half:2 * half].unsqueeze(1).to_broadcast([P, H, half])
        x1 = xv[:, :, 0:half]
        x2 = xv[:, :, half:2 * half]

        def f1(e):
            nc.gpsimd.tensor_tensor(out=tmp[:, 0], in0=x1, in1=cos, op=ALU.mult)
            nc.gpsimd.tensor_tensor(out=tmp[:, 1], in0=x2, in1=sin, op=ALU.mult)
            nc.gpsimd.tensor_tensor(out=tmp[:, 2], in0=x2, in1=cos, op=ALU.mult)
            return nc.gpsimd.tensor_tensor(out=tmp[:, 3], in0=x1, in1=sin, op=ALU.mult)
        S.pool(f1, reads=[xb, constb], writes=[tmpb])

        def f2(e):
            nc.gpsimd.tensor_tensor(out=x1, in0=tmp[:, 0], in1=tmp[:, 1], op=ALU.subtract)
            return nc.gpsimd.tensor_tensor(out=x2, in0=tmp[:, 2], in1=tmp[:, 3], op=ALU.add)
        S.pool(f2, reads=[tmpb], writes=[xb])

    def norm_transpose(xt, xtb, i, ws):
        junk, jb = ws["junk"].next()
        ss, sb = ws["ss"].next()
        nb, nbb = ws["nb"].next()
        S.act(lambda e: nc.scalar.activation(out=junk, in_=xt, func=AF.Square, accum_out=ss[:, 0, 0:1]),
              reads=[xtb], writes=[jb, sb])
        rstd_from_ss(ss, sb, 1, 1.0 / DM, True)
        S.dve(lambda e: nc.vector.tensor_scalar(out=nb, in0=xt, scalar1=ss[:, 2, 0:1], scalar2=None, op0=ALU.mult),
              reads=[xtb, sb], writes=[nbb])
        pb = ws["psT"]
        pv = psh(pb)
        transposes([(pv[:, kc * 128:(kc + 1) * 128], nb[:, kc * 128:(kc + 1) * 128]) for kc in range(8)], ident,
                   [nbb, constb], [PSB[pb]])
        S.act(lambda e: nc.scalar.copy(out=nT[:, :, i * 128:(i + 1) * 128],
                                       in_=pv[:, 0:1024].rearrange("p (k t) -> p k t", k=8)),
              reads=[PSB[pb]], writes=[nTb[i]])

    def mk_norm_ws(psT):
        return {"junk": Rot(A, 1, [128, 1024], BF16), "ss": Rot(A, 2, [128, 3, 1], F32),
                "nb": Rot(A, 2, [128, 1024], BF16), "psT": psT}

    def phase_norm(s, src):
        m = A.mark()
        ws = mk_norm_ws(7)
        xin = [(A.alloc([128, 1024], F32), Buf(), f"xin{k}") for k in range(2)]
        for i in range(NT):
            xt, xb, ch = xin[i % 2]
            S.dma(lambda e, xt=xt, i=i: nc.sync.dma_start(out=xt, in_=src[s, i * 128:(i + 1) * 128, :]), ch,
                  reads=[hb[s][i]], writes=[xb])
            norm_transpose(xt, xb, i, ws)
        S.flush()
        A.release(m)

    def qk_post(ps_i, i, ws, gidx, dstT, dst_b, scale, do_rope):
        sq, sqb = ws["sq"].next()
        ss, ssb = ws["ss4"].next()
        qn, qnb = ws["qn"].next()
        qh, qhb = ws["qh"].next()
        pv = psf(ps_i)
        S.act(lambda e: nc.scalar.activation(out=sq, in_=pv, func=AF.Square), reads=[PSB[ps_i]], writes=[sqb])
        S.dve(lambda e: nc.vector.tensor_reduce(out=ss[:, 0, :], in_=sq.rearrange("p (h d) -> p h d", h=4), axis=AX.X,
                                                op=ALU.add), reads=[sqb], writes=[ssb])
        rstd_from_ss(ss, ssb, 4, 1.0 / 128, True)

        def f(e):
            inst = None
            for h in range(4):
                inst = nc.vector.scalar_tensor_tensor(out=qn[:, h * 128:(h + 1) * 128], in0=pv[:, h * 128:(h + 1) * 128],
                                                      scalar=ss[:, 2, h:h + 1], in1=gains[:, gidx, :], op0=ALU.mult,
                                                      op1=ALU.mult)
            return inst
        S.dve(f, reads=[PSB[ps_i], ssb, constb], writes=[qnb])
        if do_rope:
            tmp, tmpb = ws["rtmp"].next()
            rope(qn.rearrange("p (h d) -> p h d", h=4), qnb, 4, 16, ropeA, i, tmp, tmpb)
        S.act(lambda e: nc.scalar.activation(out=qh, in_=qn, func=AF.Copy, scale=scale), reads=[qnb], writes=[qhb])
        pt = ws["psT"]
        ptv = psh(pt)
        transposes([(ptv[:, h * 128:(h + 1) * 128], qh[:, h * 128:(h + 1) * 128]) for h in range(4)], ident,
                   [qhb, constb], [PSB[pt]])
        S.act(lambda e: nc.scalar.copy(out=dstT[:, :, i * 128:(i + 1) * 128],
                                       in_=ptv[:, 0:512].rearrange("p (h t) -> p h t", h=4)),
              reads=[PSB[pt]], writes=[dst_b])

    def proj_tok(wb, ncols, i, ps_i, wbb, M=128, t0=None):
        t0 = i * 128 if t0 is None else t0
        mm_group(psf(ps_i)[0:M, 0:ncols], [(nT[:, kc, t0:t0 + M], wb[:, kc, 0:ncols]) for kc in range(8)],
                 [nTb[t0 // 128], wbb], [PSB[ps_i]])

    def mk_qk_ws(psT):
        return {"sq": Rot(A, 2, [128, 512], F32), "ss4": Rot(A, 2, [128, 3, 4], F32), "qn": Rot(A, 2, [128, 512], F32),
                "qh": Rot(A, 2, [128, 512], BF16), "rtmp": Rot(A, 2, [128, 4, 4, 16], F32), "psT": psT}

    def out_proj_and_norm(s, wout_dram, src, dst):
        m = A.mark()
        stage_state["pool"] = mk_stage()
        wo = A.alloc([128, 8, DM], BF16)
        wob = Buf()
        for c0 in range(0, DM, 256):
            load_w(wout_dram[:, c0:c0 + 256], 8, 256, wo[:, :, c0:c0 + 256], wob)
        ws = mk_norm_ws(7)
        hin = [(A.alloc([128, 1024], F32), Buf(), f"xin{k}") for k in range(2)]
        hnew = [(A.alloc([128, 1024], F32), Buf(), f"st{k}") for k in range(2)]
        for i in range(NT):
            hi_, hib, ch = hin[i % 2]
            hn, hnb, sch = hnew[i % 2]
            S.dma(lambda e, hi_=hi_, i=i: nc.sync.dma_start(out=hi_, in_=src[s, i * 128:(i + 1) * 128, :]), ch,
                  reads=[hb[s][i]], writes=[hib])
            for half in range(2):
                pi = 0 + 2 * (i % 2) + half
                mm_group(psf(pi), [(oT[:, c, i * 128:(i + 1) * 128], wo[:, c, half * 512:(half + 1) * 512])
                                   for c in range(8)], [oTb[i], wob], [PSB[pi]])
                S.dve(lambda e, pi=pi, half=half, hn=hn, hi_=hi_: nc.vector.tensor_tensor(
                    out=hn[:, half * 512:(half + 1) * 512], in0=psf(pi), in1=hi_[:, half * 512:(half + 1) * 512],
                    op=ALU.add), reads=[PSB[pi], hib], writes=[hnb])
            S.dma(lambda e, hn=hn, i=i: nc.sync.dma_start(out=dst[s, i * 128:(i + 1) * 128, :], in_=hn), sch,
                  reads=[hnb], writes=[hb[s][i]])
            norm_transpose(hn, hnb, i, ws)
        S.flush()
        A.release(m)

    def ffn(s, layer, final):
        m = A.mark()
        stage_state["pool"] = mk_stage()
        hres = A.alloc([128, NT, DM], F32)
        hresb = [Buf() for _ in range(NT)]
        actT = A.alloc([128, 4, SEQ], BF16)
        actb = [Buf() for _ in range(4)]
        AO = Arena(nc, 32768, base=oT_raw)
        wub = [(AO.alloc([128, 8, 512], BF16), Buf()) for _ in range(2)]
        wdb = [(AO.alloc([128, 4, DM], BF16), Buf()) for _ in range(2)]
        rr = Rot(A, 2, [128, 512], F32)
        for i in range(NT):
            S.dma(lambda e, i=i: nc.sync.dma_start(out=hres[:, i, :], in_=h_scr[s, i * 128:(i + 1) * 128, :]), "hres",
                  reads=[hb[s][i]], writes=[hresb[i]])
        gc = gcol[:, 2 + layer, :]
        for g in range(8):
            wu, wubb = wub[g % 2]
            wd, wdbb = wdb[g % 2]
            load_w(w_up[layer, :, g * 512:(g + 1) * 512], 8, 512, wu, wubb, gc)
            load_w(w_down[layer, g * 512:(g + 1) * 512, :], 4, DM, wd, wdbb)
            for fc in range(4):
                for tb in range(4):
                    pi = (fc * 4 + tb) % 4
                    mm_group(psf(pi), [(wu[:, kc, fc * 128:(fc + 1) * 128], nT[:, kc, tb * 512:(tb + 1) * 512])
                                       for kc in range(8)], nTb[4 * tb:4 * tb + 4] + [wubb], [PSB[pi]])
                    r, rb = rr.next()
                    S.act(lambda e, r=r, pi=pi: nc.scalar.activation(out=r, in_=psf(pi), func=AF.Relu),
                          reads=[PSB[pi]], writes=[rb])
                    S.pool(lambda e, r=r, fc=fc, tb=tb: nc.gpsimd.tensor_tensor(
                        out=actT[:, fc, tb * 512:(tb + 1) * 512], in0=r, in1=r, op=ALU.mult),
                        reads=[rb], writes=[actb[tb]])
            for ti in range(NT):
                for half in range(2):
                    pi = 4 + (ti * 2 + half) % 4
                    mm_group(psf(pi), [(actT[:, fc, ti * 128:(ti + 1) * 128], wd[:, fc, half * 512:(half + 1) * 512])
                                       for fc in range(4)], [actb[ti // 4], wdbb], [PSB[pi]])
                    S.dve(lambda e, pi=pi, ti=ti, half=half: nc.vector.tensor_tensor(
                        out=hres[:, ti, half * 512:(half + 1) * 512], in0=psf(pi),
                        in1=hres[:, ti, half * 512:(half + 1) * 512], op=ALU.add),
                        reads=[PSB[pi], hresb[ti]], writes=[hresb[ti]])
        dst = out if final else h_scr
        for i in range(NT):
            S.dma(lambda e, i=i: nc.sync.dma_start(out=dst[s, i * 128:(i + 1) * 128, :], in_=hres[:, i, :]),
                  f"st{i % 2}", reads=[hresb[i]], writes=[hb[s][i]])
        S.flush()
        A.release(m)

    def dsa(s):
        m = A.mark()
        kaT = A.alloc([128, 4, SEQ], BF16)
        qaT = A.alloc([128, 4, SEQ], BF16)
        va = A.alloc([128, NT, 4, 132], BF16)
        iqT = A.alloc([128, 4, SEQ], BF16)
        ikT = A.alloc([128, SEQ], BF16)
        iws = A.alloc([128, NT, 8], F32)
        kaTb, qaTb, vab, iqTb, ikTb, iwsb = Buf(), Buf(), Buf(), Buf(), Buf(), Buf()
        m2 = A.mark()
        AO = Arena(nc, 32768, base=oT_raw)
        stage_state["pool"] = mk_stage(AO)
        wbp = [(AO.alloc([128, 8, 512], BF16), Buf()) for _ in range(2)]
        ws = mk_qk_ws(7)
        cp = Rot(A, 2, [128, 512], F32)
        cpb = Rot(A, 2, [128, 512], BF16)
        itmp = Rot(A, 2, [128, 4, 8, 8], F32)
        ikf = Rot(A, 2, [128, 72], F32)
        ikd = Rot(A, 2, [128, 128], BF16)
        gc = gcol[:, 0, :]
        S.pool(lambda e: nc.gpsimd.memset(va[:, :, :, 128:129], 1.0), writes=[vab])
        blocks = [("qa", 0), ("ka", 512), ("va", 1024), ("iq", 1536), ("ik", 2048)]
        for bi, (nm, c0) in enumerate(blocks):
            ncols = 72 if nm == "ik" else 512
            wb, wbb = wbp[bi % 2]
            load_w(w_in_ab[:, c0:c0 + ncols], 8, ncols, wb[:, :, 0:ncols], wbb, gc)
            for i in range(NT):
                pi = i % 2
                proj_tok(wb, ncols, i, pi, wbb)
                if nm == "qa":
                    qk_post(pi, i, ws, 0, qaT, qaTb, 128 ** -0.5, True)
                elif nm == "ka":
                    qk_post(pi, i, ws, 1, kaT, kaTb, 1.0, True)
                elif nm == "va":
                    S.act(lambda e, pi=pi, i=i: nc.scalar.copy(out=va[:, i, :, 0:128],
                                                              in_=psf(pi).rearrange("p (h d) -> p h d", h=4)),
                          reads=[PSB[pi]], writes=[vab])
                elif nm == "iq":
                    c, cb = cp.next()
                    S.act(lambda e, c=c, pi=pi: nc.scalar.copy(out=c, in_=psf(pi)), reads=[PSB[pi]], writes=[cb])
                    tmp, tmpb = itmp.next()
                    rope(c.rearrange("p (h d) -> p h d", h=8), cb, 8, 8, ropeI, i, tmp, tmpb)
                    ch_, chb = cpb.next()
                    S.act(lambda e, c=c, ch_=ch_: nc.scalar.copy(out=ch_, in_=c), reads=[cb], writes=[chb])
                    ptv = psh(7)
                    transposes([(ptv[:, p * 128:(p + 1) * 128], ch_[:, p * 128:(p + 1) * 128]) for p in range(4)], ident,
                               [chb, constb], [PSB[7]])
                    S.act(lambda e, i=i, ptv=ptv: nc.scalar.copy(out=iqT[:, :, i * 128:(i + 1) * 128],
                                                                 in_=ptv[:, 0:512].rearrange("p (h t) -> p h t", h=4)),
                          reads=[PSB[7]], writes=[iqTb])
                else:
                    f_, fb = ikf.next()
                    S.act(lambda e, f_=f_, pi=pi: nc.scalar.copy(out=f_, in_=psf(pi)[:, 0:72]), reads=[PSB[pi]], writes=[fb])
                    tmp, tmpb = itmp.next()
                    rope(f_[:, 0:64].rearrange("p (h d) -> p h d", h=1), fb, 1, 8, ropeI, i, tmp[:, :, 0:1, :], tmpb)
                    d_, db = ikd.next()

                    def fcp(e, f_=f_, d_=d_, i=i):
                        nc.vector.tensor_copy(out=d_[:, 0:64], in_=f_[:, 0:64])
                        nc.vector.tensor_copy(out=d_[:, 64:128], in_=f_[:, 0:64])
                        return nc.vector.tensor_scalar(out=iws[:, i, :], in0=f_[:, 64:72], scalar1=1.0 / (8.0 * 8.0 ** 0.5),
                                                       scalar2=None, op0=ALU.mult)
                    S.dve(fcp, reads=[fb], writes=[db, iwsb])
                    ptv = psh(7)
                    transposes([(ptv[:, 0:128], d_)], ident, [db, constb], [PSB[7]])
                    S.act(lambda e, i=i, ptv=ptv: nc.scalar.copy(out=ikT[:, i * 128:(i + 1) * 128], in_=ptv[:, 0:128]),
                          reads=[PSB[7]], writes=[ikTb])
        S.flush()
        A.release(m2)
        AN = Arena(nc, 32768, base=nT_raw)
        score = Rot(AN, 2, [128, SEQ], F32)
        junk = AN.alloc([128, SEQ], BF16)
        junkb = Buf()
        maskr = Rot(AN, 2, [128, SEQ], BF16)
        maskTr = Rot(A, 2, [128, NT, 128], BF16)
        tmpf = Rot(A, 3, [128, 512], F32)
        ptr = Rot(A, 2, [128, 512], BF16)
        ptmr = Rot(A, 2, [128, 512], BF16)
        bsr = Rot(A, 2, [128, 8], F32)
        rzr = Rot(A, 2, [128, 4], F32)
        oar = Rot(A, 2, [128, 512], BF16)
        for j in range(NT):
            W = (j + 1) * 128
            sc, scb = score.next()
            for h in range(8):
                p0 = (h % 2) * 64
                pair = h // 2
                for kb in range((W + 511) // 512):
                    c0 = kb * 512
                    cw = min(512, W - c0)
                    pi = (h * 4 + kb) % 2
                    mm_group(psf(pi)[:, 0:cw], [(iqT[p0:p0 + 64, pair, j * 128:(j + 1) * 128], ikT[p0:p0 + 64, c0:c0 + cw])],
                             [iqTb, ikTb], [PSB[pi]])
                    t_, tb_ = tmpf.next()
                    S.act(lambda e, t_=t_, pi=pi, cw=cw: nc.scalar.activation(out=t_[:, 0:cw], in_=psf(pi)[:, 0:cw], func=AF.Relu),
                          reads=[PSB[pi]], writes=[tb_])
                    if h == 0:
                        S.dve(lambda e, t_=t_, sc=sc, c0=c0, cw=cw, j=j: nc.vector.tensor_scalar(
                            out=sc[:, c0:c0 + cw], in0=t_[:, 0:cw], scalar1=iws[:, j, 0:1], scalar2=None, op0=ALU.mult),
                            reads=[tb_, iwsb], writes=[scb])
                    else:
                        S.dve(lambda e, t_=t_, sc=sc, c0=c0, cw=cw, j=j, h=h: nc.vector.scalar_tensor_tensor(
                            out=sc[:, c0:c0 + cw], in0=t_[:, 0:cw], scalar=iws[:, j, h:h + 1], in1=sc[:, c0:c0 + cw],
                            op0=ALU.mult, op1=ALU.add), reads=[tb_, iwsb, scb], writes=[scb])
            S.dve(lambda e, sc=sc, W=W: nc.vector.memset(sc[0:64, W - 64:W], NEG), reads=[scb], writes=[scb])
            if j >= 2:
                bs, bsb = bsr.next()

                def f0(e, sc=sc, bs=bs, W=W):
                    nc.vector.tensor_reduce(out=bs[:, 0:1], in_=sc[:, 0:W - 64], axis=AX.X, op=ALU.min)
                    return nc.vector.tensor_reduce(out=bs[:, 1:2], in_=sc[:, 0:W], axis=AX.X, op=ALU.max)
                S.dve(f0, reads=[scb], writes=[bsb])
                S.dve(lambda e, bs=bs: nc.vector.tensor_tensor(out=bs[:, 1:2], in0=bs[:, 1:2], in1=bs[:, 0:1], op=ALU.subtract),
                      reads=[bsb], writes=[bsb])
                for k in range(1, BIS_ITERS + 1):
                    f = 2.0 ** (-k)
                    S.dve(lambda e, bs=bs, f=f: nc.vector.tensor_scalar(out=bs[:, 2:3], in0=bs[:, 1:2], scalar1=f,
                                                                        scalar2=bs[:, 0:1], op0=ALU.mult, op1=ALU.add),
                          reads=[bsb], writes=[bsb])
                    S.dve(lambda e, bs=bs, sc=sc, W=W: nc.vector.tensor_scalar(
                        out=junk[:, 0:W], in0=sc[:, 0:W], scalar1=bs[:, 2:3], scalar2=None, op0=ALU.is_ge, op1=ALU.add,
                        accum_out=bs[:, 3:4]), reads=[bsb, scb], writes=[bsb, junkb])
                    S.dve(lambda e, bs=bs: nc.vector.tensor_scalar(out=bs[:, 4:5], in0=bs[:, 3:4], scalar1=255.5,
                                                                   scalar2=bs[:, 1:2], op0=ALU.is_ge, op1=ALU.mult),
                          reads=[bsb], writes=[bsb])
                    S.dve(lambda e, bs=bs, f=f: nc.vector.scalar_tensor_tensor(out=bs[:, 0:1], in0=bs[:, 4:5], scalar=f,
                                                                               in1=bs[:, 0:1], op0=ALU.mult, op1=ALU.add),
                          reads=[bsb], writes=[bsb])
                thr = bs[:, 0:1]
                thrb = bsb
            else:
                thr = thr_all[:, 0:1]
                thrb = constb
            mk, mkb = maskr.next()
            S.dve(lambda e, mk=mk, sc=sc, W=W, thr=thr: nc.vector.tensor_scalar(out=mk[:, 0:W], in0=sc[:, 0:W], scalar1=thr,
                                                                               scalar2=None, op0=ALU.is_ge),
                  reads=[scb, thrb], writes=[mkb])
            mT, mTb = maskTr.next()
            for g in range((j + 4) // 4):
                kts = list(range(4 * g, min(4 * g + 4, j + 1)))
                pv = psh(2)
                transposes([(pv[:, q * 128:(q + 1) * 128], mk[:, kt * 128:(kt + 1) * 128]) for q, kt in enumerate(kts)],
                           ident, [mkb, constb], [PSB[2]])
                n = len(kts)
                S.act(lambda e, mT=mT, g=g, n=n, pv=pv: nc.scalar.copy(
                    out=mT[:, 4 * g:4 * g + n, :], in_=pv[:, 0:n * 128].rearrange("p (k t) -> p k t", k=n)),
                    reads=[PSB[2]], writes=[mTb])
            for h in range(4):
                po = 5 + h // 2
                ov = psf(po)[:, (h % 2) * 132:(h % 2) * 132 + 129]
                for g in range((j + 4) // 4):
                    kts = list(range(4 * g, min(4 * g + 4, j + 1)))
                    n = len(kts)
                    pi = 3 + (h * 4 + g) % 2

                    def fs(e, kts=kts, pi=pi, h=h, j=j):
                        inst = None
                        for q, kt in enumerate(kts):
                            inst = nc.tensor.matmul(psf(pi)[:, q * 128:(q + 1) * 128], lhsT=kaT[:, h, kt * 128:(kt + 1) * 128],
                                                    rhs=qaT[:, h, j * 128:(j + 1) * 128], start=True, stop=True)
                        return inst
                    S.pe(fs, reads=[kaTb, qaTb], writes=[PSB[pi]])
                    pt, ptb = ptr.next()
                    S.act(lambda e, pt=pt, pi=pi, n=n: nc.scalar.activation(out=pt[:, 0:n * 128], in_=psf(pi)[:, 0:n * 128],
                                                                           func=AF.Exp), reads=[PSB[pi]], writes=[ptb])
                    pm, pmb = ptmr.next()
                    S.dve(lambda e, pm=pm, pt=pt, mT=mT, g=g, n=n: nc.vector.tensor_tensor(
                        out=pm[:, 0:n * 128], in0=pt[:, 0:n * 128], in1=mT[:, 4 * g:4 * g + n, :].rearrange("p k t -> p (k t)"),
                        op=ALU.mult), reads=[ptb, mTb], writes=[pmb])

                    def fo(e, kts=kts, pm=pm, ov=ov, h=h, j=j):
                        inst = None
                        for q, kt in enumerate(kts):
                            inst = nc.tensor.matmul(ov, lhsT=pm[:, q * 128:(q + 1) * 128], rhs=va[:, kt, h, 0:129],
                                                    start=(kt == 0), stop=(kt == j))
                        return inst
                    S.pe(fo, reads=[pmb, vab], writes=[PSB[po]])
            rz, rzb = rzr.next()

            def frz(e, rz=rz):
                inst = None
                for h in range(4):
                    zc = (h % 2) * 132 + 128
                    inst = nc.vector.reciprocal(out=rz[:, h:h + 1], in_=psf(5 + h // 2)[:, zc:zc + 1])
                return inst
            S.dve(frz, reads=[PSB[5], PSB[6]], writes=[rzb])
            oa, oab = oar.next()

            def foa(e, rz=rz, oa=oa):
                inst = None
                for h in range(4):
                    c = (h % 2) * 132
                    inst = nc.scalar.activation(out=oa[:, h * 128:(h + 1) * 128], in_=psf(5 + h // 2)[:, c:c + 128],
                                                func=AF.Identity, scale=rz[:, h:h + 1])
                return inst
            S.act(foa, reads=[PSB[5], PSB[6], rzb], writes=[oab])
            ptv = psh(7)
            transposes([(ptv[:, h * 128:(h + 1) * 128], oa[:, h * 128:(h + 1) * 128]) for h in range(4)], ident,
                       [oab, constb], [PSB[7]])
            S.act(lambda e, j=j, ptv=ptv: nc.scalar.copy(out=oT[:, 0:4, j * 128:(j + 1) * 128],
                                                         in_=ptv[:, 0:512].rearrange("p (h t) -> p h t", h=4)),
                  reads=[PSB[7]], writes=[oTb[j]])
        S.flush()
        A.release(m)

    def gla(s):
        m = A.mark()
        stage_state["pool"] = mk_stage()
        wq = A.alloc([128, 8, 256], BF16)
        wk = A.alloc([128, 8, 256], BF16)
        wv = A.alloc([128, 8, 512], BF16)
        wl = A.alloc([128, 8, 16], BF16)
        wo_ = A.alloc([128, 8, 512], BF16)
        wb_ = Buf()
        gc = gcol[:, 0, :]
        load_w(w_in_ab[:, 2120:2376], 8, 256, wq, wb_, gc)
        load_w(w_in_ab[:, 2376:2632], 8, 256, wk, wb_, gc)
        load_w(w_in_ab[:, 2632:3144], 8, 512, wv, wb_, gc)
        load_w(w_in_ab[:, 3144:3160], 8, 16, wl, wb_, gc)
        load_w(w_in_ab[:, 3160:3672], 8, 512, wo_, wb_, gc)
        glr = [(A.alloc([32, 64], F32), Buf()) for _ in range(2)]
        for gl_, glb in glr:
            S.dve(lambda e, gl_=gl_: nc.vector.memset(gl_, 1.0), writes=[glb])
        spr = Rot(A, 2, [64, 256], F32)
        ebr = Rot(A, 2, [128, 2, 2, 64], F32)
        ebvr = Rot(A, 2, [64, 256], F32)
        qer = Rot(A, 2, [128, 2, 2, 64], BF16)
        for q_, qb_ in qer.items:
            S.dve(lambda e, q_=q_: nc.vector.memset(q_, 0.0), writes=[qb_])
        kdr = Rot(A, 2, [128, 2, 64], BF16)
        klr = Rot(A, 2, [64, 256], BF16)
        vcr = Rot(A, 2, [64, 512], BF16)
        sgr = Rot(A, 2, [64, 512], F32)
        gvr = Rot(A, 2, [64, 512], F32)
        atr = Rot(A, 2, [64, 4, 64], BF16)
        sqr = Rot(A, 2, [64, 512], F32)
        ssr = Rot(A, 2, [64, 3, 4], F32)
        onr = Rot(A, 2, [64, 512], F32)
        obr = Rot(A, 2, [64, 512], BF16)
        Sf = A.alloc([128, 2, 128], F32)
        Sfb = [Buf(), Buf()]
        Sbf = [(A.alloc([128, 2, 128], BF16), [Buf(), Buf()]) for _ in range(2)]
        NCH = SEQ // 64
        for c in range(NCH):
            t0 = c * 64
            ntb = nTb[t0 // 128]
            gl_, glb = glr[c % 2]
            mm_group(psf(0)[0:16, 0:64], [(wl[:, kc, 0:16], nT[:, kc, t0:t0 + 64]) for kc in range(8)], [ntb, wb_], [PSB[0]])
            S.act(lambda e, gl_=gl_: nc.scalar.copy(out=gl_[0:16, :], in_=psf(0)[0:16, 0:64]), reads=[PSB[0]], writes=[glb])
            mm_group(psf(0)[0:64, 256:512], [(gl_[0:17, 0:64], wg[0:17, :])], [glb, constb], [PSB[0]])
            sp, spb = spr.next()
            S.act(lambda e, sp=sp: nc.scalar.activation(out=sp, in_=psf(0)[0:64, 256:512], func=AF.Exp, scale=-1.0),
                  reads=[PSB[0]], writes=[spb])
            S.act(lambda e, sp=sp: nc.scalar.activation(out=sp, in_=sp, func=AF.Ln, bias=1.0), reads=[spb], writes=[spb])
            def fb(e, sp=sp):
                nc.tensor.matmul(psf(1)[:, 0:64], lhsT=sp[:, 0:128], rhs=tri_incl, start=True, stop=True)
                nc.tensor.matmul(psf(1)[:, 64:128], lhsT=sp[:, 128:256], rhs=tri_incl, start=True, stop=True)
                return nc.tensor.matmul(psf(1)[0:64, 128:384], lhsT=tri_rev, rhs=sp, start=True, stop=True)
            S.pe(fb, reads=[spb, constb], writes=[PSB[1]])
            eb, ebb = ebr.next()
            ebv, ebvb = ebvr.next()

            def fe(e, eb=eb, ebv=ebv):
                bv = psf(1)[:, 0:128].rearrange("p (g t) -> p g t", g=2)
                nc.scalar.activation(out=eb[:, 0], in_=bv, func=AF.Exp)
                nc.scalar.activation(out=eb[:, 1], in_=bv, func=AF.Exp, scale=-1.0)
                return nc.scalar.activation(out=ebv, in_=psf(1)[0:64, 128:384], func=AF.Exp)
            S.act(fe, reads=[PSB[1]], writes=[ebb, ebvb])
            def fqk(e, t0=t0):
                inst = None
                for g in range(2):
                    for kc in range(8):
                        nc.tensor.matmul(psf(2)[:, g * 64:(g + 1) * 64], lhsT=wq[:, kc, g * 128:(g + 1) * 128],
                                         rhs=nT[:, kc, t0:t0 + 64], start=(kc == 0), stop=(kc == 7))
                for g in range(2):
                    for kc in range(8):
                        nc.tensor.matmul(psf(2)[:, 128 + g * 64:128 + (g + 1) * 64], lhsT=wk[:, kc, g * 128:(g + 1) * 128],
                                         rhs=nT[:, kc, t0:t0 + 64], start=(kc == 0), stop=(kc == 7))
                for kc in range(8):
                    inst = nc.tensor.matmul(psf(2)[0:64, 256:512], lhsT=nT[:, kc, t0:t0 + 64], rhs=wk[:, kc, :],
                                            start=(kc == 0), stop=(kc == 7))
                return inst
            S.pe(fqk, reads=[ntb, wb_], writes=[PSB[2]])
            qe, qeb = qer.next()
            kd, kdb = kdr.next()
            kl, klb = klr.next()

            def fq(e, qe=qe, kd=kd, kl=kl, eb=eb, ebv=ebv):
                for hh in range(2):
                    p0 = hh * 64
                    nc.vector.scalar_tensor_tensor(out=qe[p0:p0 + 64, :, hh, :],
                                                   in0=psf(2)[p0:p0 + 64, 0:128].rearrange("p (g t) -> p g t", g=2),
                                                   scalar=0.125, in1=eb[p0:p0 + 64, 0], op0=ALU.mult, op1=ALU.mult)
                nc.vector.tensor_tensor(out=kd, in0=psf(2)[:, 128:256].rearrange("p (g t) -> p g t", g=2), in1=eb[:, 1],
                                        op=ALU.mult)
                return nc.vector.tensor_tensor(out=kl, in0=psf(2)[0:64, 256:512], in1=ebv, op=ALU.mult)
            S.dve(fq, reads=[PSB[2], ebb, ebvb], writes=[qeb, kdb, klb])
            mm_group(psf(3)[0:64, :], [(nT[:, kc, t0:t0 + 64], wv[:, kc, :]) for kc in range(8)], [ntb, wb_], [PSB[3]])
            vc, vcb = vcr.next()
            S.act(lambda e, vc=vc: nc.scalar.copy(out=vc, in_=psf(3)[0:64, :]), reads=[PSB[3]], writes=[vcb])
            mm_group(psf(4)[0:64, :], [(nT[:, kc, t0:t0 + 64], wo_[:, kc, :]) for kc in range(8)], [ntb, wb_], [PSB[4]])
            sg, sgb = sgr.next()

            def fsg(e, sg=sg):
                nc.scalar.activation(out=sg, in_=psf(4)[0:64, :], func=AF.Exp, scale=-1.0)
                nc.scalar.activation(out=sg, in_=sg, func=AF.Ln, bias=1.0)
                return nc.scalar.activation(out=sg, in_=sg, func=AF.Exp, scale=-1.0)
            S.act(fsg, reads=[PSB[4]], writes=[sgb])
            gv, gvb = gvr.next()
            S.dve(lambda e, gv=gv, sg=sg: nc.vector.tensor_tensor(out=gv, in0=psf(4)[0:64, :], in1=sg, op=ALU.mult),
                  reads=[PSB[4], sgb], writes=[gvb])
            def fat(e, kd=kd, qe=qe):
                inst = None
                for g in range(2):
                    for hh in range(2):
                        p0 = hh * 64
                        q = g * 2 + hh
                        inst = nc.tensor.matmul(psf(5)[0:64, q * 64:(q + 1) * 64], lhsT=kd[:, g, :],
                                                rhs=qe[:, g, hh, :], start=True, stop=True)
                return inst
            S.pe(fat, reads=[kdb, qeb], writes=[PSB[5]])
            at, atb = atr.next()
            S.dve(lambda e, at=at: nc.vector.tensor_tensor(
                out=at, in0=psf(5)[0:64, 0:256].rearrange("p (q t) -> p q t", q=4),
                in1=causT.unsqueeze(1).to_broadcast([64, 4, 64]), op=ALU.mult), reads=[PSB[5], constb], writes=[atb])
            sbf_prev, sbfb_prev = Sbf[(c + 1) % 2]
            sbf_cur, sbfb_cur = Sbf[c % 2]

            def fo(e, qe=qe, at=at, vc=vc, c=c, sbf_prev=sbf_prev):
                inst = None
                for g in range(2):
                    for hh in range(2):
                        p0 = hh * 64
                        q = g * 2 + hh
                        ov = psf(6)[0:64, q * 128:(q + 1) * 128]
                        if c > 0:
                            nc.tensor.matmul(ov, lhsT=qe[:, g, hh, :], rhs=sbf_prev[:, g, :], start=True, stop=False)
                        inst = nc.tensor.matmul(ov, lhsT=at[:, q, :], rhs=vc[:, q * 128:(q + 1) * 128], start=(c == 0), stop=True)
                return inst
            S.pe(fo, reads=[qeb, atb, vcb] + (sbfb_prev if c > 0 else []), writes=[PSB[6]])
            if c < NCH - 1:
                def fu(e, kl=kl, vc=vc):
                    nc.tensor.matmul(psf(7)[:, 0:256], lhsT=kl[:, 0:128], rhs=vc[:, 0:256], start=True, stop=True)
                    return nc.tensor.matmul(psf(7)[:, 256:512], lhsT=kl[:, 128:256], rhs=vc[:, 256:512], start=True, stop=True)
                S.pe(fu, reads=[klb, vcb], writes=[PSB[7]])
                for g in range(2):
                    def fs_(e, g=g, eb=eb, c=c):
                        inst = None
                        for hh in range(2):
                            p0 = hh * 64
                            uv = psf(7)[p0:p0 + 64, g * 256 + hh * 128:g * 256 + (hh + 1) * 128]
                            if c == 0:
                                inst = nc.vector.tensor_copy(out=Sf[p0:p0 + 64, g, :], in_=uv)
                            else:
                                inst = nc.vector.scalar_tensor_tensor(out=Sf[p0:p0 + 64, g, :], in0=Sf[p0:p0 + 64, g, :],
                                                                      scalar=eb[p0:p0 + 64, 0, g, 63:64], in1=uv,
                                                                      op0=ALU.mult, op1=ALU.add)
                        return inst
                    S.dve(fs_, reads=[PSB[7], ebb, Sfb[g]], writes=[Sfb[g]])
                    S.act(lambda e, g=g, sbf_cur=sbf_cur: nc.scalar.copy(out=sbf_cur[:, g, :], in_=Sf[:, g, :]),
                          reads=[Sfb[g]], writes=[sbfb_cur[g]])
            sq, sqb = sqr.next()
            ss, ssb = ssr.next()
            S.act(lambda e, sq=sq: nc.scalar.activation(out=sq, in_=psf(6)[0:64, :], func=AF.Square), reads=[PSB[6]], writes=[sqb])
            S.dve(lambda e, sq=sq, ss=ss: nc.vector.tensor_reduce(out=ss[:, 0, :], in_=sq.rearrange("p (h d) -> p h d", h=4),
                                                                  axis=AX.X, op=ALU.add), reads=[sqb], writes=[ssb])
            rstd_from_ss(ss, ssb, 4, 1.0 / 128, True)
            on, onb = onr.next()

            def fn_(e, on=on, ss=ss):
                inst = None
                for h in range(4):
                    inst = nc.vector.scalar_tensor_tensor(out=on[:, h * 128:(h + 1) * 128], in0=psf(6)[0:64, h * 128:(h + 1) * 128],
                                                          scalar=ss[:, 2, h:h + 1], in1=gains[0:64, 4, :], op0=ALU.mult,
                                                          op1=ALU.mult)
                return inst
            S.dve(fn_, reads=[PSB[6], ssb, constb], writes=[onb])
            ob, obb = obr.next()
            S.dve(lambda e, ob=ob, on=on, gv=gv: nc.vector.tensor_tensor(out=ob, in0=on, in1=gv, op=ALU.mult),
                  reads=[onb, gvb], writes=[obb])
            ptv = psh(5)
            transposes([(ptv[:, 512 + h * 64:512 + (h + 1) * 64], ob[:, h * 128:(h + 1) * 128]) for h in range(4)],
                       ident[0:64, 0:64], [obb, constb], [PSB[5]])
            S.act(lambda e, t0=t0, ptv=ptv: nc.scalar.copy(out=oT[:, 4:8, t0:t0 + 64],
                                                           in_=ptv[:, 512:768].rearrange("p (h t) -> p h t", h=4)),
                  reads=[PSB[5]], writes=[oTb[t0 // 128]])
        S.flush()
        A.release(m)

    def sb_attn(s):
        m = A.mark()
        qT = A.alloc([128, 4, SEQ], BF16)
        kT = A.alloc([128, 4, SEQ], BF16)
        v = A.alloc([128, NT, 512], BF16)
        qTb, kTb, vb_ = Buf(), Buf(), Buf()
        gc = gcol[:, 1, :]
        for hg in range(2):
            mp = A.mark()
            stage_state["pool"] = mk_stage()
            wbp = [(A.alloc([128, 8, 512], BF16), Buf()) for _ in range(2)]
            ws = mk_qk_ws(7)
            for bi, (nm, c0) in enumerate((("q", hg * 512), ("k", 1024 + hg * 512), ("v", 2048 + hg * 512))):
                wb, wbb = wbp[bi % 2]
                load_w(w_in_c[:, c0:c0 + 512], 8, 512, wb, wbb, gc)
                for i in range(NT):
                    pi = i % 2
                    proj_tok(wb, 512, i, pi, wbb)
                    if nm == "q":
                        qk_post(pi, i, ws, 2, qT, qTb, 128 ** -0.5, False)
                    elif nm == "k":
                        qk_post(pi, i, ws, 3, kT, kTb, 1.0, False)
                    else:
                        S.act(lambda e, pi=pi, i=i: nc.scalar.copy(out=v[:, i, :], in_=psf(pi)), reads=[PSB[pi]], writes=[vb_])
            S.flush()
            A.release(mp)
            def sb_stream(st_, heads, hg=hg):
                pz, pc, po = 0 + st_, 2 + st_, 4 + st_
                espr = Rot(A, 2, [128, 512], F32)
                lor = Rot(A, 2, [128, 512], BF16)
                ar = Rot(A, 2, [128, 512], BF16)
                lbs = [(A.alloc([128, 512], BF16), Buf()) for _ in range(3)]
                gs = [0]

                def S1(f):
                    h, qb, kt, step, nsteps = f["h"], f["qb"], f["kt"], f["step"], f["nsteps"]
                    cc, ncol, diag = f["cc"], f["ncol"], f["diag"]
                    q0 = qb * 512 + cc
                    mm_group(psf(pz)[:, 0:ncol], [(kT[:, h, kt * 128:(kt + 1) * 128], qT[:, h, q0:q0 + ncol])],
                             [kTb, qTb], [PSB[pz]])
                    yield
                    es, esb = espr.next()
                    f["es"], f["esb"] = es, esb
                    S.act(lambda e: nc.scalar.activation(out=es[:, 0:ncol], in_=psf(pz)[:, 0:ncol], func=AF.Exp, scale=-1.0),
                          reads=[PSB[pz]], writes=[esb])
                    yield
                    S.act(lambda e: nc.scalar.activation(out=es[:, 0:ncol], in_=es[:, 0:ncol], func=AF.Ln, bias=1.0),
                          reads=[esb], writes=[esb])
                    yield
                    lo, lob = lor.next()
                    f["lo"], f["lob"] = lo, lob
                    S.dve(lambda e: nc.vector.scalar_tensor_tensor(out=lo[:, 0:ncol], in0=psf(pz)[:, 0:ncol], scalar=-1.0,
                                                                   in1=es[:, 0:ncol], op0=ALU.mult, op1=ALU.subtract),
                          reads=[PSB[pz], esb], writes=[lob])
                    yield
                    if diag:
                        S.pool(lambda e: nc.gpsimd.tensor_tensor(out=lo[:, 0:128], in0=lo[:, 0:128], in1=strictT, op=ALU.mult),
                               reads=[lob, constb], writes=[lob])
                        yield
                    g = gs[0]
                    gs[0] += 1
                    f["lbc"] = lbs[g % 3]
                    if step < nsteps - 1:
                        lbn, lbnb = lbs[(g + 1) % 3]
                        lbc, lbcb = lbs[g % 3]
                        ccn = f["cc_next"]
                        if ccn < cc:
                            S.pool(lambda e: nc.gpsimd.memset(lbn[:, ccn:cc], 0.0), writes=[lbnb])
                            yield
                        if step == 0:
                            S.dve(lambda e: nc.vector.tensor_copy(out=lbn[:, cc:512], in_=lo[:, 0:ncol]), reads=[lob], writes=[lbnb])
                        else:
                            S.dve(lambda e: nc.vector.tensor_tensor(out=lbn[:, cc:512], in0=lbc[:, cc:512], in1=lo[:, 0:ncol],
                                                                    op=ALU.add), reads=[lob, lbcb], writes=[lbnb])
                        yield

                def S2(f):
                    h, qb, kt, step, nsteps = f["h"], f["qb"], f["kt"], f["step"], f["nsteps"]
                    cc, ncol, diag = f["cc"], f["ncol"], f["diag"]
                    es, esb, lo, lob = f["es"], f["esb"], f["lo"], f["lob"]
                    lbc, lbcb = f["lbc"]
                    if step == 0:
                        mm_group(psf(pc)[:, 0:ncol], [(triU, lo[:, 0:ncol])], [lob, constb], [PSB[pc]])
                    else:
                        mm_group(psf(pc)[:, 0:ncol], [(triU, lo[:, 0:ncol]), (ones128, lbc[:, cc:512])],
                                 [lob, lbcb, constb], [PSB[pc]])
                    yield
                    S.dve(lambda e: nc.vector.tensor_tensor(out=es[:, 0:ncol], in0=psf(pc)[:, 0:ncol], in1=es[:, 0:ncol],
                                                            op=ALU.subtract), reads=[PSB[pc], esb], writes=[esb])
                    yield
                    a_, ab_ = ar.next()
                    S.act(lambda e: nc.scalar.activation(out=a_[:, 0:ncol], in_=es[:, 0:ncol], func=AF.Exp),
                          reads=[esb], writes=[ab_])
                    yield
                    if diag:
                        S.pool(lambda e: nc.gpsimd.tensor_tensor(out=a_[:, 0:128], in0=a_[:, 0:128], in1=strictT, op=ALU.mult),
                               reads=[ab_, constb], writes=[ab_])
                        yield
                    S.pe(lambda e: nc.tensor.matmul(psf(po)[:, cc:512], lhsT=v[:, kt, h * 128:(h + 1) * 128], rhs=a_[:, 0:ncol],
                                                    start=(step == 0), stop=(step == nsteps - 1)),
                         reads=[vb_, ab_], writes=[PSB[po]])
                    yield
                    if step == nsteps - 1:
                        hh = hg * 4 + h
                        S.act(lambda e: nc.scalar.copy(out=oT[:, hh, qb * 512:(qb + 1) * 512], in_=psf(po)),
                              reads=[PSB[po]], writes=oTb[4 * qb:4 * qb + 4])
                        yield

                pending = None
                for h in heads:
                    for qb in range(4):
                        kts = list(range(4 * qb + 3, -1, -1))
                        ccs = [max(0, kt - 4 * qb) * 128 for kt in kts]
                        for step, kt in enumerate(kts):
                            f = {"h": h, "qb": qb, "kt": kt, "step": step, "nsteps": len(kts), "cc": ccs[step],
                                 "ncol": 512 - ccs[step], "diag": kt >= 4 * qb,
                                 "cc_next": ccs[step + 1] if step + 1 < len(kts) else 0}
                            yield from S1(f)
                            if pending is not None:
                                yield from S2(pending)
                            pending = f
                yield from S2(pending)

            run_streams([sb_stream(0, [0, 1]), sb_stream(1, [2, 3])])
            S.flush()
            A.release(mp)
        A.release(m)

    def dump_h(s):
        m = A.mark()
        t = [(A.alloc([128, 1024], F32), Buf(), f"xin{k}") for k in range(2)]
        for i in range(NT):
            tt, tb, ch = t[i % 2]
            S.dma(lambda e, tt=tt, i=i: nc.sync.dma_start(out=tt, in_=h_scr[s, i * 128:(i + 1) * 128, :]), ch,
                  reads=[hb[s][i]], writes=[tb])
            S.dma(lambda e, tt=tt, i=i: nc.sync.dma_start(out=out[s, i * 128:(i + 1) * 128, :], in_=tt), f"st{i % 2}",
                  reads=[tb], writes=[])
        S.flush()
        A.release(m)

    for s in range(NSEQ):
        if stop != "const":
            phase_norm(s, x)
        if stop == "norm":
            continue
        if stop == "const":
            continue
        dsa(s)
        if stop == "dsa":
            continue
        phase_norm(s, x)
        gla(s)
        if stop == "gla":
            continue
        out_proj_and_norm(s, w_out_ab, x, h_scr)
        if stop == "mix0":
            dump_h(s)
            continue
        ffn(s, 0, False)
        if stop == "ffn0":
            dump_h(s)
            continue
        phase_norm(s, h_scr)
        sb_attn(s)
        out_proj_and_norm(s, w_out_c, h_scr, h_scr)
        if stop == "mix1":
            dump_h(s)
            continue
        ffn(s, 1, True)
    S.flush(final=True)
    return nc


def host_constants():
    c = {}
    c["c_ident"] = np.eye(128, dtype=np.float32)
    pos = np.arange(SEQ, dtype=np.float32)
    inv_a = np.power(np.float32(500000.0), -np.arange(16, dtype=np.float32) * 2.0 / 32).astype(np.float32)
    ang = pos[:, None] * inv_a[None, :]
    c["c_rope_a"] = np.concatenate([np.cos(ang), np.sin(ang)], axis=1).astype(np.float32)
    inv_i = np.power(np.float32(500000.0), -np.arange(8, dtype=np.float32) * 2.0 / 16).astype(np.float32)
    ang = pos[:, None] * inv_i[None, :]
    c["c_rope_i"] = np.concatenate([np.cos(ang), np.sin(ang)], axis=1).astype(np.float32)
    a = np.arange(64)
    m64 = np.zeros((64, 3, 64), np.float32)
    m64[:, 0, :] = (a[:, None] <= a[None, :]) * (-1.0 / 16.0)
    m64[:, 1, :] = (a[:, None] > a[None, :]) * (-1.0 / 16.0)
    m64[:, 2, :] = (a[:, None] <= a[None, :]) * 1.0
    c["c_m64"] = m64
    b = np.arange(128)
    m128 = np.zeros((128, 3, 128), np.float32)
    m128[:, 0, :] = (b[:, None] > b[None, :]) * 1.0
    m128[:, 1, :] = 1.0
    m128[:, 2, :] = (b[:, None] < b[None, :]) * 1.0
    c["c_m128"] = m128
    return c


_CACHE = {}


def kernel(x, g_mix, g_ffn, w_in_ab, gq_a, gk_a, w_gate_up, b_gate, g_gla, w_out_ab,
           w_in_c, gq_c, gk_c, w_out_c, w_up, w_down, _ncores=8, _stop=None):
    f = lambda a: np.ascontiguousarray(np.asarray(a, dtype=np.float32))
    x = f(x)
    nseq = x.shape[0] // _ncores
    shared = {
        "w_in_ab": f(w_in_ab)[0], "w_gate_up": f(w_gate_up)[0], "b_gate": f(b_gate), "w_out_ab": f(w_out_ab)[0],
        "w_in_c": f(w_in_c)[0], "w_out_c": f(w_out_c)[0], "w_up": f(w_up), "w_down": f(w_down),
    }
    gains = np.stack([np.broadcast_to(f(g).reshape(1, 128), (128, 128)) for g in (gq_a, gk_a, gq_c, gk_c, g_gla)], axis=1)
    shared["c_gains"] = np.ascontiguousarray(gains, dtype=np.float32)
    gcols = np.stack([f(g_mix)[0].reshape(8, 128).T, f(g_mix)[1].reshape(8, 128).T,
                      f(g_ffn)[0].reshape(8, 128).T, f(g_ffn)[1].reshape(8, 128).T], axis=1)
    shared["c_gcol"] = np.ascontiguousarray(gcols, dtype=np.float32)
    shared.update(host_constants())
    key = (nseq, _stop)
    if key not in _CACHE:
        _CACHE[key] = build_program(nseq, _stop)
    nc = _CACHE[key]
    in_maps = []
    for c in range(_ncores):
        d = dict(shared)
        d["x"] = np.ascontiguousarray(x[c * nseq:(c + 1) * nseq])
        in_maps.append(d)
    res = run_bass_kernel_spmd(nc, in_maps, core_ids=list(range(_ncores)))
    return np.concatenate([np.asarray(r["out"], dtype=np.float32) for r in res.results], axis=0)
```

```python
import numpy as np
import concourse.bass as bass
import concourse.mybir as mybir
from concourse.bass_utils import run_bass_kernel_spmd
from concourse.alu_op_type import AluOpType as ALU

F32 = mybir.dt.float32
BF16 = mybir.dt.bfloat16
AF = mybir.ActivationFunctionType
AX = mybir.AxisListType

SEQ = 2048
DM = 1024
NT = SEQ // 128
EPS = 1e-6
ABW = 3672
NEG = -1.0e30
BIS_ITERS = 16
ARENA_BYTES = 175 * 1024

_ENG_ATTR = {"pe": "tensor", "act": "scalar", "dve": "vector", "pool": "gpsimd", "sp": "sync"}


class Buf:
    __slots__ = ("lw", "rd")

    def __init__(self):
        self.lw = None
        self.rd = {}


class Sched:
    ENG = ("pe", "act", "dve", "pool", "sp")

    def __init__(self, nc):
        self.nc = nc
        self.sem = {e: nc.alloc_semaphore("sem_" + e) for e in self.ENG}
        self.cnt = {e: 0 for e in self.ENG}
        self.ops = {e: [] for e in self.ENG}
        self.waited = {e: {} for e in self.ENG}
        self.chan = {}

    def channel(self, name):
        if name not in self.chan:
            self.chan[name] = [self.nc.alloc_semaphore("ch_" + name), 0]
        return name

    def _deps(self, reads, writes):
        d = {}
        for b in reads:
            if b.lw is not None and d.get(b.lw[0], 0) < b.lw[1]:
                d[b.lw[0]] = b.lw[1]
        for b in writes:
            if b.lw is not None and d.get(b.lw[0], 0) < b.lw[1]:
                d[b.lw[0]] = b.lw[1]
            for k, v in b.rd.items():
                if d.get(k, 0) < v:
                    d[k] = v
        return d

    def op(self, eng, fn, reads=(), writes=()):
        d = self._deps(reads, writes)
        self.cnt[eng] += 1
        idx = self.cnt[eng]
        for b in reads:
            b.rd[eng] = idx
        for b in writes:
            b.lw = (eng, idx)
            b.rd = {}
        self.ops[eng].append((d, fn, None))

    def pe(self, fn, reads=(), writes=()):
        self.op("pe", fn, reads, writes)

    def act(self, fn, reads=(), writes=()):
        self.op("act", fn, reads, writes)

    def dve(self, fn, reads=(), writes=()):
        self.op("dve", fn, reads, writes)

    def pool(self, fn, reads=(), writes=()):
        self.op("pool", fn, reads, writes)

    def dma(self, fn, chan, reads=(), writes=(), queue="sp"):
        d = self._deps(reads, writes)
        c = self.chan[chan]
        c[1] += 16
        key = "ch:" + chan
        for b in reads:
            b.rd[key] = c[1]
        for b in writes:
            b.lw = (key, c[1])
            b.rd = {}
        self.ops[queue].append((d, fn, chan))

    def _semof(self, k):
        if k.startswith("ch:"):
            return self.chan[k[3:]][0]
        return self.sem[k]

    def flush(self, final=False):
        nc = self.nc
        if final:
            d = {}
            for name, c in self.chan.items():
                if c[1] > 0:
                    d["ch:" + name] = c[1]
            self.ops["sp"].append((d, None, None))
        with nc.Block() as blk:
            for eng in self.ENG:
                ops = self.ops[eng]

                def body(e, eng=eng, ops=ops):
                    w = self.waited[eng]
                    for d, fn, chan in ops:
                        for k, v in d.items():
                            if k == eng and eng == "pe":
                                continue
                            if w.get(k, 0) < v:
                                e.wait_ge(self._semof(k), v)
                                w[k] = v
                        if fn is None:
                            continue
                        inst = fn(e)
                        if chan is None:
                            inst.then_inc(self.sem[eng], 1)
                        else:
                            inst.then_inc(self.chan[chan][0], 16)

                getattr(blk, _ENG_ATTR[eng])(body)
        self.ops = {e: [] for e in self.ENG}


class Arena:
    def __init__(self, nc, nbytes, base=None):
        self.t = nc.alloc_sbuf_tensor("arena", [128, nbytes // 4], F32) if base is None else base
        self.top = 0
        self.nbytes = nbytes

    def mark(self):
        return self.top

    def release(self, m):
        self.top = m

    def alloc(self, shape, dtype):
        esz = 4 if dtype == F32 else 2
        n = int(np.prod(shape[1:]))
        nb = (n * esz + 63) // 64 * 64
        off = self.top
        self.top += nb
        assert self.top <= self.nbytes, ("arena overflow", self.top)
        ap = self.t[0:shape[0], off // 4:(off + nb) // 4]
        if dtype != F32:
            ap = ap.bitcast(dtype)
        ap = ap[:, 0:n]
        if len(shape) == 3:
            ap = ap.rearrange("p (a b) -> p a b", a=shape[1])
        elif len(shape) == 4:
            ap = ap.rearrange("p (a b c) -> p a b c", a=shape[1], b=shape[2])
        return ap


class Rot:
    def __init__(self, arena, n, shape, dtype):
        self.items = [(arena.alloc(shape, dtype), Buf()) for _ in range(n)]
        self.i = 0

    def next(self):
        it = self.items[self.i % len(self.items)]
        self.i += 1
        return it


def run_streams(gens):
    gens = list(gens)
    while gens:
        for g in list(gens):
            try:
                next(g)
            except StopIteration:
                gens.remove(g)


def build_program(NSEQ=2, stop=None):
    nc = bass.Bass("TRN2", target_bir_lowering=False)
    S = Sched(nc)

    def dram_in(name, shape):
        return nc.dram_tensor(name, shape, F32, kind="ExternalInput").ap()

    x = dram_in("x", [NSEQ, SEQ, DM])
    w_in_ab = dram_in("w_in_ab", [DM, ABW])
    w_gate_up = dram_in("w_gate_up", [16, 256])
    b_gate = dram_in("b_gate", [1, 256])
    w_out_ab = dram_in("w_out_ab", [DM, DM])
    w_in_c = dram_in("w_in_c", [DM, 3 * DM])
    w_out_c = dram_in("w_out_c", [DM, DM])
    w_up = dram_in("w_up", [2, DM, 4 * DM])
    w_down = dram_in("w_down", [2, 4 * DM, DM])
    c_ident = dram_in("c_ident", [128, 128])
    c_rope_a = dram_in("c_rope_a", [SEQ, 32])
    c_rope_i = dram_in("c_rope_i", [SEQ, 16])
    c_m64 = dram_in("c_m64", [64, 3, 64])
    c_m128 = dram_in("c_m128", [128, 3, 128])
    c_gains = dram_in("c_gains", [128, 5, 128])
    c_gcol = dram_in("c_gcol", [128, 4, 8])
    out = nc.dram_tensor("out", [NSEQ, SEQ, DM], F32, kind="ExternalOutput").ap()
    h_scr = nc.dram_tensor("h_scr", [NSEQ, SEQ, DM], F32).ap()
    hb = [[Buf() for _ in range(NT)] for _ in range(NSEQ)]

    A = Arena(nc, ARENA_BYTES)
    PS = [nc.alloc_psum_tensor(f"psb{i}", [128, 512], F32) for i in range(8)]
    PSB = [Buf() for _ in range(8)]

    def psf(i):
        return PS[i][:, :]

    def psh(i):
        return PS[i][:, :].bitcast(BF16)

    for nm in ("c0", "c1", "c2", "c3", "xin0", "xin1", "st0", "st1", "stg0", "stg1", "hres"):
        S.channel(nm)

    nT_raw = A.alloc([128, 8192], F32)
    nT = nT_raw.bitcast(BF16).rearrange("p (a b) -> p a b", a=8)
    nTb = [Buf() for _ in range(NT)]
    oT_raw = A.alloc([128, 8192], F32)
    oT = oT_raw.bitcast(BF16).rearrange("p (a b) -> p a b", a=8)
    oTb = [Buf() for _ in range(NT)]
    identf = A.alloc([128, 128], F32)
    ident = A.alloc([128, 128], BF16)
    ropeA = A.alloc([128, NT, 32], F32)
    ropeI = A.alloc([128, NT, 16], F32)
    m64 = A.alloc([64, 3, 64], F32)
    m128f = A.alloc([128, 3, 128], F32)
    m128 = A.alloc([128, 3, 128], BF16)
    gains = A.alloc([128, 5, 128], F32)
    gcol = A.alloc([128, 4, 8], F32)
    wg = A.alloc([32, 256], F32)
    thr_all = A.alloc([128, 1], F32)
    constb = Buf()

    def bc_row(ap):
        r = ap.partition_broadcast(128)
        if len(r.shape) == 3:
            r = r.rearrange("p a b -> p (a b)")
        return r

    def dma_simple(out_ap, in_ap, chan, writes, reads=(), ncdma=True):
        S.dma(lambda e: nc.sync.dma_start(out=out_ap, in_=in_ap), chan, reads=reads, writes=writes)

    dma_simple(identf, c_ident, "c0", [constb])
    dma_simple(ropeA, c_rope_a.rearrange("(i p) c -> p i c", p=128), "c0", [constb])
    dma_simple(ropeI, c_rope_i.rearrange("(i p) c -> p i c", p=128), "c0", [constb])
    dma_simple(m64, c_m64, "c0", [constb])
    dma_simple(m128f, c_m128, "c0", [constb])
    dma_simple(gains, c_gains, "c0", [constb])
    dma_simple(wg[0:16, :], w_gate_up, "c0", [constb])
    dma_simple(wg[16:17, :], b_gate, "c0", [constb])
    dma_simple(gcol, c_gcol, "c0", [constb])
    S.dve(lambda e: nc.vector.tensor_copy(out=ident, in_=identf), reads=[constb], writes=[constb])
    S.dve(lambda e: nc.vector.tensor_copy(out=m128, in_=m128f), reads=[constb], writes=[constb])
    S.dve(lambda e: nc.vector.memset(thr_all, -1.0e29), reads=[], writes=[constb])
    S.flush()
    base_mark = A.mark()

    triU = m128[:, 0, :]
    ones128 = m128[:, 1, :]
    strictT = m128[:, 2, :]
    tri_incl = m64[:, 0, :]
    tri_rev = m64[:, 1, :]
    causT = m64[:, 2, :]

    def mk_stage(ar=None):
        ar = A if ar is None else ar
        return [(ar.alloc([128, 2048], F32), Buf(), S.channel(f"stg{i}")) for i in range(2)]

    stage_state = {"pool": None, "i": 0}

    def load_w(src2d, KC, ncols, dst, dstb, gc=None):
        srcv = src2d.rearrange("(k p) c -> p k c", p=128)
        kstep = max(1, 2048 // ncols)
        for k0 in range(0, KC, kstep):
            kn = min(kstep, KC - k0)
            st, stb, ch = stage_state["pool"][stage_state["i"] % 2]
            stage_state["i"] += 1
            stv = st[:, 0:kn * ncols].rearrange("p (k c) -> p k c", k=kn)
            S.dma(lambda e, stv=stv, k0=k0, kn=kn: nc.sync.dma_start(out=stv, in_=srcv[:, k0:k0 + kn, :]),
                  ch, writes=[stb])
            if gc is None:
                S.pool(lambda e, stv=stv, k0=k0, kn=kn: nc.gpsimd.tensor_copy(out=dst[:, k0:k0 + kn, :], in_=stv),
                       reads=[stb], writes=[dstb])
            else:
                S.pool(lambda e, stv=stv, k0=k0, kn=kn: nc.gpsimd.tensor_tensor(
                    out=dst[:, k0:k0 + kn, :], in0=stv,
                    in1=gc[:, k0:k0 + kn].unsqueeze(2).to_broadcast([128, kn, ncols]), op=ALU.mult),
                    reads=[stb, constb], writes=[dstb])

    def mm_group(out_ap, pairs, rd, wr):
        def f(e):
            n = len(pairs)
            inst = None
            for q, (l, r) in enumerate(pairs):
                inst = nc.tensor.matmul(out_ap, lhsT=l, rhs=r, start=(q == 0), stop=(q == n - 1))
            return inst
        S.pe(f, reads=rd, writes=wr)

    def transposes(outs_ins, idn, rd, wr):
        def f(e):
            inst = None
            for o, i_ in outs_ins:
                inst = nc.tensor.transpose(o, i_, idn)
            return inst
        S.pe(f, reads=rd, writes=wr)

    def rstd_from_ss(ssv, ssb, n, inv_n, add_eps):
        P = ssv.shape[0]
        if add_eps:
            S.dve(lambda e: nc.vector.tensor_scalar(out=ssv[:, 0, :], in0=ssv[:, 0, :], scalar1=inv_n, scalar2=EPS,
                                                    op0=ALU.mult, op1=ALU.add), reads=[ssb], writes=[ssb])
            S.act(lambda e: nc.scalar.activation(out=ssv[:, 1, :], in_=ssv[:, 0, :], func=AF.Ln), reads=[ssb], writes=[ssb])
        else:
            S.act(lambda e: nc.scalar.activation(out=ssv[:, 1, :], in_=ssv[:, 0, :], func=AF.Ln, scale=inv_n),
                  reads=[ssb], writes=[ssb])
        S.act(lambda e: nc.scalar.activation(out=ssv[:, 2, :], in_=ssv[:, 1, :], func=AF.Exp, scale=-0.5),
              reads=[ssb], writes=[ssb])

    def rope(xv, xb, H, half, table, i, tmp, tmpb, P=128, prow=None):
        if prow is None:
            cs = table[0:P, i, :]
        else:
            cs = prow
        cos = cs[:, 0:half].unsqueeze(1).to_broadcast([P, H, half])
        sin = cs[:, half:2 * half].unsqueeze(1).to_broadcast([P, H, half])
        x1 = xv[:, :, 0:half]
        x2 = xv[:, :, half:2 * half]

        def f1(e):
            nc.gpsimd.tensor_tensor(out=tmp[:, 0], in0=x1, in1=cos, op=ALU.mult)
            nc.gpsimd.tensor_tensor(out=tmp[:, 1], in0=x2, in1=sin, op=ALU.mult)
            nc.gpsimd.tensor_tensor(out=tmp[:, 2], in0=x2, in1=cos, op=ALU.mult)
            return nc.gpsimd.tensor_tensor(out=tmp[:, 3], in0=x1, in1=sin, op=ALU.mult)
        S.pool(f1, reads=[xb, constb], writes=[tmpb])

        def f2(e):
            nc.gpsimd.tensor_tensor(out=x1, in0=tmp[:, 0], in1=tmp[:, 1], op=ALU.subtract)
            return nc.gpsimd.tensor_tensor(out=x2, in0=tmp[:, 2], in1=tmp[:, 3], op=ALU.add)
        S.pool(f2, reads=[tmpb], writes=[xb])

    def norm_transpose(xt, xtb, i, ws):
        junk, jb = ws["junk"].next()
        ss, sb = ws["ss"].next()
        nb, nbb = ws["nb"].next()
        S.act(lambda e: nc.scalar.activation(out=junk, in_=xt, func=AF.Square, accum_out=ss[:, 0, 0:1]),
              reads=[xtb], writes=[jb, sb])
        rstd_from_ss(ss, sb, 1, 1.0 / DM, True)
        S.dve(lambda e: nc.vector.tensor_scalar(out=nb, in0=xt, scalar1=ss[:, 2, 0:1], scalar2=None, op0=ALU.mult),
              reads=[xtb, sb], writes=[nbb])
        pb = ws["psT"]
        pv = psh(pb)
        transposes([(pv[:, kc * 128:(kc + 1) * 128], nb[:, kc * 128:(kc + 1) * 128]) for kc in range(8)], ident,
                   [nbb, constb], [PSB[pb]])
        S.act(lambda e: nc.scalar.copy(out=nT[:, :, i * 128:(i + 1) * 128],
                                       in_=pv[:, 0:1024].rearrange("p (k t) -> p k t", k=8)),
              reads=[PSB[pb]], writes=[nTb[i]])

    def mk_norm_ws(psT):
        return {"junk": Rot(A, 1, [128, 1024], BF16), "ss": Rot(A, 2, [128, 3, 1], F32),
                "nb": Rot(A, 2, [128, 1024], BF16), "psT": psT}

    def phase_norm(s, src):
        m = A.mark()
        ws = mk_norm_ws(7)
        xin = [(A.alloc([128, 1024], F32), Buf(), f"xin{k}") for k in range(2)]
        for i in range(NT):
            xt, xb, ch = xin[i % 2]
            S.dma(lambda e, xt=xt, i=i: nc.sync.dma_start(out=xt, in_=src[s, i * 128:(i + 1) * 128, :]), ch,
                  reads=[hb[s][i]], writes=[xb])
            norm_transpose(xt, xb, i, ws)
        S.flush()
        A.release(m)

    def qk_post(ps_i, i, ws, gidx, dstT, dst_b, scale, do_rope):
        sq, sqb = ws["sq"].next()
        ss, ssb = ws["ss4"].next()
        qn, qnb = ws["qn"].next()
        qh, qhb = ws["qh"].next()
        pv = psf(ps_i)
        S.act(lambda e: nc.scalar.activation(out=sq, in_=pv, func=AF.Square), reads=[PSB[ps_i]], writes=[sqb])
        yield
        S.dve(lambda e: nc.vector.tensor_reduce(out=ss[:, 0, :], in_=sq.rearrange("p (h d) -> p h d", h=4), axis=AX.X,
                                                op=ALU.add), reads=[sqb], writes=[ssb])
        yield
        S.dve(lambda e: nc.vector.tensor_scalar(out=ss[:, 0, :], in0=ss[:, 0, :], scalar1=1.0 / 128, scalar2=EPS,
                                                op0=ALU.mult, op1=ALU.add), reads=[ssb], writes=[ssb])
        yield
        S.act(lambda e: nc.scalar.activation(out=ss[:, 1, :], in_=ss[:, 0, :], func=AF.Ln), reads=[ssb], writes=[ssb])
        yield
        S.act(lambda e: nc.scalar.activation(out=ss[:, 2, :], in_=ss[:, 1, :], func=AF.Exp, scale=-0.5),
              reads=[ssb], writes=[ssb])
        yield

        def f(e):
            inst = None
            for h in range(4):
                inst = nc.vector.scalar_tensor_tensor(out=qn[:, h * 128:(h + 1) * 128], in0=pv[:, h * 128:(h + 1) * 128],
                                                      scalar=ss[:, 2, h:h + 1], in1=gains[:, gidx, :], op0=ALU.mult,
                                                      op1=ALU.mult)
            return inst
        S.dve(f, reads=[PSB[ps_i], ssb, constb], writes=[qnb])
        yield
        if do_rope:
            tmp, tmpb = ws["rtmp"].next()
            rope(qn.rearrange("p (h d) -> p h d", h=4), qnb, 4, 16, ropeA, i, tmp, tmpb)
            yield
        S.act(lambda e: nc.scalar.activation(out=qh, in_=qn, func=AF.Copy, scale=scale), reads=[qnb], writes=[qhb])
        yield
        pt = ws["psT"]
        ptv = psh(pt)
        transposes([(ptv[:, h * 128:(h + 1) * 128], qh[:, h * 128:(h + 1) * 128]) for h in range(4)], ident,
                   [qhb, constb], [PSB[pt]])
        yield
        S.act(lambda e: nc.scalar.copy(out=dstT[:, :, i * 128:(i + 1) * 128],
                                       in_=ptv[:, 0:512].rearrange("p (h t) -> p h t", h=4)),
              reads=[PSB[pt]], writes=[dst_b])
        yield

    def proj_streamed(wb, wbb, ncols, handler):
        def stream(k):
            for i in range(k, NT, 2):
                proj_tok(wb, ncols, i, k, wbb)
                yield
                yield from handler(k, i, k)
        run_streams([stream(0), stream(1)])

    def proj_tok(wb, ncols, i, ps_i, wbb, M=128, t0=None):
        t0 = i * 128 if t0 is None else t0
        mm_group(psf(ps_i)[0:M, 0:ncols], [(nT[:, kc, t0:t0 + M], wb[:, kc, 0:ncols]) for kc in range(8)],
                 [nTb[t0 // 128], wbb], [PSB[ps_i]])

    def mk_qk_ws(psT):
        return {"sq": Rot(A, 1, [128, 512], F32), "ss4": Rot(A, 2, [128, 3, 4], F32), "qn": Rot(A, 1, [128, 512], F32),
                "qh": Rot(A, 1, [128, 512], BF16), "rtmp": Rot(A, 1, [128, 4, 4, 16], F32), "psT": psT}

    def out_proj_and_norm(s, wout_dram, src, dst):
        m = A.mark()
        stage_state["pool"] = mk_stage()
        wo = A.alloc([128, 8, DM], BF16)
        wob = Buf()
        for c0 in range(0, DM, 256):
            load_w(wout_dram[:, c0:c0 + 256], 8, 256, wo[:, :, c0:c0 + 256], wob)
        ws = mk_norm_ws(7)
        hin = [(A.alloc([128, 1024], F32), Buf(), f"xin{k}") for k in range(2)]
        hnew = [(A.alloc([128, 1024], F32), Buf(), f"st{k}") for k in range(2)]
        for i in range(NT):
            hi_, hib, ch = hin[i % 2]
            hn, hnb, sch = hnew[i % 2]
            S.dma(lambda e, hi_=hi_, i=i: nc.sync.dma_start(out=hi_, in_=src[s, i * 128:(i + 1) * 128, :]), ch,
                  reads=[hb[s][i]], writes=[hib])
            for half in range(2):
                pi = 0 + 2 * (i % 2) + half
                mm_group(psf(pi), [(oT[:, c, i * 128:(i + 1) * 128], wo[:, c, half * 512:(half + 1) * 512])
                                   for c in range(8)], [oTb[i], wob], [PSB[pi]])
                S.dve(lambda e, pi=pi, half=half, hn=hn, hi_=hi_: nc.vector.tensor_tensor(
                    out=hn[:, half * 512:(half + 1) * 512], in0=psf(pi), in1=hi_[:, half * 512:(half + 1) * 512],
                    op=ALU.add), reads=[PSB[pi], hib], writes=[hnb])
            S.dma(lambda e, hn=hn, i=i: nc.sync.dma_start(out=dst[s, i * 128:(i + 1) * 128, :], in_=hn), sch,
                  reads=[hnb], writes=[hb[s][i]])
            norm_transpose(hn, hnb, i, ws)
        S.flush()
        A.release(m)

    def ffn(s, layer, final):
        m = A.mark()
        stage_state["pool"] = mk_stage()
        hres = A.alloc([128, NT, DM], F32)
        hresb = [Buf() for _ in range(NT)]
        actT = A.alloc([128, 4, SEQ], BF16)
        actb = [Buf() for _ in range(4)]
        AO = Arena(nc, 32768, base=oT_raw)
        wub = [(AO.alloc([128, 8, 512], BF16), Buf()) for _ in range(2)]
        wdb = [(AO.alloc([128, 4, DM], BF16), Buf()) for _ in range(2)]
        rr = Rot(A, 2, [128, 512], F32)
        for i in range(NT):
            S.dma(lambda e, i=i: nc.sync.dma_start(out=hres[:, i, :], in_=h_scr[s, i * 128:(i + 1) * 128, :]), "hres",
                  reads=[hb[s][i]], writes=[hresb[i]])
        gc = gcol[:, 2 + layer, :]
        for g in range(8):
            wu, wubb = wub[g % 2]
            wd, wdbb = wdb[g % 2]
            load_w(w_up[layer, :, g * 512:(g + 1) * 512], 8, 512, wu, wubb, gc)
            load_w(w_down[layer, g * 512:(g + 1) * 512, :], 4, DM, wd, wdbb)
            for fc in range(4):
                for tb in range(4):
                    pi = (fc * 4 + tb) % 4
                    mm_group(psf(pi), [(wu[:, kc, fc * 128:(fc + 1) * 128], nT[:, kc, tb * 512:(tb + 1) * 512])
                                       for kc in range(8)], nTb[4 * tb:4 * tb + 4] + [wubb], [PSB[pi]])
                    r, rb = rr.next()
                    S.act(lambda e, r=r, pi=pi: nc.scalar.activation(out=r, in_=psf(pi), func=AF.Relu),
                          reads=[PSB[pi]], writes=[rb])
                    S.pool(lambda e, r=r, fc=fc, tb=tb: nc.gpsimd.tensor_tensor(
                        out=actT[:, fc, tb * 512:(tb + 1) * 512], in0=r, in1=r, op=ALU.mult),
                        reads=[rb], writes=[actb[tb]])
            for ti in range(NT):
                for half in range(2):
                    pi = 4 + (ti * 2 + half) % 4
                    mm_group(psf(pi), [(actT[:, fc, ti * 128:(ti + 1) * 128], wd[:, fc, half * 512:(half + 1) * 512])
                                       for fc in range(4)], [actb[ti // 4], wdbb], [PSB[pi]])
                    S.dve(lambda e, pi=pi, ti=ti, half=half: nc.vector.tensor_tensor(
                        out=hres[:, ti, half * 512:(half + 1) * 512], in0=psf(pi),
                        in1=hres[:, ti, half * 512:(half + 1) * 512], op=ALU.add),
                        reads=[PSB[pi], hresb[ti]], writes=[hresb[ti]])
        dst = out if final else h_scr
        for i in range(NT):
            S.dma(lambda e, i=i: nc.sync.dma_start(out=dst[s, i * 128:(i + 1) * 128, :], in_=hres[:, i, :]),
                  f"st{i % 2}", reads=[hresb[i]], writes=[hb[s][i]])
        S.flush()
        A.release(m)

    def dsa(s):
        m = A.mark()
        kaT = A.alloc([128, 4, SEQ], BF16)
        qaT = A.alloc([128, 4, SEQ], BF16)
        va = A.alloc([128, NT, 4, 132], BF16)
        iqT = A.alloc([128, 4, SEQ], BF16)
        ikT = A.alloc([128, SEQ], BF16)
        iws = A.alloc([128, NT, 8], F32)
        kaTb, qaTb, vab, iqTb, ikTb, iwsb = Buf(), Buf(), Buf(), Buf(), Buf(), Buf()
        m2 = A.mark()
        AO = Arena(nc, 32768, base=oT_raw)
        stage_state["pool"] = mk_stage(AO)
        wbp = [(AO.alloc([128, 8, 512], BF16), Buf()) for _ in range(2)]
        wss = [mk_qk_ws(6), mk_qk_ws(7)]
        cp = [Rot(A, 1, [128, 512], F32) for _ in range(2)]
        cpb = [Rot(A, 1, [128, 512], BF16) for _ in range(2)]
        itmp = [Rot(A, 1, [128, 4, 8, 8], F32) for _ in range(2)]
        ikf = [Rot(A, 1, [128, 72], F32) for _ in range(2)]
        ikd = [Rot(A, 1, [128, 128], BF16) for _ in range(2)]
        gc = gcol[:, 0, :]
        S.pool(lambda e: nc.gpsimd.memset(va[:, :, :, 128:129], 1.0), writes=[vab])

        def h_va(pi, i, k):
            S.act(lambda e: nc.scalar.copy(out=va[:, i, :, 0:128], in_=psf(pi).rearrange("p (h d) -> p h d", h=4)),
                  reads=[PSB[pi]], writes=[vab])
            yield

        def h_iq(pi, i, k):
            c, cb = cp[k].next()
            S.act(lambda e: nc.scalar.copy(out=c, in_=psf(pi)), reads=[PSB[pi]], writes=[cb])
            yield
            tmp, tmpb = itmp[k].next()
            rope(c.rearrange("p (h d) -> p h d", h=8), cb, 8, 8, ropeI, i, tmp, tmpb)
            yield
            ch_, chb = cpb[k].next()
            S.act(lambda e: nc.scalar.copy(out=ch_, in_=c), reads=[cb], writes=[chb])
            yield
            ptv = psh(6 + k)
            transposes([(ptv[:, p * 128:(p + 1) * 128], ch_[:, p * 128:(p + 1) * 128]) for p in range(4)], ident,
                       [chb, constb], [PSB[6 + k]])
            yield
            S.act(lambda e: nc.scalar.copy(out=iqT[:, :, i * 128:(i + 1) * 128],
                                           in_=ptv[:, 0:512].rearrange("p (h t) -> p h t", h=4)),
                  reads=[PSB[6 + k]], writes=[iqTb])
            yield

        def h_ik(pi, i, k):
            f_, fb = ikf[k].next()
            S.act(lambda e: nc.scalar.copy(out=f_, in_=psf(pi)[:, 0:72]), reads=[PSB[pi]], writes=[fb])
            yield
            tmp, tmpb = itmp[k].next()
            rope(f_[:, 0:64].rearrange("p (h d) -> p h d", h=1), fb, 1, 8, ropeI, i, tmp[:, :, 0:1, :], tmpb)
            yield
            d_, db = ikd[k].next()

            def fcp(e):
                nc.vector.tensor_copy(out=d_[:, 0:64], in_=f_[:, 0:64])
                nc.vector.tensor_copy(out=d_[:, 64:128], in_=f_[:, 0:64])
                return nc.vector.tensor_scalar(out=iws[:, i, :], in0=f_[:, 64:72], scalar1=1.0 / (8.0 * 8.0 ** 0.5),
                                               scalar2=None, op0=ALU.mult)
            S.dve(fcp, reads=[fb], writes=[db, iwsb])
            yield
            ptv = psh(6 + k)
            transposes([(ptv[:, 0:128], d_)], ident, [db, constb], [PSB[6 + k]])
            yield
            S.act(lambda e: nc.scalar.copy(out=ikT[:, i * 128:(i + 1) * 128], in_=ptv[:, 0:128]),
                  reads=[PSB[6 + k]], writes=[ikTb])
            yield

        blocks = [("qa", 0), ("ka", 512), ("va", 1024), ("iq", 1536), ("ik", 2048)]
        for bi, (nm, c0) in enumerate(blocks):
            ncols = 72 if nm == "ik" else 512
            wb, wbb = wbp[bi % 2]
            load_w(w_in_ab[:, c0:c0 + ncols], 8, ncols, wb[:, :, 0:ncols], wbb, gc)
            if nm == "qa":
                proj_streamed(wb, wbb, 512, lambda pi, i, k: qk_post(pi, i, wss[k], 0, qaT, qaTb, 128 ** -0.5, True))
            elif nm == "ka":
                proj_streamed(wb, wbb, 512, lambda pi, i, k: qk_post(pi, i, wss[k], 1, kaT, kaTb, 1.0, True))
            elif nm == "va":
                proj_streamed(wb, wbb, 512, h_va)
            elif nm == "iq":
                proj_streamed(wb, wbb, 512, h_iq)
            else:
                proj_streamed(wb, wbb, 72, h_ik)
        S.flush()
        A.release(m2)
        AN = Arena(nc, 32768, base=nT_raw)
        junk = AN.alloc([128, SEQ], BF16)
        junkb = Buf()

        def dsa_stream(k):
            sc = AN.alloc([128, SEQ], F32)
            scb = Buf()
            mk = AN.alloc([128, SEQ], BF16)
            mkb = Buf()
            mT = A.alloc([128, NT, 128], BF16)
            mTb = Buf()
            tmpf = Rot(A, 2, [128, 512], F32)
            ptr = Rot(A, 2, [128, 512], BF16)
            ptmr = Rot(A, 2, [128, 512], BF16)
            bsr = Rot(A, 2, [128, 8], F32)
            rzr = Rot(A, 2, [128, 4], F32)
            oar = Rot(A, 1, [128, 512], BF16)
            pidx, pst, ppv = k, 2 + k, 4 + k
            yield
            for j in range(k, NT, 2):
                W = (j + 1) * 128
                for h in range(8):
                    p0 = (h % 2) * 64
                    pair = h // 2
                    for kb in range((W + 511) // 512):
                        c0 = kb * 512
                        cw = min(512, W - c0)
                        mm_group(psf(pidx)[:, 0:cw], [(iqT[p0:p0 + 64, pair, j * 128:(j + 1) * 128], ikT[p0:p0 + 64, c0:c0 + cw])],
                                 [iqTb, ikTb], [PSB[pidx]])
                        yield
                        t_, tb_ = tmpf.next()
                        S.act(lambda e, t_=t_, cw=cw: nc.scalar.activation(out=t_[:, 0:cw], in_=psf(pidx)[:, 0:cw], func=AF.Relu),
                              reads=[PSB[pidx]], writes=[tb_])
                        yield
                        if h == 0:
                            S.dve(lambda e, t_=t_, c0=c0, cw=cw, j=j: nc.vector.tensor_scalar(out=sc[:, c0:c0 + cw], in0=t_[:, 0:cw], scalar1=iws[:, j, 0:1],
                                                                    scalar2=None, op0=ALU.mult), reads=[tb_, iwsb], writes=[scb])
                        else:
                            S.dve(lambda e, t_=t_, c0=c0, cw=cw, j=j, h=h: nc.vector.scalar_tensor_tensor(out=sc[:, c0:c0 + cw], in0=t_[:, 0:cw],
                                                                           scalar=iws[:, j, h:h + 1], in1=sc[:, c0:c0 + cw],
                                                                           op0=ALU.mult, op1=ALU.add),
                                  reads=[tb_, iwsb, scb], writes=[scb])
                        yield
                S.dve(lambda e, W=W: nc.vector.memset(sc[0:64, W - 64:W], NEG), reads=[scb], writes=[scb])
                yield
                if j >= 2:
                    bs, bsb = bsr.next()

                    def f0(e, bs=bs, W=W):
                        nc.vector.tensor_reduce(out=bs[:, 0:1], in_=sc[:, 0:W - 64], axis=AX.X, op=ALU.min)
                        return nc.vector.tensor_reduce(out=bs[:, 1:2], in_=sc[:, 0:W], axis=AX.X, op=ALU.max)
                    S.dve(f0, reads=[scb], writes=[bsb])
                    yield
                    S.dve(lambda e, bs=bs: nc.vector.tensor_tensor(out=bs[:, 1:2], in0=bs[:, 1:2], in1=bs[:, 0:1], op=ALU.subtract),
                          reads=[bsb], writes=[bsb])
                    yield
                    for it in range(1, BIS_ITERS + 1):
                        f = 2.0 ** (-it)
                        S.dve(lambda e, bs=bs, f=f: nc.vector.tensor_scalar(out=bs[:, 2:3], in0=bs[:, 1:2], scalar1=f, scalar2=bs[:, 0:1],
                                                                op0=ALU.mult, op1=ALU.add), reads=[bsb], writes=[bsb])
                        yield
                        S.dve(lambda e, bs=bs, W=W: nc.vector.tensor_scalar(out=junk[:, 0:W], in0=sc[:, 0:W], scalar1=bs[:, 2:3], scalar2=None,
                                                                op0=ALU.is_ge, op1=ALU.add, accum_out=bs[:, 3:4]),
                              reads=[bsb, scb], writes=[bsb, junkb])
                        yield
                        S.dve(lambda e, bs=bs: nc.vector.tensor_scalar(out=bs[:, 4:5], in0=bs[:, 3:4], scalar1=255.5, scalar2=bs[:, 1:2],
                                                                op0=ALU.is_ge, op1=ALU.mult), reads=[bsb], writes=[bsb])
                        yield
                        S.dve(lambda e, bs=bs, f=f: nc.vector.scalar_tensor_tensor(out=bs[:, 0:1], in0=bs[:, 4:5], scalar=f, in1=bs[:, 0:1],
                                                                       op0=ALU.mult, op1=ALU.add), reads=[bsb], writes=[bsb])
                        yield
                    thr, thrb = bs[:, 0:1], bsb
                else:
                    thr, thrb = thr_all[:, 0:1], constb
                S.dve(lambda e, W=W, thr=thr: nc.vector.tensor_scalar(out=mk[:, 0:W], in0=sc[:, 0:W], scalar1=thr, scalar2=None, op0=ALU.is_ge),
                      reads=[scb, thrb], writes=[mkb])
                yield
                for g in range((j + 4) // 4):
                    kts = list(range(4 * g, min(4 * g + 4, j + 1)))
                    n = len(kts)
                    pv = psh(pidx)
                    transposes([(pv[:, q * 128:(q + 1) * 128], mk[:, kt * 128:(kt + 1) * 128]) for q, kt in enumerate(kts)],
                               ident, [mkb, constb], [PSB[pidx]])
                    yield
                    S.act(lambda e, g=g, n=n, pv=pv: nc.scalar.copy(out=mT[:, 4 * g:4 * g + n, :],
                                                   in_=pv[:, 0:n * 128].rearrange("p (k t) -> p k t", k=n)),
                          reads=[PSB[pidx]], writes=[mTb])
                    yield
                oa, oab = oar.next()
                for h in range(4):
                    oc = (h % 2) * 256
                    ov = psf(ppv)[:, oc:oc + 129]
                    for g in range((j + 4) // 4):
                        kts = list(range(4 * g, min(4 * g + 4, j + 1)))
                        n = len(kts)

                        def fs(e, kts=kts, h=h, j=j):
                            inst = None
                            for q, kt in enumerate(kts):
                                inst = nc.tensor.matmul(psf(pst)[:, q * 128:(q + 1) * 128], lhsT=kaT[:, h, kt * 128:(kt + 1) * 128],
                                                        rhs=qaT[:, h, j * 128:(j + 1) * 128], start=True, stop=True)
                            return inst
                        S.pe(fs, reads=[kaTb, qaTb], writes=[PSB[pst]])
                        yield
                        pt, ptb = ptr.next()
                        S.act(lambda e, pt=pt, n=n: nc.scalar.activation(out=pt[:, 0:n * 128], in_=psf(pst)[:, 0:n * 128], func=AF.Exp),
                              reads=[PSB[pst]], writes=[ptb])
                        yield
                        pm, pmb = ptmr.next()
                        S.dve(lambda e, pm=pm, pt=pt, g=g, n=n: nc.vector.tensor_tensor(out=pm[:, 0:n * 128], in0=pt[:, 0:n * 128],
                                                                in1=mT[:, 4 * g:4 * g + n, :].rearrange("p k t -> p (k t)"),
                                                                op=ALU.mult), reads=[ptb, mTb], writes=[pmb])
                        yield

                        def fo(e, kts=kts, pm=pm, ov=ov, h=h, j=j):
                            inst = None
                            for q, kt in enumerate(kts):
                                inst = nc.tensor.matmul(ov, lhsT=pm[:, q * 128:(q + 1) * 128], rhs=va[:, kt, h, 0:129],
                                                        start=(kt == 0), stop=(kt == j))
                            return inst
                        S.pe(fo, reads=[pmb, vab], writes=[PSB[ppv]])
                        yield
                    rz, rzb = rzr.next()
                    S.dve(lambda e, rz=rz, oc=oc: nc.vector.reciprocal(out=rz[:, 0:1], in_=psf(ppv)[:, oc + 128:oc + 129]),
                          reads=[PSB[ppv]], writes=[rzb])
                    yield
                    S.act(lambda e, oa=oa, h=h, oc=oc, rz=rz: nc.scalar.activation(out=oa[:, h * 128:(h + 1) * 128], in_=psf(ppv)[:, oc:oc + 128],
                                                         func=AF.Identity, scale=rz[:, 0:1]),
                          reads=[PSB[ppv], rzb], writes=[oab])
                    yield
                ptv = psh(pst)
                transposes([(ptv[:, h * 128:(h + 1) * 128], oa[:, h * 128:(h + 1) * 128]) for h in range(4)], ident,
                           [oab, constb], [PSB[pst]])
                yield
                S.act(lambda e, j=j, ptv=ptv: nc.scalar.copy(out=oT[:, 0:4, j * 128:(j + 1) * 128],
                                               in_=ptv[:, 0:512].rearrange("p (h t) -> p h t", h=4)),
                      reads=[PSB[pst]], writes=[oTb[j]])
                yield

        run_streams([dsa_stream(0), dsa_stream(1)])
        S.flush()
        A.release(m)

    def gla(s):
        m = A.mark()
        stage_state["pool"] = mk_stage()
        wq = A.alloc([128, 8, 256], BF16)
        wk = A.alloc([128, 8, 256], BF16)
        wv = A.alloc([128, 8, 512], BF16)
        wl = A.alloc([128, 8, 16], BF16)
        wo_ = A.alloc([128, 8, 512], BF16)
        wb_ = Buf()
        gc = gcol[:, 0, :]
        load_w(w_in_ab[:, 2120:2376], 8, 256, wq, wb_, gc)
        load_w(w_in_ab[:, 2376:2632], 8, 256, wk, wb_, gc)
        load_w(w_in_ab[:, 2632:3144], 8, 512, wv, wb_, gc)
        load_w(w_in_ab[:, 3144:3160], 8, 16, wl, wb_, gc)
        load_w(w_in_ab[:, 3160:3672], 8, 512, wo_, wb_, gc)
        glr = [(A.alloc([32, 64], F32), Buf()) for _ in range(2)]
        for gl_, glb in glr:
            S.dve(lambda e, gl_=gl_: nc.vector.memset(gl_, 1.0), writes=[glb])
        spr = Rot(A, 2, [64, 256], F32)
        ebr = Rot(A, 2, [128, 2, 2, 64], F32)
        ebvr = Rot(A, 2, [64, 256], F32)
        qer = Rot(A, 2, [128, 2, 2, 64], BF16)
        for q_, qb_ in qer.items:
            S.dve(lambda e, q_=q_: nc.vector.memset(q_, 0.0), writes=[qb_])
        kdr = Rot(A, 2, [128, 2, 64], BF16)
        klr = Rot(A, 2, [64, 256], BF16)
        vcr = Rot(A, 2, [64, 512], BF16)
        sgr = Rot(A, 2, [64, 512], F32)
        gvr = Rot(A, 2, [64, 512], F32)
        atr = Rot(A, 2, [64, 4, 64], BF16)
        sqr = Rot(A, 2, [64, 512], F32)
        ssr = Rot(A, 2, [64, 3, 4], F32)
        onr = Rot(A, 2, [64, 512], F32)
        obr = Rot(A, 2, [64, 512], BF16)
        Sf = A.alloc([128, 2, 128], F32)
        Sfb = [Buf(), Buf()]
        Sbf = [(A.alloc([128, 2, 128], BF16), [Buf(), Buf()]) for _ in range(2)]
        NCH = SEQ // 64
        for c in range(NCH):
            t0 = c * 64
            ntb = nTb[t0 // 128]
            gl_, glb = glr[c % 2]
            mm_group(psf(0)[0:16, 0:64], [(wl[:, kc, 0:16], nT[:, kc, t0:t0 + 64]) for kc in range(8)], [ntb, wb_], [PSB[0]])
            S.act(lambda e, gl_=gl_: nc.scalar.copy(out=gl_[0:16, :], in_=psf(0)[0:16, 0:64]), reads=[PSB[0]], writes=[glb])
            mm_group(psf(0)[0:64, 256:512], [(gl_[0:17, 0:64], wg[0:17, :])], [glb, constb], [PSB[0]])
            sp, spb = spr.next()
            S.act(lambda e, sp=sp: nc.scalar.activation(out=sp, in_=psf(0)[0:64, 256:512], func=AF.Exp, scale=-1.0),
                  reads=[PSB[0]], writes=[spb])
            S.act(lambda e, sp=sp: nc.scalar.activation(out=sp, in_=sp, func=AF.Ln, bias=1.0), reads=[spb], writes=[spb])
            def fb(e, sp=sp):
                nc.tensor.matmul(psf(1)[:, 0:64], lhsT=sp[:, 0:128], rhs=tri_incl, start=True, stop=True)
                nc.tensor.matmul(psf(1)[:, 64:128], lhsT=sp[:, 128:256], rhs=tri_incl, start=True, stop=True)
                return nc.tensor.matmul(psf(1)[0:64, 128:384], lhsT=tri_rev, rhs=sp, start=True, stop=True)
            S.pe(fb, reads=[spb, constb], writes=[PSB[1]])
            eb, ebb = ebr.next()
            ebv, ebvb = ebvr.next()

            def fe(e, eb=eb, ebv=ebv):
                bv = psf(1)[:, 0:128].rearrange("p (g t) -> p g t", g=2)
                nc.scalar.activation(out=eb[:, 0], in_=bv, func=AF.Exp)
                nc.scalar.activation(out=eb[:, 1], in_=bv, func=AF.Exp, scale=-1.0)
                return nc.scalar.activation(out=ebv, in_=psf(1)[0:64, 128:384], func=AF.Exp)
            S.act(fe, reads=[PSB[1]], writes=[ebb, ebvb])
            def fqk(e, t0=t0):
                inst = None
                for g in range(2):
                    for kc in range(8):
                        nc.tensor.matmul(psf(2)[:, g * 64:(g + 1) * 64], lhsT=wq[:, kc, g * 128:(g + 1) * 128],
                                         rhs=nT[:, kc, t0:t0 + 64], start=(kc == 0), stop=(kc == 7))
                for g in range(2):
                    for kc in range(8):
                        nc.tensor.matmul(psf(2)[:, 128 + g * 64:128 + (g + 1) * 64], lhsT=wk[:, kc, g * 128:(g + 1) * 128],
                                         rhs=nT[:, kc, t0:t0 + 64], start=(kc == 0), stop=(kc == 7))
                for kc in range(8):
                    inst = nc.tensor.matmul(psf(2)[0:64, 256:512], lhsT=nT[:, kc, t0:t0 + 64], rhs=wk[:, kc, :],
                                            start=(kc == 0), stop=(kc == 7))
                return inst
            S.pe(fqk, reads=[ntb, wb_], writes=[PSB[2]])
            qe, qeb = qer.next()
            kd, kdb = kdr.next()
            kl, klb = klr.next()

            def fq(e, qe=qe, kd=kd, kl=kl, eb=eb, ebv=ebv):
                for hh in range(2):
                    p0 = hh * 64
                    nc.vector.scalar_tensor_tensor(out=qe[p0:p0 + 64, :, hh, :],
                                                   in0=psf(2)[p0:p0 + 64, 0:128].rearrange("p (g t) -> p g t", g=2),
                                                   scalar=0.125, in1=eb[p0:p0 + 64, 0], op0=ALU.mult, op1=ALU.mult)
                nc.vector.tensor_tensor(out=kd, in0=psf(2)[:, 128:256].rearrange("p (g t) -> p g t", g=2), in1=eb[:, 1],
                                        op=ALU.mult)
                return nc.vector.tensor_tensor(out=kl, in0=psf(2)[0:64, 256:512], in1=ebv, op=ALU.mult)
            S.dve(fq, reads=[PSB[2], ebb, ebvb], writes=[qeb, kdb, klb])
            mm_group(psf(3)[0:64, :], [(nT[:, kc, t0:t0 + 64], wv[:, kc, :]) for kc in range(8)], [ntb, wb_], [PSB[3]])
            vc, vcb = vcr.next()
            S.act(lambda e, vc=vc: nc.scalar.copy(out=vc, in_=psf(3)[0:64, :]), reads=[PSB[3]], writes=[vcb])
            mm_group(psf(4)[0:64, :], [(nT[:, kc, t0:t0 + 64], wo_[:, kc, :]) for kc in range(8)], [ntb, wb_], [PSB[4]])
            sg, sgb = sgr.next()

            def fsg(e, sg=sg):
                nc.scalar.activation(out=sg, in_=psf(4)[0:64, :], func=AF.Exp, scale=-1.0)
                nc.scalar.activation(out=sg, in_=sg, func=AF.Ln, bias=1.0)
                return nc.scalar.activation(out=sg, in_=sg, func=AF.Exp, scale=-1.0)
            S.act(fsg, reads=[PSB[4]], writes=[sgb])
            gv, gvb = gvr.next()
            S.dve(lambda e, gv=gv, sg=sg: nc.vector.tensor_tensor(out=gv, in0=psf(4)[0:64, :], in1=sg, op=ALU.mult),
                  reads=[PSB[4], sgb], writes=[gvb])
            def fat(e, kd=kd, qe=qe):
                inst = None
                for g in range(2):
                    for hh in range(2):
                        p0 = hh * 64
                        q = g * 2 + hh
                        inst = nc.tensor.matmul(psf(5)[0:64, q * 64:(q + 1) * 64], lhsT=kd[:, g, :],
                                                rhs=qe[:, g, hh, :], start=True, stop=True)
                return inst
            S.pe(fat, reads=[kdb, qeb], writes=[PSB[5]])
            at, atb = atr.next()
            S.dve(lambda e, at=at: nc.vector.tensor_tensor(
                out=at, in0=psf(5)[0:64, 0:256].rearrange("p (q t) -> p q t", q=4),
                in1=causT.unsqueeze(1).to_broadcast([64, 4, 64]), op=ALU.mult), reads=[PSB[5], constb], writes=[atb])
            sbf_prev, sbfb_prev = Sbf[(c + 1) % 2]
            sbf_cur, sbfb_cur = Sbf[c % 2]

            def fo(e, qe=qe, at=at, vc=vc, c=c, sbf_prev=sbf_prev):
                inst = None
                for g in range(2):
                    for hh in range(2):
                        p0 = hh * 64
                        q = g * 2 + hh
                        ov = psf(6)[0:64, q * 128:(q + 1) * 128]
                        if c > 0:
                            nc.tensor.matmul(ov, lhsT=qe[:, g, hh, :], rhs=sbf_prev[:, g, :], start=True, stop=False)
                        inst = nc.tensor.matmul(ov, lhsT=at[:, q, :], rhs=vc[:, q * 128:(q + 1) * 128], start=(c == 0), stop=True)
                return inst
            S.pe(fo, reads=[qeb, atb, vcb] + (sbfb_prev if c > 0 else []), writes=[PSB[6]])
            if c < NCH - 1:
                def fu(e, kl=kl, vc=vc):
                    nc.tensor.matmul(psf(7)[:, 0:256], lhsT=kl[:, 0:128], rhs=vc[:, 0:256], start=True, stop=True)
                    return nc.tensor.matmul(psf(7)[:, 256:512], lhsT=kl[:, 128:256], rhs=vc[:, 256:512], start=True, stop=True)
                S.pe(fu, reads=[klb, vcb], writes=[PSB[7]])
                for g in range(2):
                    def fs_(e, g=g, eb=eb, c=c):
                        inst = None
                        for hh in range(2):
                            p0 = hh * 64
                            uv = psf(7)[p0:p0 + 64, g * 256 + hh * 128:g * 256 + (hh + 1) * 128]
                            if c == 0:
                                inst = nc.vector.tensor_copy(out=Sf[p0:p0 + 64, g, :], in_=uv)
                            else:
                                inst = nc.vector.scalar_tensor_tensor(out=Sf[p0:p0 + 64, g, :], in0=Sf[p0:p0 + 64, g, :],
                                                                      scalar=eb[p0:p0 + 64, 0, g, 63:64], in1=uv,
                                                                      op0=ALU.mult, op1=ALU.add)
                        return inst
                    S.dve(fs_, reads=[PSB[7], ebb, Sfb[g]], writes=[Sfb[g]])
                    S.act(lambda e, g=g, sbf_cur=sbf_cur: nc.scalar.copy(out=sbf_cur[:, g, :], in_=Sf[:, g, :]),
                          reads=[Sfb[g]], writes=[sbfb_cur[g]])
            sq, sqb = sqr.next()
            ss, ssb = ssr.next()
            S.act(lambda e, sq=sq: nc.scalar.activation(out=sq, in_=psf(6)[0:64, :], func=AF.Square), reads=[PSB[6]], writes=[sqb])
            S.dve(lambda e, sq=sq, ss=ss: nc.vector.tensor_reduce(out=ss[:, 0, :], in_=sq.rearrange("p (h d) -> p h d", h=4),
                                                                  axis=AX.X, op=ALU.add), reads=[sqb], writes=[ssb])
            rstd_from_ss(ss, ssb, 4, 1.0 / 128, True)
            on, onb = onr.next()

            def fn_(e, on=on, ss=ss):
                inst = None
                for h in range(4):
                    inst = nc.vector.scalar_tensor_tensor(out=on[:, h * 128:(h + 1) * 128], in0=psf(6)[0:64, h * 128:(h + 1) * 128],
                                                          scalar=ss[:, 2, h:h + 1], in1=gains[0:64, 4, :], op0=ALU.mult,
                                                          op1=ALU.mult)
                return inst
            S.dve(fn_, reads=[PSB[6], ssb, constb], writes=[onb])
            ob, obb = obr.next()
            S.dve(lambda e, ob=ob, on=on, gv=gv: nc.vector.tensor_tensor(out=ob, in0=on, in1=gv, op=ALU.mult),
                  reads=[onb, gvb], writes=[obb])
            ptv = psh(5)
            transposes([(ptv[:, 512 + h * 64:512 + (h + 1) * 64], ob[:, h * 128:(h + 1) * 128]) for h in range(4)],
                       ident[0:64, 0:64], [obb, constb], [PSB[5]])
            S.act(lambda e, t0=t0, ptv=ptv: nc.scalar.copy(out=oT[:, 4:8, t0:t0 + 64],
                                                           in_=ptv[:, 512:768].rearrange("p (h t) -> p h t", h=4)),
                  reads=[PSB[5]], writes=[oTb[t0 // 128]])
        S.flush()
        A.release(m)

    def sb_attn(s):
        m = A.mark()
        qT = A.alloc([128, 4, SEQ], BF16)
        kT = A.alloc([128, 4, SEQ], BF16)
        v = A.alloc([128, NT, 512], BF16)
        qTb, kTb, vb_ = Buf(), Buf(), Buf()
        gc = gcol[:, 1, :]
        for hg in range(2):
            mp = A.mark()
            stage_state["pool"] = mk_stage()
            wbp = [(A.alloc([128, 8, 512], BF16), Buf()) for _ in range(2)]
            wss = [mk_qk_ws(6), mk_qk_ws(7)]
            for bi, (nm, c0) in enumerate((("q", hg * 512), ("k", 1024 + hg * 512), ("v", 2048 + hg * 512))):
                wb, wbb = wbp[bi % 2]
                load_w(w_in_c[:, c0:c0 + 512], 8, 512, wb, wbb, gc)
                if nm == "q":
                    proj_streamed(wb, wbb, 512, lambda pi, i, k: qk_post(pi, i, wss[k], 2, qT, qTb, 128 ** -0.5, False))
                elif nm == "k":
                    proj_streamed(wb, wbb, 512, lambda pi, i, k: qk_post(pi, i, wss[k], 3, kT, kTb, 1.0, False))
                else:
                    def hv(pi, i, k):
                        S.act(lambda e: nc.scalar.copy(out=v[:, i, :], in_=psf(pi)), reads=[PSB[pi]], writes=[vb_])
                        yield
                    proj_streamed(wb, wbb, 512, hv)
            S.flush()
            A.release(mp)
            def sb_stream(st_, heads, hg=hg):
                pz, pc, po = 0 + st_, 2 + st_, 4 + st_
                espr = Rot(A, 2, [128, 512], F32)
                lor = Rot(A, 2, [128, 512], BF16)
                ar = Rot(A, 2, [128, 512], BF16)
                lbs = [(A.alloc([128, 512], BF16), Buf()) for _ in range(3)]
                gs = [0]

                def S1(f):
                    h, qb, kt, step, nsteps = f["h"], f["qb"], f["kt"], f["step"], f["nsteps"]
                    cc, ncol, diag = f["cc"], f["ncol"], f["diag"]
                    q0 = qb * 512 + cc
                    mm_group(psf(pz)[:, 0:ncol], [(kT[:, h, kt * 128:(kt + 1) * 128], qT[:, h, q0:q0 + ncol])],
                             [kTb, qTb], [PSB[pz]])
                    yield
                    es, esb = espr.next()
                    f["es"], f["esb"] = es, esb
                    S.act(lambda e: nc.scalar.activation(out=es[:, 0:ncol], in_=psf(pz)[:, 0:ncol], func=AF.Exp, scale=-1.0),
                          reads=[PSB[pz]], writes=[esb])
                    yield
                    S.act(lambda e: nc.scalar.activation(out=es[:, 0:ncol], in_=es[:, 0:ncol], func=AF.Ln, bias=1.0),
                          reads=[esb], writes=[esb])
                    yield
                    lo, lob = lor.next()
                    f["lo"], f["lob"] = lo, lob
                    S.dve(lambda e: nc.vector.scalar_tensor_tensor(out=lo[:, 0:ncol], in0=psf(pz)[:, 0:ncol], scalar=-1.0,
                                                                   in1=es[:, 0:ncol], op0=ALU.mult, op1=ALU.subtract),
                          reads=[PSB[pz], esb], writes=[lob])
                    yield
                    if diag:
                        S.pool(lambda e: nc.gpsimd.tensor_tensor(out=lo[:, 0:128], in0=lo[:, 0:128], in1=strictT, op=ALU.mult),
                               reads=[lob, constb], writes=[lob])
                        yield
                    g = gs[0]
                    gs[0] += 1
                    f["lbc"] = lbs[g % 3]
                    if step < nsteps - 1:
                        lbn, lbnb = lbs[(g + 1) % 3]
                        lbc, lbcb = lbs[g % 3]
                        ccn = f["cc_next"]
                        if ccn < cc:
                            S.pool(lambda e: nc.gpsimd.memset(lbn[:, ccn:cc], 0.0), writes=[lbnb])
                            yield
                        if step == 0:
                            S.dve(lambda e: nc.vector.tensor_copy(out=lbn[:, cc:512], in_=lo[:, 0:ncol]), reads=[lob], writes=[lbnb])
                        else:
                            S.dve(lambda e: nc.vector.tensor_tensor(out=lbn[:, cc:512], in0=lbc[:, cc:512], in1=lo[:, 0:ncol],
                                                                    op=ALU.add), reads=[lob, lbcb], writes=[lbnb])
                        yield

                def S2(f):
                    h, qb, kt, step, nsteps = f["h"], f["qb"], f["kt"], f["step"], f["nsteps"]
                    cc, ncol, diag = f["cc"], f["ncol"], f["diag"]
                    es, esb, lo, lob = f["es"], f["esb"], f["lo"], f["lob"]
                    lbc, lbcb = f["lbc"]
                    if step == 0:
                        mm_group(psf(pc)[:, 0:ncol], [(triU, lo[:, 0:ncol])], [lob, constb], [PSB[pc]])
                    else:
                        mm_group(psf(pc)[:, 0:ncol], [(triU, lo[:, 0:ncol]), (ones128, lbc[:, cc:512])],
                                 [lob, lbcb, constb], [PSB[pc]])
                    yield
                    S.dve(lambda e: nc.vector.tensor_tensor(out=es[:, 0:ncol], in0=psf(pc)[:, 0:ncol], in1=es[:, 0:ncol],
                                                            op=ALU.subtract), reads=[PSB[pc], esb], writes=[esb])
                    yield
                    a_, ab_ = ar.next()
                    S.act(lambda e: nc.scalar.activation(out=a_[:, 0:ncol], in_=es[:, 0:ncol], func=AF.Exp),
                          reads=[esb], writes=[ab_])
                    yield
                    if diag:
                        S.pool(lambda e: nc.gpsimd.tensor_tensor(out=a_[:, 0:128], in0=a_[:, 0:128], in1=strictT, op=ALU.mult),
                               reads=[ab_, constb], writes=[ab_])
                        yield
                    S.pe(lambda e: nc.tensor.matmul(psf(po)[:, cc:512], lhsT=v[:, kt, h * 128:(h + 1) * 128], rhs=a_[:, 0:ncol],
                                                    start=(step == 0), stop=(step == nsteps - 1)),
                         reads=[vb_, ab_], writes=[PSB[po]])
                    yield
                    if step == nsteps - 1:
                        hh = hg * 4 + h
                        S.act(lambda e: nc.scalar.copy(out=oT[:, hh, qb * 512:(qb + 1) * 512], in_=psf(po)),
                              reads=[PSB[po]], writes=oTb[4 * qb:4 * qb + 4])
                        yield

                pending = None
                for h in heads:
                    for qb in range(4):
                        kts = list(range(4 * qb + 3, -1, -1))
                        ccs = [max(0, kt - 4 * qb) * 128 for kt in kts]
                        for step, kt in enumerate(kts):
                            f = {"h": h, "qb": qb, "kt": kt, "step": step, "nsteps": len(kts), "cc": ccs[step],
                                 "ncol": 512 - ccs[step], "diag": kt >= 4 * qb,
                                 "cc_next": ccs[step + 1] if step + 1 < len(kts) else 0}
                            yield from S1(f)
                            if pending is not None:
                                yield from S2(pending)
                            pending = f
                yield from S2(pending)

            run_streams([sb_stream(0, [0, 1]), sb_stream(1, [2, 3])])
            S.flush()
            A.release(mp)
        A.release(m)

    def dump_h(s):
        m = A.mark()
        t = [(A.alloc([128, 1024], F32), Buf(), f"xin{k}") for k in range(2)]
        for i in range(NT):
            tt, tb, ch = t[i % 2]
            S.dma(lambda e, tt=tt, i=i: nc.sync.dma_start(out=tt, in_=h_scr[s, i * 128:(i + 1) * 128, :]), ch,
                  reads=[hb[s][i]], writes=[tb])
            S.dma(lambda e, tt=tt, i=i: nc.sync.dma_start(out=out[s, i * 128:(i + 1) * 128, :], in_=tt), f"st{i % 2}",
                  reads=[tb], writes=[])
        S.flush()
        A.release(m)

    for s in range(NSEQ):
        if stop != "const":
            phase_norm(s, x)
        if stop == "norm":
            continue
        if stop == "const":
            continue
        dsa(s)
        if stop == "dsa":
            continue
        phase_norm(s, x)
        gla(s)
        if stop == "gla":
            continue
        out_proj_and_norm(s, w_out_ab, x, h_scr)
        if stop == "mix0":
            dump_h(s)
            continue
        ffn(s, 0, False)
        if stop == "ffn0":
            dump_h(s)
            continue
        phase_norm(s, h_scr)
        sb_attn(s)
        out_proj_and_norm(s, w_out_c, h_scr, h_scr)
        if stop == "mix1":
            dump_h(s)
            continue
        ffn(s, 1, True)
    S.flush(final=True)
    return nc


def host_constants():
    c = {}
    c["c_ident"] = np.eye(128, dtype=np.float32)
    pos = np.arange(SEQ, dtype=np.float32)
    inv_a = np.power(np.float32(500000.0), -np.arange(16, dtype=np.float32) * 2.0 / 32).astype(np.float32)
    ang = pos[:, None] * inv_a[None, :]
    c["c_rope_a"] = np.concatenate([np.cos(ang), np.sin(ang)], axis=1).astype(np.float32)
    inv_i = np.power(np.float32(500000.0), -np.arange(8, dtype=np.float32) * 2.0 / 16).astype(np.float32)
    ang = pos[:, None] * inv_i[None, :]
    c["c_rope_i"] = np.concatenate([np.cos(ang), np.sin(ang)], axis=1).astype(np.float32)
    a = np.arange(64)
    m64 = np.zeros((64, 3, 64), np.float32)
    m64[:, 0, :] = (a[:, None] <= a[None, :]) * (-1.0 / 16.0)
    m64[:, 1, :] = (a[:, None] > a[None, :]) * (-1.0 / 16.0)
    m64[:, 2, :] = (a[:, None] <= a[None, :]) * 1.0
    c["c_m64"] = m64
    b = np.arange(128)
    m128 = np.zeros((128, 3, 128), np.float32)
    m128[:, 0, :] = (b[:, None] > b[None, :]) * 1.0
    m128[:, 1, :] = 1.0
    m128[:, 2, :] = (b[:, None] < b[None, :]) * 1.0
    c["c_m128"] = m128
    return c


_CACHE = {}


def kernel(x, g_mix, g_ffn, w_in_ab, gq_a, gk_a, w_gate_up, b_gate, g_gla, w_out_ab,
           w_in_c, gq_c, gk_c, w_out_c, w_up, w_down, _ncores=8, _stop=None):
    f = lambda a: np.ascontiguousarray(np.asarray(a, dtype=np.float32))
    x = f(x)
    nseq = x.shape[0] // _ncores
    shared = {
        "w_in_ab": f(w_in_ab)[0], "w_gate_up": f(w_gate_up)[0], "b_gate": f(b_gate), "w_out_ab": f(w_out_ab)[0],
        "w_in_c": f(w_in_c)[0], "w_out_c": f(w_out_c)[0], "w_up": f(w_up), "w_down": f(w_down),
    }
    gains = np.stack([np.broadcast_to(f(g).reshape(1, 128), (128, 128)) for g in (gq_a, gk_a, gq_c, gk_c, g_gla)], axis=1)
    shared["c_gains"] = np.ascontiguousarray(gains, dtype=np.float32)
    gcols = np.stack([f(g_mix)[0].reshape(8, 128).T, f(g_mix)[1].reshape(8, 128).T,
                      f(g_ffn)[0].reshape(8, 128).T, f(g_ffn)[1].reshape(8, 128).T], axis=1)
    shared["c_gcol"] = np.ascontiguousarray(gcols, dtype=np.float32)
    shared.update(host_constants())
    key = (nseq, _stop)
    if key not in _CACHE:
        _CACHE[key] = build_program(nseq, _stop)
    nc = _CACHE[key]
    in_maps = []
    for c in range(_ncores):
        d = dict(shared)
        d["x"] = np.ascontiguousarray(x[c * nseq:(c + 1) * nseq])
        in_maps.append(d)
    res = run_bass_kernel_spmd(nc, in_maps, core_ids=list(range(_ncores)))
    return np.concatenate([np.asarray(r["out"], dtype=np.float32) for r in res.results], axis=0)
```

```python
import numpy as np
import concourse.bass as bass
import concourse.mybir as mybir
from concourse.bass_utils import run_bass_kernel_spmd
from concourse.alu_op_type import AluOpType as ALU

F32 = mybir.dt.float32
BF16 = mybir.dt.bfloat16
AF = mybir.ActivationFunctionType
AX = mybir.AxisListType

SEQ = 2048
DM = 1024
NT = SEQ // 128
EPS = 1e-6
ABW = 3672
NEG = -1.0e30
BIS_ITERS = 12
ARENA_BYTES = 175 * 1024

_ENG_ATTR = {"pe": "tensor", "act": "scalar", "dve": "vector", "pool": "gpsimd", "sp": "sync"}


class Buf:
    __slots__ = ("lw", "rd")

    def __init__(self):
        self.lw = None
        self.rd = {}


class Sched:
    ENG = ("pe", "act", "dve", "pool", "sp")

    def __init__(self, nc):
        self.nc = nc
        self.sem = {e: nc.alloc_semaphore("sem_" + e) for e in self.ENG}
        self.cnt = {e: 0 for e in self.ENG}
        self.ops = {e: [] for e in self.ENG}
        self.waited = {e: {} for e in self.ENG}
        self.chan = {}

    def channel(self, name):
        if name not in self.chan:
            self.chan[name] = [self.nc.alloc_semaphore("ch_" + name), 0]
        return name

    def _deps(self, reads, writes):
        d = {}
        for b in reads:
            if b.lw is not None and d.get(b.lw[0], 0) < b.lw[1]:
                d[b.lw[0]] = b.lw[1]
        for b in writes:
            if b.lw is not None and d.get(b.lw[0], 0) < b.lw[1]:
                d[b.lw[0]] = b.lw[1]
            for k, v in b.rd.items():
                if d.get(k, 0) < v:
                    d[k] = v
        return d

    def op(self, eng, fn, reads=(), writes=()):
        d = self._deps(reads, writes)
        self.cnt[eng] += 1
        idx = self.cnt[eng]
        for b in reads:
            b.rd[eng] = idx
        for b in writes:
            b.lw = (eng, idx)
            b.rd = {}
        self.ops[eng].append((d, fn, None))

    def pe(self, fn, reads=(), writes=()):
        self.op("pe", fn, reads, writes)

    def act(self, fn, reads=(), writes=()):
        self.op("act", fn, reads, writes)

    def dve(self, fn, reads=(), writes=()):
        self.op("dve", fn, reads, writes)

    def pool(self, fn, reads=(), writes=()):
        self.op("pool", fn, reads, writes)

    def dma(self, fn, chan, reads=(), writes=(), queue="sp"):
        d = self._deps(reads, writes)
        c = self.chan[chan]
        c[1] += 16
        key = "ch:" + chan
        for b in reads:
            b.rd[key] = c[1]
        for b in writes:
            b.lw = (key, c[1])
            b.rd = {}
        self.ops[queue].append((d, fn, chan))

    def _semof(self, k):
        if k.startswith("ch:"):
            return self.chan[k[3:]][0]
        return self.sem[k]

    def flush(self, final=False):
        nc = self.nc
        if final:
            d = {}
            for name, c in self.chan.items():
                if c[1] > 0:
                    d["ch:" + name] = c[1]
            self.ops["sp"].append((d, None, None))
        with nc.Block() as blk:
            for eng in self.ENG:
                ops = self.ops[eng]

                def body(e, eng=eng, ops=ops):
                    w = self.waited[eng]
                    for d, fn, chan in ops:
                        for k, v in d.items():
                            if k == eng and eng == "pe":
                                continue
                            if w.get(k, 0) < v:
                                e.wait_ge(self._semof(k), v)
                                w[k] = v
                        if fn is None:
                            continue
                        inst = fn(e)
                        if chan is None:
                            inst.then_inc(self.sem[eng], 1)
                        else:
                            inst.then_inc(self.chan[chan][0], 16)

                getattr(blk, _ENG_ATTR[eng])(body)
        self.ops = {e: [] for e in self.ENG}


class Arena:
    def __init__(self, nc, nbytes, base=None):
        self.t = nc.alloc_sbuf_tensor("arena", [128, nbytes // 4], F32) if base is None else base
        self.top = 0
        self.nbytes = nbytes

    def mark(self):
        return self.top

    def release(self, m):
        self.top = m

    def alloc(self, shape, dtype):
        esz = 4 if dtype == F32 else 2
        n = int(np.prod(shape[1:]))
        nb = (n * esz + 63) // 64 * 64
        off = self.top
        self.top += nb
        assert self.top <= self.nbytes, ("arena overflow", self.top)
        ap = self.t[0:shape[0], off // 4:(off + nb) // 4]
        if dtype != F32:
            ap = ap.bitcast(dtype)
        ap = ap[:, 0:n]
        if len(shape) == 3:
            ap = ap.rearrange("p (a b) -> p a b", a=shape[1])
        elif len(shape) == 4:
            ap = ap.rearrange("p (a b c) -> p a b c", a=shape[1], b=shape[2])
        return ap


class Rot:
    def __init__(self, arena, n, shape, dtype):
        self.items = [(arena.alloc(shape, dtype), Buf()) for _ in range(n)]
        self.i = 0

    def next(self):
        it = self.items[self.i % len(self.items)]
        self.i += 1
        return it


def run_streams(gens):
    gens = list(gens)
    while gens:
        for g in list(gens):
            try:
                next(g)
            except StopIteration:
                gens.remove(g)


def build_program(NSEQ=2, stop=None):
    nc = bass.Bass("TRN2", target_bir_lowering=False)
    S = Sched(nc)

    def dram_in(name, shape):
        return nc.dram_tensor(name, shape, F32, kind="ExternalInput").ap()

    x = dram_in("x", [NSEQ, SEQ, DM])
    w_in_ab = dram_in("w_in_ab", [DM, ABW])
    w_gate_up = dram_in("w_gate_up", [16, 256])
    b_gate = dram_in("b_gate", [1, 256])
    w_out_ab = dram_in("w_out_ab", [DM, DM])
    w_in_c = dram_in("w_in_c", [DM, 3 * DM])
    w_out_c = dram_in("w_out_c", [DM, DM])
    w_up = dram_in("w_up", [2, DM, 4 * DM])
    w_down = dram_in("w_down", [2, 4 * DM, DM])
    c_ident = dram_in("c_ident", [128, 128])
    c_rope_a = dram_in("c_rope_a", [SEQ, 32])
    c_rope_i = dram_in("c_rope_i", [SEQ, 16])
    c_m64 = dram_in("c_m64", [64, 3, 64])
    c_m128 = dram_in("c_m128", [128, 3, 128])
    c_gains = dram_in("c_gains", [128, 5, 128])
    c_gcol = dram_in("c_gcol", [128, 4, 8])
    out = nc.dram_tensor("out", [NSEQ, SEQ, DM], F32, kind="ExternalOutput").ap()
    h_scr = nc.dram_tensor("h_scr", [NSEQ, SEQ, DM], F32).ap()
    hb = [[Buf() for _ in range(NT)] for _ in range(NSEQ)]

    A = Arena(nc, ARENA_BYTES)
    PS = [nc.alloc_psum_tensor(f"psb{i}", [128, 512], F32) for i in range(8)]
    PSB = [Buf() for _ in range(8)]

    def psf(i):
        return PS[i][:, :]

    def psh(i):
        return PS[i][:, :].bitcast(BF16)

    for nm in ("c0", "c1", "c2", "c3", "xin0", "xin1", "xin2", "xin3", "st0", "st1", "stg0", "stg1", "hres"):
        S.channel(nm)

    nT_raw = A.alloc([128, 8192], F32)
    nT = nT_raw.bitcast(BF16).rearrange("p (a b) -> p a b", a=8)
    nTb = [Buf() for _ in range(NT)]
    oT_raw = A.alloc([128, 8192], F32)
    oT = oT_raw.bitcast(BF16).rearrange("p (a b) -> p a b", a=8)
    oTb = [Buf() for _ in range(NT)]
    identf = A.alloc([128, 128], F32)
    ident = A.alloc([128, 128], BF16)
    ropeA = A.alloc([128, NT, 32], F32)
    ropeI = A.alloc([128, NT, 16], F32)
    m64 = A.alloc([64, 3, 64], F32)
    m128f = A.alloc([128, 3, 128], F32)
    m128 = A.alloc([128, 3, 128], BF16)
    gains = A.alloc([128, 5, 128], F32)
    gcol = A.alloc([128, 4, 8], F32)
    wg = A.alloc([32, 256], F32)
    thr_all = A.alloc([128, 1], F32)
    constb = Buf()

    def bc_row(ap):
        r = ap.partition_broadcast(128)
        if len(r.shape) == 3:
            r = r.rearrange("p a b -> p (a b)")
        return r

    def dma_simple(out_ap, in_ap, chan, writes, reads=(), ncdma=True):
        S.dma(lambda e: nc.sync.dma_start(out=out_ap, in_=in_ap), chan, reads=reads, writes=writes)

    dma_simple(identf, c_ident, "c0", [constb])
    dma_simple(ropeA, c_rope_a.rearrange("(i p) c -> p i c", p=128), "c0", [constb])
    dma_simple(ropeI, c_rope_i.rearrange("(i p) c -> p i c", p=128), "c0", [constb])
    dma_simple(m64, c_m64, "c0", [constb])
    dma_simple(m128f, c_m128, "c0", [constb])
    dma_simple(gains, c_gains, "c0", [constb])
    dma_simple(wg[0:16, :], w_gate_up, "c0", [constb])
    dma_simple(wg[16:17, :], b_gate, "c0", [constb])
    dma_simple(gcol, c_gcol, "c0", [constb])
    S.dve(lambda e: nc.vector.tensor_copy(out=ident, in_=identf), reads=[constb], writes=[constb])
    S.dve(lambda e: nc.vector.tensor_copy(out=m128, in_=m128f), reads=[constb], writes=[constb])
    S.dve(lambda e: nc.vector.memset(thr_all, -1.0e29), reads=[], writes=[constb])
    S.flush()
    base_mark = A.mark()

    triU = m128[:, 0, :]
    ones128 = m128[:, 1, :]
    strictT = m128[:, 2, :]
    tri_incl = m64[:, 0, :]
    tri_rev = m64[:, 1, :]
    causT = m64[:, 2, :]

    def mk_stage(ar=None):
        ar = A if ar is None else ar
        return [(ar.alloc([128, 2048], F32), Buf(), S.channel(f"stg{i}")) for i in range(2)]

    stage_state = {"pool": None, "i": 0}

    def load_w(src2d, KC, ncols, dst, dstb, gc=None):
        srcv = src2d.rearrange("(k p) c -> p k c", p=128)
        kstep = max(1, 2048 // ncols)
        for k0 in range(0, KC, kstep):
            kn = min(kstep, KC - k0)
            st, stb, ch = stage_state["pool"][stage_state["i"] % 2]
            stage_state["i"] += 1
            stv = st[:, 0:kn * ncols].rearrange("p (k c) -> p k c", k=kn)
            S.dma(lambda e, stv=stv, k0=k0, kn=kn: nc.sync.dma_start(out=stv, in_=srcv[:, k0:k0 + kn, :]),
                  ch, writes=[stb])
            if gc is None:
                S.pool(lambda e, stv=stv, k0=k0, kn=kn: nc.gpsimd.tensor_copy(out=dst[:, k0:k0 + kn, :], in_=stv),
                       reads=[stb], writes=[dstb])
            else:
                S.pool(lambda e, stv=stv, k0=k0, kn=kn: nc.gpsimd.tensor_tensor(
                    out=dst[:, k0:k0 + kn, :], in0=stv,
                    in1=gc[:, k0:k0 + kn].unsqueeze(2).to_broadcast([128, kn, ncols]), op=ALU.mult),
                    reads=[stb, constb], writes=[dstb])

    def mm_group(out_ap, pairs, rd, wr):
        def f(e):
            n = len(pairs)
            inst = None
            for q, (l, r) in enumerate(pairs):
                inst = nc.tensor.matmul(out_ap, lhsT=l, rhs=r, start=(q == 0), stop=(q == n - 1))
            return inst
        S.pe(f, reads=rd, writes=wr)

    def transposes(outs_ins, idn, rd, wr):
        def f(e):
            inst = None
            for o, i_ in outs_ins:
                inst = nc.tensor.transpose(o, i_, idn)
            return inst
        S.pe(f, reads=rd, writes=wr)

    def rstd_from_ss(ssv, ssb, n, inv_n, add_eps):
        P = ssv.shape[0]
        if add_eps:
            S.dve(lambda e: nc.vector.tensor_scalar(out=ssv[:, 0, :], in0=ssv[:, 0, :], scalar1=inv_n, scalar2=EPS,
                                                    op0=ALU.mult, op1=ALU.add), reads=[ssb], writes=[ssb])
            S.act(lambda e: nc.scalar.activation(out=ssv[:, 1, :], in_=ssv[:, 0, :], func=AF.Ln), reads=[ssb], writes=[ssb])
        else:
            S.act(lambda e: nc.scalar.activation(out=ssv[:, 1, :], in_=ssv[:, 0, :], func=AF.Ln, scale=inv_n),
                  reads=[ssb], writes=[ssb])
        S.act(lambda e: nc.scalar.activation(out=ssv[:, 2, :], in_=ssv[:, 1, :], func=AF.Exp, scale=-0.5),
              reads=[ssb], writes=[ssb])

    def rope(xv, xb, H, half, table, i, tmp, tmpb, P=128, prow=None):
        if prow is None:
            cs = table[0:P, i, :]
        else:
            cs = prow
        cos = cs[:, 0:half].unsqueeze(1).to_broadcast([P, H, half])
        sin = cs[:, half:2 * half].unsqueeze(1).to_broadcast([P, H, half])
        x1 = xv[:, :, 0:half]
        x2 = xv[:, :, half:2 * half]

        def f1(e):
            nc.gpsimd.tensor_tensor(out=tmp[:, 0], in0=x1, in1=cos, op=ALU.mult)
            nc.gpsimd.tensor_tensor(out=tmp[:, 1], in0=x2, in1=sin, op=ALU.mult)
            nc.gpsimd.tensor_tensor(out=tmp[:, 2], in0=x2, in1=cos, op=ALU.mult)
            return nc.gpsimd.tensor_tensor(out=tmp[:, 3], in0=x1, in1=sin, op=ALU.mult)
        S.pool(f1, reads=[xb, constb], writes=[tmpb])

        def f2(e):
            nc.gpsimd.tensor_tensor(out=x1, in0=tmp[:, 0], in1=tmp[:, 1], op=ALU.subtract)
            return nc.gpsimd.tensor_tensor(out=x2, in0=tmp[:, 2], in1=tmp[:, 3], op=ALU.add)
        S.pool(f2, reads=[tmpb], writes=[xb])

    def norm_transpose(xt, xtb, i, ws):
        junk, jb = ws["junk"].next()
        ss, sb = ws["ss"].next()
        nb, nbb = ws["nb"].next()
        S.act(lambda e: nc.scalar.activation(out=junk, in_=xt, func=AF.Square, accum_out=ss[:, 0, 0:1]),
              reads=[xtb], writes=[jb, sb])
        yield
        S.dve(lambda e: nc.vector.tensor_scalar(out=ss[:, 0, :], in0=ss[:, 0, :], scalar1=1.0 / DM, scalar2=EPS,
                                                op0=ALU.mult, op1=ALU.add), reads=[sb], writes=[sb])
        yield
        S.act(lambda e: nc.scalar.activation(out=ss[:, 1, :], in_=ss[:, 0, :], func=AF.Ln), reads=[sb], writes=[sb])
        yield
        S.act(lambda e: nc.scalar.activation(out=ss[:, 2, :], in_=ss[:, 1, :], func=AF.Exp, scale=-0.5), reads=[sb], writes=[sb])
        yield
        S.dve(lambda e: nc.vector.tensor_scalar(out=nb, in0=xt, scalar1=ss[:, 2, 0:1], scalar2=None, op0=ALU.mult),
              reads=[xtb, sb], writes=[nbb])
        yield
        pb = ws["psT"]
        pv = psh(pb)
        transposes([(pv[:, kc * 128:(kc + 1) * 128], nb[:, kc * 128:(kc + 1) * 128]) for kc in range(8)], ident,
                   [nbb, constb], [PSB[pb]])
        yield
        S.act(lambda e: nc.scalar.copy(out=nT[:, :, i * 128:(i + 1) * 128],
                                       in_=pv[:, 0:1024].rearrange("p (k t) -> p k t", k=8)),
              reads=[PSB[pb]], writes=[nTb[i]])
        yield

    def mk_norm_ws(psT):
        return {"junk": Rot(A, 1, [128, 1024], BF16), "ss": Rot(A, 2, [128, 3, 1], F32),
                "nb": Rot(A, 1, [128, 1024], BF16), "psT": psT}

    def phase_norm(s, src):
        m = A.mark()
        NS = 4
        wsl = [mk_norm_ws(4 + k) for k in range(NS)]
        xin = [(A.alloc([128, 1024], F32), Buf(), S.channel(f"xin{k}")) for k in range(NS)]

        def one(k, i):
            xt, xb, ch = xin[k]
            S.dma(lambda e: nc.sync.dma_start(out=xt, in_=src[s, i * 128:(i + 1) * 128, :]), ch,
                  reads=[hb[s][i]], writes=[xb])
            yield
            yield from norm_transpose(xt, xb, i, wsl[k])

        def stream(k):
            for i in range(k, NT, NS):
                yield from one(k, i)
        run_streams([stream(k) for k in range(NS)])
        S.flush()
        A.release(m)

    def qk_post(ps_i, i, ws, gidx, dstT, dst_b, scale, do_rope):
        sq, sqb = ws["sq"].next()
        ss, ssb = ws["ss4"].next()
        qn, qnb = ws["qn"].next()
        qh, qhb = ws["qh"].next()
        pv = psf(ps_i)
        S.act(lambda e: nc.scalar.activation(out=sq, in_=pv, func=AF.Square), reads=[PSB[ps_i]], writes=[sqb])
        yield
        S.dve(lambda e: nc.vector.tensor_reduce(out=ss[:, 0, :], in_=sq.rearrange("p (h d) -> p h d", h=4), axis=AX.X,
                                                op=ALU.add), reads=[sqb], writes=[ssb])
        yield
        S.dve(lambda e: nc.vector.tensor_scalar(out=ss[:, 0, :], in0=ss[:, 0, :], scalar1=1.0 / 128, scalar2=EPS,
                                                op0=ALU.mult, op1=ALU.add), reads=[ssb], writes=[ssb])
        yield
        S.act(lambda e: nc.scalar.activation(out=ss[:, 1, :], in_=ss[:, 0, :], func=AF.Ln), reads=[ssb], writes=[ssb])
        yield
        S.act(lambda e: nc.scalar.activation(out=ss[:, 2, :], in_=ss[:, 1, :], func=AF.Exp, scale=-0.5),
              reads=[ssb], writes=[ssb])
        yield

        def f(e):
            inst = None
            for h in range(4):
                inst = nc.vector.scalar_tensor_tensor(out=qn[:, h * 128:(h + 1) * 128], in0=pv[:, h * 128:(h + 1) * 128],
                                                      scalar=ss[:, 2, h:h + 1], in1=gains[:, gidx, :], op0=ALU.mult,
                                                      op1=ALU.mult)
            return inst
        S.dve(f, reads=[PSB[ps_i], ssb, constb], writes=[qnb])
        yield
        if do_rope:
            tmp, tmpb = ws["rtmp"].next()
            rope(qn.rearrange("p (h d) -> p h d", h=4), qnb, 4, 16, ropeA, i, tmp, tmpb)
            yield
        S.act(lambda e: nc.scalar.activation(out=qh, in_=qn, func=AF.Copy, scale=scale), reads=[qnb], writes=[qhb])
        yield
        pt = ws["psT"]
        ptv = psh(pt)
        transposes([(ptv[:, h * 128:(h + 1) * 128], qh[:, h * 128:(h + 1) * 128]) for h in range(4)], ident,
                   [qhb, constb], [PSB[pt]])
        yield
        S.act(lambda e: nc.scalar.copy(out=dstT[:, :, i * 128:(i + 1) * 128],
                                       in_=ptv[:, 0:512].rearrange("p (h t) -> p h t", h=4)),
              reads=[PSB[pt]], writes=[dst_b])
        yield

    def proj_streamed(wb, wbb, ncols, handler):
        def stream(k):
            for i in range(k, NT, 2):
                proj_tok(wb, ncols, i, k, wbb)
                yield
                yield from handler(k, i, k)
        run_streams([stream(0), stream(1)])

    def proj_tok(wb, ncols, i, ps_i, wbb, M=128, t0=None):
        t0 = i * 128 if t0 is None else t0
        mm_group(psf(ps_i)[0:M, 0:ncols], [(nT[:, kc, t0:t0 + M], wb[:, kc, 0:ncols]) for kc in range(8)],
                 [nTb[t0 // 128], wbb], [PSB[ps_i]])

    def mk_qk_ws(psT):
        return {"sq": Rot(A, 1, [128, 512], F32), "ss4": Rot(A, 2, [128, 3, 4], F32), "qn": Rot(A, 1, [128, 512], F32),
                "qh": Rot(A, 1, [128, 512], BF16), "rtmp": Rot(A, 1, [128, 4, 4, 16], F32), "psT": psT}

    def out_proj_and_norm(s, wout_dram, src, dst):
        m = A.mark()
        stage_state["pool"] = mk_stage()
        wo = A.alloc([128, 8, DM], BF16)
        wob = Buf()
        for c0 in range(0, DM, 256):
            load_w(wout_dram[:, c0:c0 + 256], 8, 256, wo[:, :, c0:c0 + 256], wob)
        wsl = [mk_norm_ws(6), mk_norm_ws(7)]
        hin = [(A.alloc([128, 1024], F32), Buf(), f"xin{k}") for k in range(2)]
        hnew = [(A.alloc([128, 1024], F32), Buf(), f"st{k}") for k in range(2)]

        def one(k, i):
            hi_, hib, ch = hin[k]
            hn, hnb, sch = hnew[k]
            S.dma(lambda e: nc.sync.dma_start(out=hi_, in_=src[s, i * 128:(i + 1) * 128, :]), ch,
                  reads=[hb[s][i]], writes=[hib])
            yield
            for half in range(2):
                pi = 2 * k + half
                mm_group(psf(pi), [(oT[:, c, i * 128:(i + 1) * 128], wo[:, c, half * 512:(half + 1) * 512])
                                   for c in range(8)], [oTb[i], wob], [PSB[pi]])
                yield
                S.dve(lambda e, pi=pi, half=half: nc.vector.tensor_tensor(
                    out=hn[:, half * 512:(half + 1) * 512], in0=psf(pi), in1=hi_[:, half * 512:(half + 1) * 512],
                    op=ALU.add), reads=[PSB[pi], hib], writes=[hnb])
                yield
            S.dma(lambda e: nc.sync.dma_start(out=dst[s, i * 128:(i + 1) * 128, :], in_=hn), sch,
                  reads=[hnb], writes=[hb[s][i]])
            yield
            yield from norm_transpose(hn, hnb, i, wsl[k])

        def stream(k):
            for i in range(k, NT, 2):
                yield from one(k, i)
        run_streams([stream(0), stream(1)])
        S.flush()
        A.release(m)

    def ffn(s, layer, final):
        m = A.mark()
        stage_state["pool"] = mk_stage()
        hres = A.alloc([128, NT, DM], F32)
        hresb = [Buf() for _ in range(NT)]
        actT = A.alloc([128, 4, SEQ], BF16)
        actb = [Buf() for _ in range(4)]
        AO = Arena(nc, 32768, base=oT_raw)
        wub = [(AO.alloc([128, 8, 512], BF16), Buf()) for _ in range(2)]
        wdb = [(AO.alloc([128, 4, DM], BF16), Buf()) for _ in range(2)]
        rr = Rot(A, 2, [128, 512], F32)
        for i in range(NT):
            S.dma(lambda e, i=i: nc.sync.dma_start(out=hres[:, i, :], in_=h_scr[s, i * 128:(i + 1) * 128, :]), "hres",
                  reads=[hb[s][i]], writes=[hresb[i]])
        gc = gcol[:, 2 + layer, :]
        for g in range(8):
            wu, wubb = wub[g % 2]
            wd, wdbb = wdb[g % 2]
            load_w(w_up[layer, :, g * 512:(g + 1) * 512], 8, 512, wu, wubb, gc)
            load_w(w_down[layer, g * 512:(g + 1) * 512, :], 4, DM, wd, wdbb)
            for fc in range(4):
                for tb in range(4):
                    pi = (fc * 4 + tb) % 4
                    mm_group(psf(pi), [(wu[:, kc, fc * 128:(fc + 1) * 128], nT[:, kc, tb * 512:(tb + 1) * 512])
                                       for kc in range(8)], nTb[4 * tb:4 * tb + 4] + [wubb], [PSB[pi]])
                    r, rb = rr.next()
                    S.act(lambda e, r=r, pi=pi: nc.scalar.activation(out=r, in_=psf(pi), func=AF.Relu),
                          reads=[PSB[pi]], writes=[rb])
                    S.pool(lambda e, r=r, fc=fc, tb=tb: nc.gpsimd.tensor_tensor(
                        out=actT[:, fc, tb * 512:(tb + 1) * 512], in0=r, in1=r, op=ALU.mult),
                        reads=[rb], writes=[actb[tb]])
            for ti in range(NT):
                for half in range(2):
                    pi = 4 + (ti * 2 + half) % 4
                    mm_group(psf(pi), [(actT[:, fc, ti * 128:(ti + 1) * 128], wd[:, fc, half * 512:(half + 1) * 512])
                                       for fc in range(4)], [actb[ti // 4], wdbb], [PSB[pi]])
                    S.dve(lambda e, pi=pi, ti=ti, half=half: nc.vector.tensor_tensor(
                        out=hres[:, ti, half * 512:(half + 1) * 512], in0=psf(pi),
                        in1=hres[:, ti, half * 512:(half + 1) * 512], op=ALU.add),
                        reads=[PSB[pi], hresb[ti]], writes=[hresb[ti]])
        dst = out if final else h_scr
        for i in range(NT):
            S.dma(lambda e, i=i: nc.sync.dma_start(out=dst[s, i * 128:(i + 1) * 128, :], in_=hres[:, i, :]),
                  f"st{i % 2}", reads=[hresb[i]], writes=[hb[s][i]])
        S.flush()
        A.release(m)

    def dsa(s):
        m = A.mark()
        kaT = A.alloc([128, 4, SEQ], BF16)
        qaT = A.alloc([128, 4, SEQ], BF16)
        va = A.alloc([128, NT, 4, 132], BF16)
        iqT = A.alloc([128, 4, SEQ], BF16)
        ikT = A.alloc([128, SEQ], BF16)
        iws = A.alloc([128, NT, 8], F32)
        kaTb, qaTb, vab, iqTb, ikTb, iwsb = Buf(), Buf(), Buf(), Buf(), Buf(), Buf()
        m2 = A.mark()
        AO = Arena(nc, 32768, base=oT_raw)
        stage_state["pool"] = mk_stage(AO)
        wbp = [(AO.alloc([128, 8, 512], BF16), Buf()) for _ in range(2)]
        wss = [mk_qk_ws(6), mk_qk_ws(7)]
        cp = [Rot(A, 1, [128, 512], F32) for _ in range(2)]
        cpb = [Rot(A, 1, [128, 512], BF16) for _ in range(2)]
        itmp = [Rot(A, 1, [128, 4, 8, 8], F32) for _ in range(2)]
        ikf = [Rot(A, 1, [128, 72], F32) for _ in range(2)]
        ikd = [Rot(A, 1, [128, 128], BF16) for _ in range(2)]
        gc = gcol[:, 0, :]
        S.pool(lambda e: nc.gpsimd.memset(va[:, :, :, 128:129], 1.0), writes=[vab])

        def h_va(pi, i, k):
            S.act(lambda e: nc.scalar.copy(out=va[:, i, :, 0:128], in_=psf(pi).rearrange("p (h d) -> p h d", h=4)),
                  reads=[PSB[pi]], writes=[vab])
            yield

        def h_iq(pi, i, k):
            c, cb = cp[k].next()
            S.act(lambda e: nc.scalar.copy(out=c, in_=psf(pi)), reads=[PSB[pi]], writes=[cb])
            yield
            tmp, tmpb = itmp[k].next()
            rope(c.rearrange("p (h d) -> p h d", h=8), cb, 8, 8, ropeI, i, tmp, tmpb)
            yield
            ch_, chb = cpb[k].next()
            S.act(lambda e: nc.scalar.copy(out=ch_, in_=c), reads=[cb], writes=[chb])
            yield
            ptv = psh(6 + k)
            transposes([(ptv[:, p * 128:(p + 1) * 128], ch_[:, p * 128:(p + 1) * 128]) for p in range(4)], ident,
                       [chb, constb], [PSB[6 + k]])
            yield
            S.act(lambda e: nc.scalar.copy(out=iqT[:, :, i * 128:(i + 1) * 128],
                                           in_=ptv[:, 0:512].rearrange("p (h t) -> p h t", h=4)),
                  reads=[PSB[6 + k]], writes=[iqTb])
            yield

        def h_ik(pi, i, k):
            f_, fb = ikf[k].next()
            S.act(lambda e: nc.scalar.copy(out=f_, in_=psf(pi)[:, 0:72]), reads=[PSB[pi]], writes=[fb])
            yield
            tmp, tmpb = itmp[k].next()
            rope(f_[:, 0:64].rearrange("p (h d) -> p h d", h=1), fb, 1, 8, ropeI, i, tmp[:, :, 0:1, :], tmpb)
            yield
            d_, db = ikd[k].next()

            def fcp(e):
                nc.vector.tensor_copy(out=d_[:, 0:64], in_=f_[:, 0:64])
                nc.vector.tensor_copy(out=d_[:, 64:128], in_=f_[:, 0:64])
                return nc.vector.tensor_scalar(out=iws[:, i, :], in0=f_[:, 64:72], scalar1=1.0 / (8.0 * 8.0 ** 0.5),
                                               scalar2=None, op0=ALU.mult)
            S.dve(fcp, reads=[fb], writes=[db, iwsb])
            yield
            ptv = psh(6 + k)
            transposes([(ptv[:, 0:128], d_)], ident, [db, constb], [PSB[6 + k]])
            yield
            S.act(lambda e: nc.scalar.copy(out=ikT[:, i * 128:(i + 1) * 128], in_=ptv[:, 0:128]),
                  reads=[PSB[6 + k]], writes=[ikTb])
            yield

        blocks = [("qa", 0), ("ka", 512), ("va", 1024), ("iq", 1536), ("ik", 2048)]
        for bi, (nm, c0) in enumerate(blocks):
            ncols = 72 if nm == "ik" else 512
            wb, wbb = wbp[bi % 2]
            load_w(w_in_ab[:, c0:c0 + ncols], 8, ncols, wb[:, :, 0:ncols], wbb, gc)
            if nm == "qa":
                proj_streamed(wb, wbb, 512, lambda pi, i, k: qk_post(pi, i, wss[k], 0, qaT, qaTb, 128 ** -0.5, True))
            elif nm == "ka":
                proj_streamed(wb, wbb, 512, lambda pi, i, k: qk_post(pi, i, wss[k], 1, kaT, kaTb, 1.0, True))
            elif nm == "va":
                proj_streamed(wb, wbb, 512, h_va)
            elif nm == "iq":
                proj_streamed(wb, wbb, 512, h_iq)
            else:
                proj_streamed(wb, wbb, 72, h_ik)
        S.flush()
        A.release(m2)
        AN = Arena(nc, 32768, base=nT_raw)
        junk = AN.alloc([128, SEQ], BF16)
        junkb = Buf()

        def dsa_stream(k):
            sc = AN.alloc([128, SEQ], F32)
            scb = Buf()
            mk = AN.alloc([128, SEQ], BF16)
            mkb = Buf()
            mT = A.alloc([128, NT, 128], BF16)
            mTb = Buf()
            tmpf = Rot(A, 2, [128, 512], F32)
            ptr = Rot(A, 2, [128, 512], BF16)
            ptmr = Rot(A, 2, [128, 512], BF16)
            bsr = Rot(A, 2, [128, 8], F32)
            rzr = Rot(A, 2, [128, 4], F32)
            oar = Rot(A, 1, [128, 512], BF16)
            pidx, pst, ppv = k, 2 + k, 4 + k
            yield
            for j in range(k, NT, 2):
                W = (j + 1) * 128
                for h in range(8):
                    p0 = (h % 2) * 64
                    pair = h // 2
                    for kb in range((W + 511) // 512):
                        c0 = kb * 512
                        cw = min(512, W - c0)
                        mm_group(psf(pidx)[:, 0:cw], [(iqT[p0:p0 + 64, pair, j * 128:(j + 1) * 128], ikT[p0:p0 + 64, c0:c0 + cw])],
                                 [iqTb, ikTb], [PSB[pidx]])
                        yield
                        t_, tb_ = tmpf.next()
                        S.act(lambda e, t_=t_, cw=cw: nc.scalar.activation(out=t_[:, 0:cw], in_=psf(pidx)[:, 0:cw], func=AF.Relu),
                              reads=[PSB[pidx]], writes=[tb_])
                        yield
                        if h == 0:
                            S.dve(lambda e, t_=t_, c0=c0, cw=cw, j=j: nc.vector.tensor_scalar(out=sc[:, c0:c0 + cw], in0=t_[:, 0:cw], scalar1=iws[:, j, 0:1],
                                                                    scalar2=None, op0=ALU.mult), reads=[tb_, iwsb], writes=[scb])
                        else:
                            S.dve(lambda e, t_=t_, c0=c0, cw=cw, j=j, h=h: nc.vector.scalar_tensor_tensor(out=sc[:, c0:c0 + cw], in0=t_[:, 0:cw],
                                                                           scalar=iws[:, j, h:h + 1], in1=sc[:, c0:c0 + cw],
                                                                           op0=ALU.mult, op1=ALU.add),
                                  reads=[tb_, iwsb, scb], writes=[scb])
                        yield
                S.dve(lambda e, W=W: nc.vector.memset(sc[0:64, W - 64:W], NEG), reads=[scb], writes=[scb])
                yield
                if j >= 2:
                    bs, bsb = bsr.next()

                    def f0(e, bs=bs, W=W):
                        nc.vector.tensor_reduce(out=bs[:, 0:1], in_=sc[:, 0:W - 64], axis=AX.X, op=ALU.min)
                        return nc.vector.tensor_reduce(out=bs[:, 1:2], in_=sc[:, 0:W], axis=AX.X, op=ALU.max)
                    S.dve(f0, reads=[scb], writes=[bsb])
                    yield
                    S.dve(lambda e, bs=bs: nc.vector.tensor_tensor(out=bs[:, 1:2], in0=bs[:, 1:2], in1=bs[:, 0:1], op=ALU.subtract),
                          reads=[bsb], writes=[bsb])
                    yield
                    for it in range(1, BIS_ITERS + 1):
                        f = 2.0 ** (-it)
                        S.dve(lambda e, bs=bs, f=f: nc.vector.tensor_scalar(out=bs[:, 2:3], in0=bs[:, 1:2], scalar1=f, scalar2=bs[:, 0:1],
                                                                op0=ALU.mult, op1=ALU.add), reads=[bsb], writes=[bsb])
                        yield
                        S.dve(lambda e, bs=bs, W=W: nc.vector.tensor_scalar(out=junk[:, 0:W], in0=sc[:, 0:W], scalar1=bs[:, 2:3], scalar2=None,
                                                                op0=ALU.is_ge, op1=ALU.add, accum_out=bs[:, 3:4]),
                              reads=[bsb, scb], writes=[bsb, junkb])
                        yield
                        S.dve(lambda e, bs=bs: nc.vector.tensor_scalar(out=bs[:, 4:5], in0=bs[:, 3:4], scalar1=255.5, scalar2=bs[:, 1:2],
                                                                op0=ALU.is_ge, op1=ALU.mult), reads=[bsb], writes=[bsb])
                        yield
                        S.dve(lambda e, bs=bs, f=f: nc.vector.scalar_tensor_tensor(out=bs[:, 0:1], in0=bs[:, 4:5], scalar=f, in1=bs[:, 0:1],
                                                                       op0=ALU.mult, op1=ALU.add), reads=[bsb], writes=[bsb])
                        yield
                    thr, thrb = bs[:, 0:1], bsb
                else:
                    thr, thrb = thr_all[:, 0:1], constb
                S.dve(lambda e, W=W, thr=thr: nc.vector.tensor_scalar(out=mk[:, 0:W], in0=sc[:, 0:W], scalar1=thr, scalar2=None, op0=ALU.is_ge),
                      reads=[scb, thrb], writes=[mkb])
                yield
                for g in range((j + 4) // 4):
                    kts = list(range(4 * g, min(4 * g + 4, j + 1)))
                    n = len(kts)
                    pv = psh(pidx)
                    transposes([(pv[:, q * 128:(q + 1) * 128], mk[:, kt * 128:(kt + 1) * 128]) for q, kt in enumerate(kts)],
                               ident, [mkb, constb], [PSB[pidx]])
                    yield
                    S.act(lambda e, g=g, n=n, pv=pv: nc.scalar.copy(out=mT[:, 4 * g:4 * g + n, :],
                                                   in_=pv[:, 0:n * 128].rearrange("p (k t) -> p k t", k=n)),
                          reads=[PSB[pidx]], writes=[mTb])
                    yield
                oa, oab = oar.next()
                for h in range(4):
                    oc = (h % 2) * 256
                    ov = psf(ppv)[:, oc:oc + 129]
                    for g in range((j + 4) // 4):
                        kts = list(range(4 * g, min(4 * g + 4, j + 1)))
                        n = len(kts)

                        def fs(e, kts=kts, h=h, j=j):
                            inst = None
                            for q, kt in enumerate(kts):
                                inst = nc.tensor.matmul(psf(pst)[:, q * 128:(q + 1) * 128], lhsT=kaT[:, h, kt * 128:(kt + 1) * 128],
                                                        rhs=qaT[:, h, j * 128:(j + 1) * 128], start=True, stop=True)
                            return inst
                        S.pe(fs, reads=[kaTb, qaTb], writes=[PSB[pst]])
                        yield
                        pt, ptb = ptr.next()
                        S.act(lambda e, pt=pt, n=n: nc.scalar.activation(out=pt[:, 0:n * 128], in_=psf(pst)[:, 0:n * 128], func=AF.Exp),
                              reads=[PSB[pst]], writes=[ptb])
                        yield
                        pm, pmb = ptmr.next()
                        S.pool(lambda e, pm=pm, pt=pt, g=g, n=n: nc.gpsimd.tensor_tensor(out=pm[:, 0:n * 128], in0=pt[:, 0:n * 128],
                                                                in1=mT[:, 4 * g:4 * g + n, :].rearrange("p k t -> p (k t)"),
                                                                op=ALU.mult), reads=[ptb, mTb], writes=[pmb])
                        yield

                        def fo(e, kts=kts, pm=pm, ov=ov, h=h, j=j):
                            inst = None
                            for q, kt in enumerate(kts):
                                inst = nc.tensor.matmul(ov, lhsT=pm[:, q * 128:(q + 1) * 128], rhs=va[:, kt, h, 0:129],
                                                        start=(kt == 0), stop=(kt == j))
                            return inst
                        S.pe(fo, reads=[pmb, vab], writes=[PSB[ppv]])
                        yield
                    rz, rzb = rzr.next()
                    S.dve(lambda e, rz=rz, oc=oc: nc.vector.reciprocal(out=rz[:, 0:1], in_=psf(ppv)[:, oc + 128:oc + 129]),
                          reads=[PSB[ppv]], writes=[rzb])
                    yield
                    S.act(lambda e, oa=oa, h=h, oc=oc, rz=rz: nc.scalar.activation(out=oa[:, h * 128:(h + 1) * 128], in_=psf(ppv)[:, oc:oc + 128],
                                                         func=AF.Identity, scale=rz[:, 0:1]),
                          reads=[PSB[ppv], rzb], writes=[oab])
                    yield
                ptv = psh(pst)
                transposes([(ptv[:, h * 128:(h + 1) * 128], oa[:, h * 128:(h + 1) * 128]) for h in range(4)], ident,
                           [oab, constb], [PSB[pst]])
                yield
                S.act(lambda e, j=j, ptv=ptv: nc.scalar.copy(out=oT[:, 0:4, j * 128:(j + 1) * 128],
                                               in_=ptv[:, 0:512].rearrange("p (h t) -> p h t", h=4)),
                      reads=[PSB[pst]], writes=[oTb[j]])
                yield

        run_streams([dsa_stream(0), dsa_stream(1)])
        S.flush()
        A.release(m)

    def gla(s):
        m = A.mark()
        stage_state["pool"] = mk_stage()
        wq = A.alloc([128, 8, 256], BF16)
        wk = A.alloc([128, 8, 256], BF16)
        wv = A.alloc([128, 8, 512], BF16)
        wl = A.alloc([128, 8, 16], BF16)
        wo_ = A.alloc([128, 8, 512], BF16)
        wb_ = Buf()
        gc = gcol[:, 0, :]
        load_w(w_in_ab[:, 2120:2376], 8, 256, wq, wb_, gc)
        load_w(w_in_ab[:, 2376:2632], 8, 256, wk, wb_, gc)
        load_w(w_in_ab[:, 2632:3144], 8, 512, wv, wb_, gc)
        load_w(w_in_ab[:, 3144:3160], 8, 16, wl, wb_, gc)
        load_w(w_in_ab[:, 3160:3672], 8, 512, wo_, wb_, gc)
        glr = [(A.alloc([32, 64], F32), Buf()) for _ in range(2)]
        for gl_, glb in glr:
            S.dve(lambda e, gl_=gl_: nc.vector.memset(gl_, 1.0), writes=[glb])
        spr = Rot(A, 2, [64, 256], F32)
        ebr = Rot(A, 2, [128, 2, 2, 64], F32)
        ebvr = Rot(A, 2, [64, 256], F32)
        qer = Rot(A, 2, [128, 2, 2, 64], BF16)
        for q_, qb_ in qer.items:
            S.dve(lambda e, q_=q_: nc.vector.memset(q_, 0.0), writes=[qb_])
        kdr = Rot(A, 2, [128, 2, 64], BF16)
        klr = Rot(A, 2, [64, 256], BF16)
        vcr = Rot(A, 2, [64, 512], BF16)
        sgr = Rot(A, 2, [64, 512], F32)
        gvr = Rot(A, 2, [64, 512], F32)
        atr = Rot(A, 2, [64, 4, 64], BF16)
        sqr = Rot(A, 2, [64, 512], F32)
        ssr = Rot(A, 2, [64, 3, 4], F32)
        onr = Rot(A, 2, [64, 512], F32)
        obr = Rot(A, 2, [64, 512], BF16)
        Sf = A.alloc([128, 2, 128], F32)
        Sfb = [Buf(), Buf()]
        Sbf = [(A.alloc([128, 2, 128], BF16), [Buf(), Buf()]) for _ in range(2)]
        def gla_prep(c, F):
            t0 = c * 64
            ntb = nTb[t0 // 128]
            gl_, glb = glr[c % 2]
            mm_group(psf(0)[0:16, 0:64], [(wl[:, kc, 0:16], nT[:, kc, t0:t0 + 64]) for kc in range(8)], [ntb, wb_], [PSB[0]])
            yield
            S.act(lambda e, gl_=gl_: nc.scalar.copy(out=gl_[0:16, :], in_=psf(0)[0:16, 0:64]), reads=[PSB[0]], writes=[glb])
            yield
            mm_group(psf(0)[0:64, 256:512], [(gl_[0:17, 0:64], wg[0:17, :])], [glb, constb], [PSB[0]])
            yield
            sp, spb = spr.next()
            S.act(lambda e, sp=sp: nc.scalar.activation(out=sp, in_=psf(0)[0:64, 256:512], func=AF.Exp, scale=-1.0),
                  reads=[PSB[0]], writes=[spb])
            yield
            S.act(lambda e, sp=sp: nc.scalar.activation(out=sp, in_=sp, func=AF.Ln, bias=1.0), reads=[spb], writes=[spb])
            yield
            def fb(e, sp=sp):
                nc.tensor.matmul(psf(1)[:, 0:64], lhsT=sp[:, 0:128], rhs=tri_incl, start=True, stop=True)
                nc.tensor.matmul(psf(1)[:, 64:128], lhsT=sp[:, 128:256], rhs=tri_incl, start=True, stop=True)
                return nc.tensor.matmul(psf(1)[0:64, 128:384], lhsT=tri_rev, rhs=sp, start=True, stop=True)
            S.pe(fb, reads=[spb, constb], writes=[PSB[1]])
            yield
            eb, ebb = ebr.next()
            ebv, ebvb = ebvr.next()

            def fe(e, eb=eb, ebv=ebv):
                bv = psf(1)[:, 0:128].rearrange("p (g t) -> p g t", g=2)
                nc.scalar.activation(out=eb[:, 0], in_=bv, func=AF.Exp)
                nc.scalar.activation(out=eb[:, 1], in_=bv, func=AF.Exp, scale=-1.0)
                return nc.scalar.activation(out=ebv, in_=psf(1)[0:64, 128:384], func=AF.Exp)
            S.act(fe, reads=[PSB[1]], writes=[ebb, ebvb])
            yield
            def fqk(e, t0=t0):
                inst = None
                for g in range(2):
                    for kc in range(8):
                        nc.tensor.matmul(psf(2)[:, g * 64:(g + 1) * 64], lhsT=wq[:, kc, g * 128:(g + 1) * 128],
                                         rhs=nT[:, kc, t0:t0 + 64], start=(kc == 0), stop=(kc == 7))
                for g in range(2):
                    for kc in range(8):
                        nc.tensor.matmul(psf(2)[:, 128 + g * 64:128 + (g + 1) * 64], lhsT=wk[:, kc, g * 128:(g + 1) * 128],
                                         rhs=nT[:, kc, t0:t0 + 64], start=(kc == 0), stop=(kc == 7))
                for kc in range(8):
                    inst = nc.tensor.matmul(psf(2)[0:64, 256:512], lhsT=nT[:, kc, t0:t0 + 64], rhs=wk[:, kc, :],
                                            start=(kc == 0), stop=(kc == 7))
                return inst
            S.pe(fqk, reads=[ntb, wb_], writes=[PSB[2]])
            yield
            qe, qeb = qer.next()
            kd, kdb = kdr.next()
            kl, klb = klr.next()

            def fq(e, qe=qe, kd=kd, kl=kl, eb=eb, ebv=ebv):
                for hh in range(2):
                    p0 = hh * 64
                    nc.vector.scalar_tensor_tensor(out=qe[p0:p0 + 64, :, hh, :],
                                                   in0=psf(2)[p0:p0 + 64, 0:128].rearrange("p (g t) -> p g t", g=2),
                                                   scalar=0.125, in1=eb[p0:p0 + 64, 0], op0=ALU.mult, op1=ALU.mult)
                nc.vector.tensor_tensor(out=kd, in0=psf(2)[:, 128:256].rearrange("p (g t) -> p g t", g=2), in1=eb[:, 1],
                                        op=ALU.mult)
                return nc.vector.tensor_tensor(out=kl, in0=psf(2)[0:64, 256:512], in1=ebv, op=ALU.mult)
            S.dve(fq, reads=[PSB[2], ebb, ebvb], writes=[qeb, kdb, klb])
            yield
            mm_group(psf(3)[0:64, :], [(nT[:, kc, t0:t0 + 64], wv[:, kc, :]) for kc in range(8)], [ntb, wb_], [PSB[3]])
            yield
            vc, vcb = vcr.next()
            S.act(lambda e, vc=vc: nc.scalar.copy(out=vc, in_=psf(3)[0:64, :]), reads=[PSB[3]], writes=[vcb])
            yield
            mm_group(psf(4)[0:64, :], [(nT[:, kc, t0:t0 + 64], wo_[:, kc, :]) for kc in range(8)], [ntb, wb_], [PSB[4]])
            yield
            sg, sgb = sgr.next()

            def fsg(e, sg=sg):
                nc.scalar.activation(out=sg, in_=psf(4)[0:64, :], func=AF.Exp, scale=-1.0)
                nc.scalar.activation(out=sg, in_=sg, func=AF.Ln, bias=1.0)
                return nc.scalar.activation(out=sg, in_=sg, func=AF.Exp, scale=-1.0)
            S.act(fsg, reads=[PSB[4]], writes=[sgb])
            yield
            gv, gvb = gvr.next()
            S.dve(lambda e, gv=gv, sg=sg: nc.vector.tensor_tensor(out=gv, in0=psf(4)[0:64, :], in1=sg, op=ALU.mult),
                  reads=[PSB[4], sgb], writes=[gvb])
            yield
            F.update(dict(qe=qe, qeb=qeb, kd=kd, kdb=kdb, kl=kl, klb=klb, vc=vc, vcb=vcb, gv=gv, gvb=gvb, eb=eb, ebb=ebb))
            yield

        def gla_scan(c, F):
            t0 = c * 64
            qe, qeb, kd, kdb, kl, klb = F['qe'], F['qeb'], F['kd'], F['kdb'], F['kl'], F['klb']
            vc, vcb, gv, gvb, eb, ebb = F['vc'], F['vcb'], F['gv'], F['gvb'], F['eb'], F['ebb']
            def fat(e, kd=kd, qe=qe):
                inst = None
                for g in range(2):
                    for hh in range(2):
                        p0 = hh * 64
                        q = g * 2 + hh
                        inst = nc.tensor.matmul(psf(5)[0:64, q * 64:(q + 1) * 64], lhsT=kd[:, g, :],
                                                rhs=qe[:, g, hh, :], start=True, stop=True)
                return inst
            S.pe(fat, reads=[kdb, qeb], writes=[PSB[5]])
            yield
            at, atb = atr.next()
            S.dve(lambda e, at=at: nc.vector.tensor_tensor(
                out=at, in0=psf(5)[0:64, 0:256].rearrange("p (q t) -> p q t", q=4),
                in1=causT.unsqueeze(1).to_broadcast([64, 4, 64]), op=ALU.mult), reads=[PSB[5], constb], writes=[atb])
            yield
            sbf_prev, sbfb_prev = Sbf[(c + 1) % 2]
            sbf_cur, sbfb_cur = Sbf[c % 2]

            def fo(e, qe=qe, at=at, vc=vc, c=c, sbf_prev=sbf_prev):
                inst = None
                for g in range(2):
                    for hh in range(2):
                        p0 = hh * 64
                        q = g * 2 + hh
                        ov = psf(6)[0:64, q * 128:(q + 1) * 128]
                        if c > 0:
                            nc.tensor.matmul(ov, lhsT=qe[:, g, hh, :], rhs=sbf_prev[:, g, :], start=True, stop=False)
                        inst = nc.tensor.matmul(ov, lhsT=at[:, q, :], rhs=vc[:, q * 128:(q + 1) * 128], start=(c == 0), stop=True)
                return inst
            S.pe(fo, reads=[qeb, atb, vcb] + (sbfb_prev if c > 0 else []), writes=[PSB[6]])
            yield
            if c < NCH - 1:
                def fu(e, kl=kl, vc=vc):
                    nc.tensor.matmul(psf(7)[:, 0:256], lhsT=kl[:, 0:128], rhs=vc[:, 0:256], start=True, stop=True)
                    return nc.tensor.matmul(psf(7)[:, 256:512], lhsT=kl[:, 128:256], rhs=vc[:, 256:512], start=True, stop=True)
                S.pe(fu, reads=[klb, vcb], writes=[PSB[7]])
                for g in range(2):
                    def fs_(e, g=g, eb=eb, c=c):
                        inst = None
                        for hh in range(2):
                            p0 = hh * 64
                            uv = psf(7)[p0:p0 + 64, g * 256 + hh * 128:g * 256 + (hh + 1) * 128]
                            if c == 0:
                                inst = nc.vector.tensor_copy(out=Sf[p0:p0 + 64, g, :], in_=uv)
                            else:
                                inst = nc.vector.scalar_tensor_tensor(out=Sf[p0:p0 + 64, g, :], in0=Sf[p0:p0 + 64, g, :],
                                                                      scalar=eb[p0:p0 + 64, 0, g, 63:64], in1=uv,
                                                                      op0=ALU.mult, op1=ALU.add)
                        return inst
                    S.dve(fs_, reads=[PSB[7], ebb, Sfb[g]], writes=[Sfb[g]])
                    S.act(lambda e, g=g, sbf_cur=sbf_cur: nc.scalar.copy(out=sbf_cur[:, g, :], in_=Sf[:, g, :]),
                          reads=[Sfb[g]], writes=[sbfb_cur[g]])
            sq, sqb = sqr.next()
            ss, ssb = ssr.next()
            S.act(lambda e, sq=sq: nc.scalar.activation(out=sq, in_=psf(6)[0:64, :], func=AF.Square), reads=[PSB[6]], writes=[sqb])
            yield
            S.dve(lambda e, sq=sq, ss=ss: nc.vector.tensor_reduce(out=ss[:, 0, :], in_=sq.rearrange("p (h d) -> p h d", h=4),
                                                                  axis=AX.X, op=ALU.add), reads=[sqb], writes=[ssb])
            yield
            rstd_from_ss(ss, ssb, 4, 1.0 / 128, True)
            yield
            on, onb = onr.next()

            def fn_(e, on=on, ss=ss):
                inst = None
                for h in range(4):
                    inst = nc.vector.scalar_tensor_tensor(out=on[:, h * 128:(h + 1) * 128], in0=psf(6)[0:64, h * 128:(h + 1) * 128],
                                                          scalar=ss[:, 2, h:h + 1], in1=gains[0:64, 4, :], op0=ALU.mult,
                                                          op1=ALU.mult)
                return inst
            S.dve(fn_, reads=[PSB[6], ssb, constb], writes=[onb])
            yield
            ob, obb = obr.next()
            S.dve(lambda e, ob=ob, on=on, gv=gv: nc.vector.tensor_tensor(out=ob, in0=on, in1=gv, op=ALU.mult),
                  reads=[onb, gvb], writes=[obb])
            yield
            ptv = psh(5)
            transposes([(ptv[:, 512 + h * 64:512 + (h + 1) * 64], ob[:, h * 128:(h + 1) * 128]) for h in range(4)],
                       ident[0:64, 0:64], [obb, constb], [PSB[5]])
            yield
            S.act(lambda e, t0=t0, ptv=ptv: nc.scalar.copy(out=oT[:, 4:8, t0:t0 + 64],
                                                           in_=ptv[:, 512:768].rearrange("p (h t) -> p h t", h=4)),
                  reads=[PSB[5]], writes=[oTb[t0 // 128]])
            yield

        NCH = SEQ // 64
        Fs = [dict() for _ in range(NCH)]
        run_streams([gla_prep(0, Fs[0])])
        for c in range(NCH):
            gens = [gla_scan(c, Fs[c])]
            if c + 1 < NCH:
                gens.insert(0, gla_prep(c + 1, Fs[c + 1]))
            run_streams(gens)
        S.flush()
        A.release(m)

    def sb_attn(s):
        m = A.mark()
        qT = A.alloc([128, 4, SEQ], BF16)
        kT = A.alloc([128, 4, SEQ], BF16)
        v = A.alloc([128, NT, 512], BF16)
        qTb, kTb, vb_ = Buf(), Buf(), Buf()
        gc = gcol[:, 1, :]
        for hg in range(2):
            mp = A.mark()
            stage_state["pool"] = mk_stage()
            wbp = [(A.alloc([128, 8, 512], BF16), Buf()) for _ in range(2)]
            wss = [mk_qk_ws(6), mk_qk_ws(7)]
            for bi, (nm, c0) in enumerate((("q", hg * 512), ("k", 1024 + hg * 512), ("v", 2048 + hg * 512))):
                wb, wbb = wbp[bi % 2]
                load_w(w_in_c[:, c0:c0 + 512], 8, 512, wb, wbb, gc)
                if nm == "q":
                    proj_streamed(wb, wbb, 512, lambda pi, i, k: qk_post(pi, i, wss[k], 2, qT, qTb, 128 ** -0.5, False))
                elif nm == "k":
                    proj_streamed(wb, wbb, 512, lambda pi, i, k: qk_post(pi, i, wss[k], 3, kT, kTb, 1.0, False))
                else:
                    def hv(pi, i, k):
                        S.act(lambda e: nc.scalar.copy(out=v[:, i, :], in_=psf(pi)), reads=[PSB[pi]], writes=[vb_])
                        yield
                    proj_streamed(wb, wbb, 512, hv)
            S.flush()
            A.release(mp)
            def sb_stream(st_, heads, hg=hg):
                pz, pc, po = 0 + st_, 2 + st_, 4 + st_
                espr = Rot(A, 2, [128, 512], F32)
                lor = Rot(A, 2, [128, 512], BF16)
                ar = Rot(A, 2, [128, 512], BF16)
                lbs = [(A.alloc([128, 512], BF16), Buf()) for _ in range(3)]
                gs = [0]

                def S1(f):
                    h, qb, kt, step, nsteps = f["h"], f["qb"], f["kt"], f["step"], f["nsteps"]
                    cc, ncol, diag = f["cc"], f["ncol"], f["diag"]
                    q0 = qb * 512 + cc
                    mm_group(psf(pz)[:, 0:ncol], [(kT[:, h, kt * 128:(kt + 1) * 128], qT[:, h, q0:q0 + ncol])],
                             [kTb, qTb], [PSB[pz]])
                    yield
                    es, esb = espr.next()
                    f["es"], f["esb"] = es, esb
                    S.act(lambda e: nc.scalar.activation(out=es[:, 0:ncol], in_=psf(pz)[:, 0:ncol], func=AF.Exp, scale=-1.0),
                          reads=[PSB[pz]], writes=[esb])
                    yield
                    S.act(lambda e: nc.scalar.activation(out=es[:, 0:ncol], in_=es[:, 0:ncol], func=AF.Ln, bias=1.0),
                          reads=[esb], writes=[esb])
                    yield
                    lo, lob = lor.next()
                    f["lo"], f["lob"] = lo, lob
                    S.dve(lambda e: nc.vector.scalar_tensor_tensor(out=lo[:, 0:ncol], in0=psf(pz)[:, 0:ncol], scalar=-1.0,
                                                                   in1=es[:, 0:ncol], op0=ALU.mult, op1=ALU.subtract),
                          reads=[PSB[pz], esb], writes=[lob])
                    yield
                    if diag:
                        S.pool(lambda e: nc.gpsimd.tensor_tensor(out=lo[:, 0:128], in0=lo[:, 0:128], in1=strictT, op=ALU.mult),
                               reads=[lob, constb], writes=[lob])
                        yield
                    g = gs[0]
                    gs[0] += 1
                    f["lbc"] = lbs[g % 3]
                    if step < nsteps - 1:
                        lbn, lbnb = lbs[(g + 1) % 3]
                        lbc, lbcb = lbs[g % 3]
                        ccn = f["cc_next"]
                        if ccn < cc:
                            S.pool(lambda e: nc.gpsimd.memset(lbn[:, ccn:cc], 0.0), writes=[lbnb])
                            yield
                        if step == 0:
                            S.dve(lambda e: nc.vector.tensor_copy(out=lbn[:, cc:512], in_=lo[:, 0:ncol]), reads=[lob], writes=[lbnb])
                        else:
                            S.dve(lambda e: nc.vector.tensor_tensor(out=lbn[:, cc:512], in0=lbc[:, cc:512], in1=lo[:, 0:ncol],
                                                                    op=ALU.add), reads=[lob, lbcb], writes=[lbnb])
                        yield

                def S2(f):
                    h, qb, kt, step, nsteps = f["h"], f["qb"], f["kt"], f["step"], f["nsteps"]
                    cc, ncol, diag = f["cc"], f["ncol"], f["diag"]
                    es, esb, lo, lob = f["es"], f["esb"], f["lo"], f["lob"]
                    lbc, lbcb = f["lbc"]
                    if step == 0:
                        mm_group(psf(pc)[:, 0:ncol], [(triU, lo[:, 0:ncol])], [lob, constb], [PSB[pc]])
                    else:
                        mm_group(psf(pc)[:, 0:ncol], [(triU, lo[:, 0:ncol]), (ones128, lbc[:, cc:512])],
                                 [lob, lbcb, constb], [PSB[pc]])
                    yield
                    S.dve(lambda e: nc.vector.tensor_tensor(out=es[:, 0:ncol], in0=psf(pc)[:, 0:ncol], in1=es[:, 0:ncol],
                                                            op=ALU.subtract), reads=[PSB[pc], esb], writes=[esb])
                    yield
                    a_, ab_ = ar.next()
                    S.act(lambda e: nc.scalar.activation(out=a_[:, 0:ncol], in_=es[:, 0:ncol], func=AF.Exp),
                          reads=[esb], writes=[ab_])
                    yield
                    if diag:
                        S.pool(lambda e: nc.gpsimd.tensor_tensor(out=a_[:, 0:128], in0=a_[:, 0:128], in1=strictT, op=ALU.mult),
                               reads=[ab_, constb], writes=[ab_])
                        yield
                    S.pe(lambda e: nc.tensor.matmul(psf(po)[:, cc:512], lhsT=v[:, kt, h * 128:(h + 1) * 128], rhs=a_[:, 0:ncol],
                                                    start=(step == 0), stop=(step == nsteps - 1)),
                         reads=[vb_, ab_], writes=[PSB[po]])
                    yield
                    if step == nsteps - 1:
                        hh = hg * 4 + h
                        S.act(lambda e: nc.scalar.copy(out=oT[:, hh, qb * 512:(qb + 1) * 512], in_=psf(po)),
                              reads=[PSB[po]], writes=oTb[4 * qb:4 * qb + 4])
                        yield

                pending = None
                for h in heads:
                    for qb in range(4):
                        kts = list(range(4 * qb + 3, -1, -1))
                        ccs = [max(0, kt - 4 * qb) * 128 for kt in kts]
                        for step, kt in enumerate(kts):
                            f = {"h": h, "qb": qb, "kt": kt, "step": step, "nsteps": len(kts), "cc": ccs[step],
                                 "ncol": 512 - ccs[step], "diag": kt >= 4 * qb,
                                 "cc_next": ccs[step + 1] if step + 1 < len(kts) else 0}
                            yield from S1(f)
                            if pending is not None:
                                yield from S2(pending)
                            pending = f
                yield from S2(pending)

            run_streams([sb_stream(0, [0, 1]), sb_stream(1, [2, 3])])
            S.flush()
            A.release(mp)
        A.release(m)

    def dump_h(s):
        m = A.mark()
        t = [(A.alloc([128, 1024], F32), Buf(), f"xin{k}") for k in range(2)]
        for i in range(NT):
            tt, tb, ch = t[i % 2]
            S.dma(lambda e, tt=tt, i=i: nc.sync.dma_start(out=tt, in_=h_scr[s, i * 128:(i + 1) * 128, :]), ch,
                  reads=[hb[s][i]], writes=[tb])
            S.dma(lambda e, tt=tt, i=i: nc.sync.dma_start(out=out[s, i * 128:(i + 1) * 128, :], in_=tt), f"st{i % 2}",
                  reads=[tb], writes=[])
        S.flush()
        A.release(m)

    for s in range(NSEQ):
        if stop != "const":
            phase_norm(s, x)
        if stop == "norm":
            continue
        if stop == "const":
            continue
        dsa(s)
        if stop == "dsa":
            continue
        phase_norm(s, x)
        gla(s)
        if stop == "gla":
            continue
        out_proj_and_norm(s, w_out_ab, x, h_scr)
        if stop == "mix0":
            dump_h(s)
            continue
        ffn(s, 0, False)
        if stop == "ffn0":
            dump_h(s)
            continue
        phase_norm(s, h_scr)
        sb_attn(s)
        out_proj_and_norm(s, w_out_c, h_scr, h_scr)
        if stop == "mix1":
            dump_h(s)
            continue
        ffn(s, 1, True)
    S.flush(final=True)
    return nc


def host_constants():
    c = {}
    c["c_ident"] = np.eye(128, dtype=np.float32)
    pos = np.arange(SEQ, dtype=np.float32)
    inv_a = np.power(np.float32(500000.0), -np.arange(16, dtype=np.float32) * 2.0 / 32).astype(np.float32)
    ang = pos[:, None] * inv_a[None, :]
    c["c_rope_a"] = np.concatenate([np.cos(ang), np.sin(ang)], axis=1).astype(np.float32)
    inv_i = np.power(np.float32(500000.0), -np.arange(8, dtype=np.float32) * 2.0 / 16).astype(np.float32)
    ang = pos[:, None] * inv_i[None, :]
    c["c_rope_i"] = np.concatenate([np.cos(ang), np.sin(ang)], axis=1).astype(np.float32)
    a = np.arange(64)
    m64 = np.zeros((64, 3, 64), np.float32)
    m64[:, 0, :] = (a[:, None] <= a[None, :]) * (-1.0 / 16.0)
    m64[:, 1, :] = (a[:, None] > a[None, :]) * (-1.0 / 16.0)
    m64[:, 2, :] = (a[:, None] <= a[None, :]) * 1.0
    c["c_m64"] = m64
    b = np.arange(128)
    m128 = np.zeros((128, 3, 128), np.float32)
    m128[:, 0, :] = (b[:, None] > b[None, :]) * 1.0
    m128[:, 1, :] = 1.0
    m128[:, 2, :] = (b[:, None] < b[None, :]) * 1.0
    c["c_m128"] = m128
    return c


_CACHE = {}


def kernel(x, g_mix, g_ffn, w_in_ab, gq_a, gk_a, w_gate_up, b_gate, g_gla, w_out_ab,
           w_in_c, gq_c, gk_c, w_out_c, w_up, w_down, _ncores=8, _stop=None):
    f = lambda a: np.ascontiguousarray(np.asarray(a, dtype=np.float32))
    x = f(x)
    nseq = x.shape[0] // _ncores
    shared = {
        "w_in_ab": f(w_in_ab)[0], "w_gate_up": f(w_gate_up)[0], "b_gate": f(b_gate), "w_out_ab": f(w_out_ab)[0],
        "w_in_c": f(w_in_c)[0], "w_out_c": f(w_out_c)[0], "w_up": f(w_up), "w_down": f(w_down),
    }
    gains = np.stack([np.broadcast_to(f(g).reshape(1, 128), (128, 128)) for g in (gq_a, gk_a, gq_c, gk_c, g_gla)], axis=1)
    shared["c_gains"] = np.ascontiguousarray(gains, dtype=np.float32)
    gcols = np.stack([f(g_mix)[0].reshape(8, 128).T, f(g_mix)[1].reshape(8, 128).T,
                      f(g_ffn)[0].reshape(8, 128).T, f(g_ffn)[1].reshape(8, 128).T], axis=1)
    shared["c_gcol"] = np.ascontiguousarray(gcols, dtype=np.float32)
    shared.update(host_constants())
    key = (nseq, _stop)
    if key not in _CACHE:
        _CACHE[key] = build_program(nseq, _stop)
    nc = _CACHE[key]
    in_maps = []
    for c in range(_ncores):
        d = dict(shared)
        d["x"] = np.ascontiguousarray(x[c * nseq:(c + 1) * nseq])
        in_maps.append(d)
    res = run_bass_kernel_spmd(nc, in_maps, core_ids=list(range(_ncores)))
    return np.concatenate([np.asarray(r["out"], dtype=np.float32) for r in res.results], axis=0)
```

```python
import numpy as np
import concourse.bass as bass
import concourse.mybir as mybir
from concourse.bass_utils import run_bass_kernel_spmd
from concourse.alu_op_type import AluOpType as ALU

F32 = mybir.dt.float32
BF16 = mybir.dt.bfloat16
AF = mybir.ActivationFunctionType
AX = mybir.AxisListType

SEQ = 2048
DM = 1024
NT = SEQ // 128
EPS = 1e-6
ABW = 3672
NEG = -1.0e30
BIS_ITERS = 12
ARENA_BYTES = 175 * 1024

_ENG_ATTR = {"pe": "tensor", "act": "scalar", "dve": "vector", "pool": "gpsimd", "sp": "sync"}


class Buf:
    __slots__ = ("lw", "rd")

    def __init__(self):
        self.lw = None
        self.rd = {}


class Sched:
    ENG = ("pe", "act", "dve", "pool", "sp")

    def __init__(self, nc):
        self.nc = nc
        self.sem = {e: nc.alloc_semaphore("sem_" + e) for e in self.ENG}
        self.cnt = {e: 0 for e in self.ENG}
        self.ops = {e: [] for e in self.ENG}
        self.waited = {e: {} for e in self.ENG}
        self.chan = {}

    def channel(self, name):
        if name not in self.chan:
            self.chan[name] = [self.nc.alloc_semaphore("ch_" + name), 0]
        return name

    def _deps(self, reads, writes):
        d = {}
        for b in reads:
            if b.lw is not None and d.get(b.lw[0], 0) < b.lw[1]:
                d[b.lw[0]] = b.lw[1]
        for b in writes:
            if b.lw is not None and d.get(b.lw[0], 0) < b.lw[1]:
                d[b.lw[0]] = b.lw[1]
            for k, v in b.rd.items():
                if d.get(k, 0) < v:
                    d[k] = v
        return d

    def op(self, eng, fn, reads=(), writes=()):
        d = self._deps(reads, writes)
        self.cnt[eng] += 1
        idx = self.cnt[eng]
        for b in reads:
            b.rd[eng] = idx
        for b in writes:
            b.lw = (eng, idx)
            b.rd = {}
        self.ops[eng].append((d, fn, None))

    def pe(self, fn, reads=(), writes=()):
        self.op("pe", fn, reads, writes)

    def act(self, fn, reads=(), writes=()):
        self.op("act", fn, reads, writes)

    def dve(self, fn, reads=(), writes=()):
        self.op("dve", fn, reads, writes)

    def pool(self, fn, reads=(), writes=()):
        self.op("pool", fn, reads, writes)

    def dma(self, fn, chan, reads=(), writes=(), queue="sp"):
        d = self._deps(reads, writes)
        c = self.chan[chan]
        c[1] += 16
        key = "ch:" + chan
        for b in reads:
            b.rd[key] = c[1]
        for b in writes:
            b.lw = (key, c[1])
            b.rd = {}
        self.ops[queue].append((d, fn, chan))

    def _semof(self, k):
        if k.startswith("ch:"):
            return self.chan[k[3:]][0]
        return self.sem[k]

    def flush(self, final=False):
        nc = self.nc
        if final:
            d = {}
            for name, c in self.chan.items():
                if c[1] > 0:
                    d["ch:" + name] = c[1]
            self.ops["sp"].append((d, None, None))
        with nc.Block() as blk:
            for eng in self.ENG:
                ops = self.ops[eng]

                def body(e, eng=eng, ops=ops):
                    w = self.waited[eng]
                    for d, fn, chan in ops:
                        for k, v in d.items():
                            if k == eng and eng == "pe":
                                continue
                            if w.get(k, 0) < v:
                                e.wait_ge(self._semof(k), v)
                                w[k] = v
                        if fn is None:
                            continue
                        inst = fn(e)
                        if chan is None:
                            inst.then_inc(self.sem[eng], 1)
                        else:
                            inst.then_inc(self.chan[chan][0], 16)

                getattr(blk, _ENG_ATTR[eng])(body)
        self.ops = {e: [] for e in self.ENG}


class Arena:
    def __init__(self, nc, nbytes, base=None):
        self.t = nc.alloc_sbuf_tensor("arena", [128, nbytes // 4], F32) if base is None else base
        self.top = 0
        self.nbytes = nbytes

    def mark(self):
        return self.top

    def release(self, m):
        self.top = m

    def alloc(self, shape, dtype):
        esz = 4 if dtype == F32 else 2
        n = int(np.prod(shape[1:]))
        nb = (n * esz + 63) // 64 * 64
        off = self.top
        self.top += nb
        assert self.top <= self.nbytes, ("arena overflow", self.top)
        ap = self.t[0:shape[0], off // 4:(off + nb) // 4]
        if dtype != F32:
            ap = ap.bitcast(dtype)
        ap = ap[:, 0:n]
        if len(shape) == 3:
            ap = ap.rearrange("p (a b) -> p a b", a=shape[1])
        elif len(shape) == 4:
            ap = ap.rearrange("p (a b c) -> p a b c", a=shape[1], b=shape[2])
        return ap


class Rot:
    def __init__(self, arena, n, shape, dtype):
        self.items = [(arena.alloc(shape, dtype), Buf()) for _ in range(n)]
        self.i = 0

    def next(self):
        it = self.items[self.i % len(self.items)]
        self.i += 1
        return it


def run_streams(gens):
    gens = list(gens)
    while gens:
        for g in list(gens):
            try:
                next(g)
            except StopIteration:
                gens.remove(g)


def build_program(NSEQ=2, stop=None):
    nc = bass.Bass("TRN2", target_bir_lowering=False)
    S = Sched(nc)

    def dram_in(name, shape):
        return nc.dram_tensor(name, shape, F32, kind="ExternalInput").ap()

    x = dram_in("x", [NSEQ, SEQ, DM])
    w_in_ab = dram_in("w_in_ab", [DM, ABW])
    w_gate_up = dram_in("w_gate_up", [16, 256])
    b_gate = dram_in("b_gate", [1, 256])
    w_out_ab = dram_in("w_out_ab", [DM, DM])
    w_in_c = dram_in("w_in_c", [DM, 3 * DM])
    w_out_c = dram_in("w_out_c", [DM, DM])
    w_up = dram_in("w_up", [2, DM, 4 * DM])
    w_down = dram_in("w_down", [2, 4 * DM, DM])
    c_ident = dram_in("c_ident", [128, 128])
    c_rope_a = dram_in("c_rope_a", [SEQ, 32])
    c_rope_i = dram_in("c_rope_i", [SEQ, 16])
    c_m64 = dram_in("c_m64", [64, 3, 64])
    c_m128 = dram_in("c_m128", [128, 3, 128])
    c_gains = dram_in("c_gains", [128, 5, 128])
    c_gcol = dram_in("c_gcol", [128, 4, 8])
    out = nc.dram_tensor("out", [NSEQ, SEQ, DM], F32, kind="ExternalOutput").ap()
    h_scr = nc.dram_tensor("h_scr", [NSEQ, SEQ, DM], F32).ap()
    hb = [[Buf() for _ in range(NT)] for _ in range(NSEQ)]

    A = Arena(nc, ARENA_BYTES)
    PS = [nc.alloc_psum_tensor(f"psb{i}", [128, 512], F32) for i in range(8)]
    PSB = [Buf() for _ in range(8)]

    def psf(i):
        return PS[i][:, :]

    def psh(i):
        return PS[i][:, :].bitcast(BF16)

    for nm in ("c0", "c1", "c2", "c3", "xin0", "xin1", "xin2", "xin3", "st0", "st1", "stg0", "stg1", "hres"):
        S.channel(nm)

    nT_raw = A.alloc([128, 8192], F32)
    nT = nT_raw.bitcast(BF16).rearrange("p (a b) -> p a b", a=8)
    nTb = [Buf() for _ in range(NT)]
    oT_raw = A.alloc([128, 8192], F32)
    oT = oT_raw.bitcast(BF16).rearrange("p (a b) -> p a b", a=8)
    oTb = [Buf() for _ in range(NT)]
    identf = A.alloc([128, 128], F32)
    ident = A.alloc([128, 128], BF16)
    ropeA = A.alloc([128, NT, 32], F32)
    ropeI = A.alloc([128, NT, 16], F32)
    m64 = A.alloc([64, 3, 64], F32)
    m128f = A.alloc([128, 3, 128], F32)
    m128 = A.alloc([128, 3, 128], BF16)
    gains = A.alloc([128, 5, 128], F32)
    gcol = A.alloc([128, 4, 8], F32)
    wg = A.alloc([32, 256], F32)
    thr_all = A.alloc([128, 1], F32)
    constb = Buf()

    def bc_row(ap):
        r = ap.partition_broadcast(128)
        if len(r.shape) == 3:
            r = r.rearrange("p a b -> p (a b)")
        return r

    def dma_simple(out_ap, in_ap, chan, writes, reads=(), ncdma=True):
        S.dma(lambda e: nc.sync.dma_start(out=out_ap, in_=in_ap), chan, reads=reads, writes=writes)

    dma_simple(identf, c_ident, "c0", [constb])
    dma_simple(ropeA, c_rope_a.rearrange("(i p) c -> p i c", p=128), "c0", [constb])
    dma_simple(ropeI, c_rope_i.rearrange("(i p) c -> p i c", p=128), "c0", [constb])
    dma_simple(m64, c_m64, "c0", [constb])
    dma_simple(m128f, c_m128, "c0", [constb])
    dma_simple(gains, c_gains, "c0", [constb])
    dma_simple(wg[0:16, :], w_gate_up, "c0", [constb])
    dma_simple(wg[16:17, :], b_gate, "c0", [constb])
    dma_simple(gcol, c_gcol, "c0", [constb])
    S.dve(lambda e: nc.vector.tensor_copy(out=ident, in_=identf), reads=[constb], writes=[constb])
    S.dve(lambda e: nc.vector.tensor_copy(out=m128, in_=m128f), reads=[constb], writes=[constb])
    S.dve(lambda e: nc.vector.memset(thr_all, -1.0e29), reads=[], writes=[constb])
    S.flush()
    base_mark = A.mark()

    triU = m128[:, 0, :]
    ones128 = m128[:, 1, :]
    strictT = m128[:, 2, :]
    tri_incl = m64[:, 0, :]
    tri_rev = m64[:, 1, :]
    causT = m64[:, 2, :]

    def mk_stage(ar=None):
        ar = A if ar is None else ar
        return [(ar.alloc([128, 2048], F32), Buf(), S.channel(f"stg{i}")) for i in range(2)]

    stage_state = {"pool": None, "i": 0}

    def load_w(src2d, KC, ncols, dst, dstb, gc=None):
        srcv = src2d.rearrange("(k p) c -> p k c", p=128)
        kstep = max(1, 2048 // ncols)
        for k0 in range(0, KC, kstep):
            kn = min(kstep, KC - k0)
            st, stb, ch = stage_state["pool"][stage_state["i"] % 2]
            stage_state["i"] += 1
            stv = st[:, 0:kn * ncols].rearrange("p (k c) -> p k c", k=kn)
            S.dma(lambda e, stv=stv, k0=k0, kn=kn: nc.sync.dma_start(out=stv, in_=srcv[:, k0:k0 + kn, :]),
                  ch, writes=[stb])
            if gc is None:
                S.pool(lambda e, stv=stv, k0=k0, kn=kn: nc.gpsimd.tensor_copy(out=dst[:, k0:k0 + kn, :], in_=stv),
                       reads=[stb], writes=[dstb])
            else:
                S.pool(lambda e, stv=stv, k0=k0, kn=kn: nc.gpsimd.tensor_tensor(
                    out=dst[:, k0:k0 + kn, :], in0=stv,
                    in1=gc[:, k0:k0 + kn].unsqueeze(2).to_broadcast([128, kn, ncols]), op=ALU.mult),
                    reads=[stb, constb], writes=[dstb])

    def mm_group(out_ap, pairs, rd, wr):
        def f(e):
            n = len(pairs)
            inst = None
            for q, (l, r) in enumerate(pairs):
                inst = nc.tensor.matmul(out_ap, lhsT=l, rhs=r, start=(q == 0), stop=(q == n - 1))
            return inst
        S.pe(f, reads=rd, writes=wr)

    def transposes(outs_ins, idn, rd, wr):
        def f(e):
            inst = None
            for o, i_ in outs_ins:
                inst = nc.tensor.transpose(o, i_, idn)
            return inst
        S.pe(f, reads=rd, writes=wr)

    def rstd_from_ss(ssv, ssb, n, inv_n, add_eps):
        P = ssv.shape[0]
        if add_eps:
            S.dve(lambda e: nc.vector.tensor_scalar(out=ssv[:, 0, :], in0=ssv[:, 0, :], scalar1=inv_n, scalar2=EPS,
                                                    op0=ALU.mult, op1=ALU.add), reads=[ssb], writes=[ssb])
            S.act(lambda e: nc.scalar.activation(out=ssv[:, 1, :], in_=ssv[:, 0, :], func=AF.Ln), reads=[ssb], writes=[ssb])
        else:
            S.act(lambda e: nc.scalar.activation(out=ssv[:, 1, :], in_=ssv[:, 0, :], func=AF.Ln, scale=inv_n),
                  reads=[ssb], writes=[ssb])
        S.act(lambda e: nc.scalar.activation(out=ssv[:, 2, :], in_=ssv[:, 1, :], func=AF.Exp, scale=-0.5),
              reads=[ssb], writes=[ssb])

    def rope(xv, xb, H, half, table, i, tmp, tmpb, P=128, prow=None):
        if prow is None:
            cs = table[0:P, i, :]
        else:
            cs = prow
        cos = cs[:, 0:half].unsqueeze(1).to_broadcast([P, H, half])
        sin = cs[:, half:2 * half].unsqueeze(1).to_broadcast([P, H, half])
        x1 = xv[:, :, 0:half]
        x2 = xv[:, :, half:2 * half]

        def f1(e):
            nc.gpsimd.tensor_tensor(out=tmp[:, 0], in0=x1, in1=cos, op=ALU.mult)
            nc.gpsimd.tensor_tensor(out=tmp[:, 1], in0=x2, in1=sin, op=ALU.mult)
            nc.gpsimd.tensor_tensor(out=tmp[:, 2], in0=x2, in1=cos, op=ALU.mult)
            return nc.gpsimd.tensor_tensor(out=tmp[:, 3], in0=x1, in1=sin, op=ALU.mult)
        S.pool(f1, reads=[xb, constb], writes=[tmpb])

        def f2(e):
            nc.gpsimd.tensor_tensor(out=x1, in0=tmp[:, 0], in1=tmp[:, 1], op=ALU.subtract)
            return nc.gpsimd.tensor_tensor(out=x2, in0=tmp[:, 2], in1=tmp[:, 3], op=ALU.add)
        S.pool(f2, reads=[tmpb], writes=[xb])

    def norm_transpose(xt, xtb, i, ws):
        junk, jb = ws["junk"].next()
        ss, sb = ws["ss"].next()
        nb, nbb = ws["nb"].next()
        S.act(lambda e: nc.scalar.activation(out=junk, in_=xt, func=AF.Square, accum_out=ss[:, 0, 0:1]),
              reads=[xtb], writes=[jb, sb])
        yield
        S.dve(lambda e: nc.vector.tensor_scalar(out=ss[:, 0, :], in0=ss[:, 0, :], scalar1=1.0 / DM, scalar2=EPS,
                                                op0=ALU.mult, op1=ALU.add), reads=[sb], writes=[sb])
        yield
        S.act(lambda e: nc.scalar.activation(out=ss[:, 1, :], in_=ss[:, 0, :], func=AF.Ln), reads=[sb], writes=[sb])
        yield
        S.act(lambda e: nc.scalar.activation(out=ss[:, 2, :], in_=ss[:, 1, :], func=AF.Exp, scale=-0.5), reads=[sb], writes=[sb])
        yield
        S.dve(lambda e: nc.vector.tensor_scalar(out=nb, in0=xt, scalar1=ss[:, 2, 0:1], scalar2=None, op0=ALU.mult),
              reads=[xtb, sb], writes=[nbb])
        yield
        pb = ws["psT"]
        pv = psh(pb)
        transposes([(pv[:, kc * 128:(kc + 1) * 128], nb[:, kc * 128:(kc + 1) * 128]) for kc in range(8)], ident,
                   [nbb, constb], [PSB[pb]])
        yield
        S.act(lambda e: nc.scalar.copy(out=nT[:, :, i * 128:(i + 1) * 128],
                                       in_=pv[:, 0:1024].rearrange("p (k t) -> p k t", k=8)),
              reads=[PSB[pb]], writes=[nTb[i]])
        yield

    def mk_norm_ws(psT):
        return {"junk": Rot(A, 1, [128, 1024], BF16), "ss": Rot(A, 2, [128, 3, 1], F32),
                "nb": Rot(A, 1, [128, 1024], BF16), "psT": psT}

    def phase_norm(s, src):
        m = A.mark()
        NS = 4
        wsl = [mk_norm_ws(4 + k) for k in range(NS)]
        xin = [(A.alloc([128, 1024], F32), Buf(), S.channel(f"xin{k}")) for k in range(NS)]

        def one(k, i):
            xt, xb, ch = xin[k]
            S.dma(lambda e: nc.sync.dma_start(out=xt, in_=src[s, i * 128:(i + 1) * 128, :]), ch,
                  reads=[hb[s][i]], writes=[xb])
            yield
            yield from norm_transpose(xt, xb, i, wsl[k])

        def stream(k):
            for i in range(k, NT, NS):
                yield from one(k, i)
        run_streams([stream(k) for k in range(NS)])
        S.flush()
        A.release(m)

    def qk_post(ps_i, i, ws, gidx, dstT, dst_b, scale, do_rope):
        sq, sqb = ws["sq"].next()
        ss, ssb = ws["ss4"].next()
        qn, qnb = ws["qn"].next()
        qh, qhb = ws["qh"].next()
        pv = psf(ps_i)
        S.act(lambda e: nc.scalar.activation(out=sq, in_=pv, func=AF.Square), reads=[PSB[ps_i]], writes=[sqb])
        yield
        S.dve(lambda e: nc.vector.tensor_reduce(out=ss[:, 0, :], in_=sq.rearrange("p (h d) -> p h d", h=4), axis=AX.X,
                                                op=ALU.add), reads=[sqb], writes=[ssb])
        yield
        S.dve(lambda e: nc.vector.tensor_scalar(out=ss[:, 0, :], in0=ss[:, 0, :], scalar1=1.0 / 128, scalar2=EPS,
                                                op0=ALU.mult, op1=ALU.add), reads=[ssb], writes=[ssb])
        yield
        S.act(lambda e: nc.scalar.activation(out=ss[:, 1, :], in_=ss[:, 0, :], func=AF.Ln), reads=[ssb], writes=[ssb])
        yield
        S.act(lambda e: nc.scalar.activation(out=ss[:, 2, :], in_=ss[:, 1, :], func=AF.Exp, scale=-0.5),
              reads=[ssb], writes=[ssb])
        yield

        def f(e):
            inst = None
            for h in range(4):
                inst = nc.vector.scalar_tensor_tensor(out=qn[:, h * 128:(h + 1) * 128], in0=pv[:, h * 128:(h + 1) * 128],
                                                      scalar=ss[:, 2, h:h + 1], in1=gains[:, gidx, :], op0=ALU.mult,
                                                      op1=ALU.mult)
            return inst
        S.dve(f, reads=[PSB[ps_i], ssb, constb], writes=[qnb])
        yield
        if do_rope:
            tmp, tmpb = ws["rtmp"].next()
            rope(qn.rearrange("p (h d) -> p h d", h=4), qnb, 4, 16, ropeA, i, tmp, tmpb)
            yield
        S.act(lambda e: nc.scalar.activation(out=qh, in_=qn, func=AF.Copy, scale=scale), reads=[qnb], writes=[qhb])
        yield
        pt = ws["psT"]
        ptv = psh(pt)
        transposes([(ptv[:, h * 128:(h + 1) * 128], qh[:, h * 128:(h + 1) * 128]) for h in range(4)], ident,
                   [qhb, constb], [PSB[pt]])
        yield
        S.act(lambda e: nc.scalar.copy(out=dstT[:, :, i * 128:(i + 1) * 128],
                                       in_=ptv[:, 0:512].rearrange("p (h t) -> p h t", h=4)),
              reads=[PSB[pt]], writes=[dst_b])
        yield

    NPS = 3

    def proj_streamed(wb, wbb, ncols, handler):
        def stream(k):
            for i in range(k, NT, NPS):
                proj_tok(wb, ncols, i, k, wbb)
                yield
                yield from handler(k, i, k)
        run_streams([stream(k) for k in range(NPS)])

    def proj_tok(wb, ncols, i, ps_i, wbb, M=128, t0=None):
        t0 = i * 128 if t0 is None else t0
        mm_group(psf(ps_i)[0:M, 0:ncols], [(nT[:, kc, t0:t0 + M], wb[:, kc, 0:ncols]) for kc in range(8)],
                 [nTb[t0 // 128], wbb], [PSB[ps_i]])

    def mk_qk_ws(psT):
        return {"sq": Rot(A, 1, [128, 512], F32), "ss4": Rot(A, 2, [128, 3, 4], F32), "qn": Rot(A, 1, [128, 512], F32),
                "qh": Rot(A, 1, [128, 512], BF16), "rtmp": Rot(A, 1, [128, 4, 4, 16], F32), "psT": psT}

    def out_proj_and_norm(s, wout_dram, src, dst):
        m = A.mark()
        stage_state["pool"] = mk_stage()
        wo = A.alloc([128, 8, DM], BF16)
        wob = Buf()
        for c0 in range(0, DM, 256):
            load_w(wout_dram[:, c0:c0 + 256], 8, 256, wo[:, :, c0:c0 + 256], wob)
        wsl = [mk_norm_ws(6), mk_norm_ws(7)]
        hin = [(A.alloc([128, 1024], F32), Buf(), f"xin{k}") for k in range(2)]
        hnew = [(A.alloc([128, 1024], F32), Buf(), f"st{k}") for k in range(2)]

        def one(k, i):
            hi_, hib, ch = hin[k]
            hn, hnb, sch = hnew[k]
            S.dma(lambda e: nc.sync.dma_start(out=hi_, in_=src[s, i * 128:(i + 1) * 128, :]), ch,
                  reads=[hb[s][i]], writes=[hib])
            yield
            for half in range(2):
                pi = 2 * k + half
                mm_group(psf(pi), [(oT[:, c, i * 128:(i + 1) * 128], wo[:, c, half * 512:(half + 1) * 512])
                                   for c in range(8)], [oTb[i], wob], [PSB[pi]])
                yield
                S.dve(lambda e, pi=pi, half=half: nc.vector.tensor_tensor(
                    out=hn[:, half * 512:(half + 1) * 512], in0=psf(pi), in1=hi_[:, half * 512:(half + 1) * 512],
                    op=ALU.add), reads=[PSB[pi], hib], writes=[hnb])
                yield
            S.dma(lambda e: nc.sync.dma_start(out=dst[s, i * 128:(i + 1) * 128, :], in_=hn), sch,
                  reads=[hnb], writes=[hb[s][i]])
            yield
            yield from norm_transpose(hn, hnb, i, wsl[k])

        def stream(k):
            for i in range(k, NT, 2):
                yield from one(k, i)
        run_streams([stream(0), stream(1)])
        S.flush()
        A.release(m)

    def ffn(s, layer, final):
        m = A.mark()
        stage_state["pool"] = mk_stage()
        hres = A.alloc([128, NT, DM], F32)
        hresb = [Buf() for _ in range(NT)]
        actT = A.alloc([128, 4, SEQ], BF16)
        actb = [Buf() for _ in range(4)]
        AO = Arena(nc, 32768, base=oT_raw)
        wub = [(AO.alloc([128, 8, 512], BF16), Buf()) for _ in range(2)]
        wdb = [(AO.alloc([128, 4, DM], BF16), Buf()) for _ in range(2)]
        rr = Rot(A, 2, [128, 512], F32)
        for i in range(NT):
            S.dma(lambda e, i=i: nc.sync.dma_start(out=hres[:, i, :], in_=h_scr[s, i * 128:(i + 1) * 128, :]), "hres",
                  reads=[hb[s][i]], writes=[hresb[i]])
        gc = gcol[:, 2 + layer, :]
        for g in range(8):
            wu, wubb = wub[g % 2]
            wd, wdbb = wdb[g % 2]
            load_w(w_up[layer, :, g * 512:(g + 1) * 512], 8, 512, wu, wubb, gc)
            load_w(w_down[layer, g * 512:(g + 1) * 512, :], 4, DM, wd, wdbb)
            for fc in range(4):
                for tb in range(4):
                    pi = (fc * 4 + tb) % 4
                    mm_group(psf(pi), [(wu[:, kc, fc * 128:(fc + 1) * 128], nT[:, kc, tb * 512:(tb + 1) * 512])
                                       for kc in range(8)], nTb[4 * tb:4 * tb + 4] + [wubb], [PSB[pi]])
                    r, rb = rr.next()
                    S.act(lambda e, r=r, pi=pi: nc.scalar.activation(out=r, in_=psf(pi), func=AF.Relu),
                          reads=[PSB[pi]], writes=[rb])
                    S.pool(lambda e, r=r, fc=fc, tb=tb: nc.gpsimd.tensor_tensor(
                        out=actT[:, fc, tb * 512:(tb + 1) * 512], in0=r, in1=r, op=ALU.mult),
                        reads=[rb], writes=[actb[tb]])
            for ti in range(NT):
                for half in range(2):
                    pi = 4 + (ti * 2 + half) % 4
                    mm_group(psf(pi), [(actT[:, fc, ti * 128:(ti + 1) * 128], wd[:, fc, half * 512:(half + 1) * 512])
                                       for fc in range(4)], [actb[ti // 4], wdbb], [PSB[pi]])
                    S.dve(lambda e, pi=pi, ti=ti, half=half: nc.vector.tensor_tensor(
                        out=hres[:, ti, half * 512:(half + 1) * 512], in0=psf(pi),
                        in1=hres[:, ti, half * 512:(half + 1) * 512], op=ALU.add),
                        reads=[PSB[pi], hresb[ti]], writes=[hresb[ti]])
        dst = out if final else h_scr
        for i in range(NT):
            S.dma(lambda e, i=i: nc.sync.dma_start(out=dst[s, i * 128:(i + 1) * 128, :], in_=hres[:, i, :]),
                  f"st{i % 2}", reads=[hresb[i]], writes=[hb[s][i]])
        S.flush()
        A.release(m)

    def dsa(s):
        m = A.mark()
        kaT = A.alloc([128, 4, SEQ], BF16)
        qaT = A.alloc([128, 4, SEQ], BF16)
        va = A.alloc([128, NT, 4, 132], BF16)
        iqT = A.alloc([128, 4, SEQ], BF16)
        ikT = A.alloc([128, SEQ], BF16)
        iws = A.alloc([128, NT, 8], F32)
        kaTb, qaTb, vab, iqTb, ikTb, iwsb = Buf(), Buf(), Buf(), Buf(), Buf(), Buf()
        m2 = A.mark()
        AO = Arena(nc, 32768, base=oT_raw)
        stage_state["pool"] = mk_stage(AO)
        wbp = [(AO.alloc([128, 8, 512], BF16), Buf()) for _ in range(2)]
        wss = [mk_qk_ws(5 + k) for k in range(NPS)]
        cp = [wss[k]["qn"] for k in range(NPS)]
        cpb = [wss[k]["qh"] for k in range(NPS)]
        itmp = [Rot(A, 1, [128, 4, 8, 8], F32) for _ in range(NPS)]
        ikf = [Rot(A, 1, [128, 72], F32) for _ in range(NPS)]
        ikd = [Rot(A, 1, [128, 128], BF16) for _ in range(NPS)]
        gc = gcol[:, 0, :]
        S.pool(lambda e: nc.gpsimd.memset(va[:, :, :, 128:129], 1.0), writes=[vab])

        def h_va(pi, i, k):
            S.act(lambda e: nc.scalar.copy(out=va[:, i, :, 0:128], in_=psf(pi).rearrange("p (h d) -> p h d", h=4)),
                  reads=[PSB[pi]], writes=[vab])
            yield

        def h_iq(pi, i, k):
            c, cb = cp[k].next()
            S.act(lambda e: nc.scalar.copy(out=c, in_=psf(pi)), reads=[PSB[pi]], writes=[cb])
            yield
            tmp, tmpb = itmp[k].next()
            rope(c.rearrange("p (h d) -> p h d", h=8), cb, 8, 8, ropeI, i, tmp, tmpb)
            yield
            ch_, chb = cpb[k].next()
            S.act(lambda e: nc.scalar.copy(out=ch_, in_=c), reads=[cb], writes=[chb])
            yield
            ptv = psh(5 + k)
            transposes([(ptv[:, p * 128:(p + 1) * 128], ch_[:, p * 128:(p + 1) * 128]) for p in range(4)], ident,
                       [chb, constb], [PSB[5 + k]])
            yield
            S.act(lambda e: nc.scalar.copy(out=iqT[:, :, i * 128:(i + 1) * 128],
                                           in_=ptv[:, 0:512].rearrange("p (h t) -> p h t", h=4)),
                  reads=[PSB[5 + k]], writes=[iqTb])
            yield

        def h_ik(pi, i, k):
            f_, fb = ikf[k].next()
            S.act(lambda e: nc.scalar.copy(out=f_, in_=psf(pi)[:, 0:72]), reads=[PSB[pi]], writes=[fb])
            yield
            tmp, tmpb = itmp[k].next()
            rope(f_[:, 0:64].rearrange("p (h d) -> p h d", h=1), fb, 1, 8, ropeI, i, tmp[:, :, 0:1, :], tmpb)
            yield
            d_, db = ikd[k].next()

            def fcp(e):
                nc.vector.tensor_copy(out=d_[:, 0:64], in_=f_[:, 0:64])
                nc.vector.tensor_copy(out=d_[:, 64:128], in_=f_[:, 0:64])
                return nc.vector.tensor_scalar(out=iws[:, i, :], in0=f_[:, 64:72], scalar1=1.0 / (8.0 * 8.0 ** 0.5),
                                               scalar2=None, op0=ALU.mult)
            S.dve(fcp, reads=[fb], writes=[db, iwsb])
            yield
            ptv = psh(5 + k)
            transposes([(ptv[:, 0:128], d_)], ident, [db, constb], [PSB[5 + k]])
            yield
            S.act(lambda e: nc.scalar.copy(out=ikT[:, i * 128:(i + 1) * 128], in_=ptv[:, 0:128]),
                  reads=[PSB[5 + k]], writes=[ikTb])
            yield

        blocks = [("qa", 0), ("ka", 512), ("va", 1024), ("iq", 1536), ("ik", 2048)]
        for bi, (nm, c0) in enumerate(blocks):
            ncols = 72 if nm == "ik" else 512
            wb, wbb = wbp[bi % 2]
            load_w(w_in_ab[:, c0:c0 + ncols], 8, ncols, wb[:, :, 0:ncols], wbb, gc)
            if nm == "qa":
                proj_streamed(wb, wbb, 512, lambda pi, i, k: qk_post(pi, i, wss[k], 0, qaT, qaTb, 128 ** -0.5, True))
            elif nm == "ka":
                proj_streamed(wb, wbb, 512, lambda pi, i, k: qk_post(pi, i, wss[k], 1, kaT, kaTb, 1.0, True))
            elif nm == "va":
                proj_streamed(wb, wbb, 512, h_va)
            elif nm == "iq":
                proj_streamed(wb, wbb, 512, h_iq)
            else:
                proj_streamed(wb, wbb, 72, h_ik)
        S.flush()
        A.release(m2)
        AN = Arena(nc, 32768, base=nT_raw)
        junk = AN.alloc([128, SEQ], BF16)
        junkb = Buf()

        def dsa_stream(k):
            sc = AN.alloc([128, SEQ], F32)
            scb = Buf()
            mk = AN.alloc([128, SEQ], BF16)
            mkb = Buf()
            mT = A.alloc([128, NT, 128], BF16)
            mTb = Buf()
            tmpf = Rot(A, 2, [128, 512], F32)
            ptr = Rot(A, 2, [128, 512], BF16)
            ptmr = Rot(A, 2, [128, 512], BF16)
            bsr = Rot(A, 2, [128, 8], F32)
            rzr = Rot(A, 2, [128, 4], F32)
            oar = Rot(A, 1, [128, 512], BF16)
            pidx, pst, ppv = k, 2 + k, 4 + k
            yield
            for j in range(k, NT, 2):
                W = (j + 1) * 128
                for h in range(8):
                    p0 = (h % 2) * 64
                    pair = h // 2
                    for kb in range((W + 511) // 512):
                        c0 = kb * 512
                        cw = min(512, W - c0)
                        mm_group(psf(pidx)[:, 0:cw], [(iqT[p0:p0 + 64, pair, j * 128:(j + 1) * 128], ikT[p0:p0 + 64, c0:c0 + cw])],
                                 [iqTb, ikTb], [PSB[pidx]])
                        yield
                        t_, tb_ = tmpf.next()
                        S.act(lambda e, t_=t_, cw=cw: nc.scalar.activation(out=t_[:, 0:cw], in_=psf(pidx)[:, 0:cw], func=AF.Relu),
                              reads=[PSB[pidx]], writes=[tb_])
                        yield
                        if h == 0:
                            S.dve(lambda e, t_=t_, c0=c0, cw=cw, j=j: nc.vector.tensor_scalar(out=sc[:, c0:c0 + cw], in0=t_[:, 0:cw], scalar1=iws[:, j, 0:1],
                                                                    scalar2=None, op0=ALU.mult), reads=[tb_, iwsb], writes=[scb])
                        else:
                            S.dve(lambda e, t_=t_, c0=c0, cw=cw, j=j, h=h: nc.vector.scalar_tensor_tensor(out=sc[:, c0:c0 + cw], in0=t_[:, 0:cw],
                                                                           scalar=iws[:, j, h:h + 1], in1=sc[:, c0:c0 + cw],
                                                                           op0=ALU.mult, op1=ALU.add),
                                  reads=[tb_, iwsb, scb], writes=[scb])
                        yield
                S.dve(lambda e, W=W: nc.vector.memset(sc[0:64, W - 64:W], NEG), reads=[scb], writes=[scb])
                yield
                if j >= 2:
                    bs, bsb = bsr.next()

                    def f0(e, bs=bs, W=W):
                        nc.vector.tensor_reduce(out=bs[:, 0:1], in_=sc[:, 0:W - 64], axis=AX.X, op=ALU.min)
                        return nc.vector.tensor_reduce(out=bs[:, 1:2], in_=sc[:, 0:W], axis=AX.X, op=ALU.max)
                    S.dve(f0, reads=[scb], writes=[bsb])
                    yield
                    S.dve(lambda e, bs=bs: nc.vector.tensor_tensor(out=bs[:, 1:2], in0=bs[:, 1:2], in1=bs[:, 0:1], op=ALU.subtract),
                          reads=[bsb], writes=[bsb])
                    yield
                    for it in range(1, BIS_ITERS + 1):
                        f = 2.0 ** (-it)
                        S.dve(lambda e, bs=bs, f=f: nc.vector.tensor_scalar(out=bs[:, 2:3], in0=bs[:, 1:2], scalar1=f, scalar2=bs[:, 0:1],
                                                                op0=ALU.mult, op1=ALU.add), reads=[bsb], writes=[bsb])
                        yield
                        S.dve(lambda e, bs=bs, W=W: nc.vector.tensor_scalar(out=junk[:, 0:W], in0=sc[:, 0:W], scalar1=bs[:, 2:3], scalar2=None,
                                                                op0=ALU.is_ge, op1=ALU.add, accum_out=bs[:, 3:4]),
                              reads=[bsb, scb], writes=[bsb, junkb])
                        yield
                        S.dve(lambda e, bs=bs: nc.vector.tensor_scalar(out=bs[:, 4:5], in0=bs[:, 3:4], scalar1=255.5, scalar2=bs[:, 1:2],
                                                                op0=ALU.is_ge, op1=ALU.mult), reads=[bsb], writes=[bsb])
                        yield
                        S.dve(lambda e, bs=bs, f=f: nc.vector.scalar_tensor_tensor(out=bs[:, 0:1], in0=bs[:, 4:5], scalar=f, in1=bs[:, 0:1],
                                                                       op0=ALU.mult, op1=ALU.add), reads=[bsb], writes=[bsb])
                        yield
                    thr, thrb = bs[:, 0:1], bsb
                else:
                    thr, thrb = thr_all[:, 0:1], constb
                S.dve(lambda e, W=W, thr=thr: nc.vector.tensor_scalar(out=mk[:, 0:W], in0=sc[:, 0:W], scalar1=thr, scalar2=None, op0=ALU.is_ge),
                      reads=[scb, thrb], writes=[mkb])
                yield
                for g in range((j + 4) // 4):
                    kts = list(range(4 * g, min(4 * g + 4, j + 1)))
                    n = len(kts)
                    pv = psh(pidx)
                    transposes([(pv[:, q * 128:(q + 1) * 128], mk[:, kt * 128:(kt + 1) * 128]) for q, kt in enumerate(kts)],
                               ident, [mkb, constb], [PSB[pidx]])
                    yield
                    S.act(lambda e, g=g, n=n, pv=pv: nc.scalar.copy(out=mT[:, 4 * g:4 * g + n, :],
                                                   in_=pv[:, 0:n * 128].rearrange("p (k t) -> p k t", k=n)),
                          reads=[PSB[pidx]], writes=[mTb])
                    yield
                oa, oab = oar.next()
                for h in range(4):
                    oc = (h % 2) * 256
                    ov = psf(ppv)[:, oc:oc + 129]
                    for g in range((j + 4) // 4):
                        kts = list(range(4 * g, min(4 * g + 4, j + 1)))
                        n = len(kts)

                        def fs(e, kts=kts, h=h, j=j):
                            inst = None
                            for q, kt in enumerate(kts):
                                inst = nc.tensor.matmul(psf(pst)[:, q * 128:(q + 1) * 128], lhsT=kaT[:, h, kt * 128:(kt + 1) * 128],
                                                        rhs=qaT[:, h, j * 128:(j + 1) * 128], start=True, stop=True)
                            return inst
                        S.pe(fs, reads=[kaTb, qaTb], writes=[PSB[pst]])
                        yield
                        pt, ptb = ptr.next()
                        S.act(lambda e, pt=pt, n=n: nc.scalar.activation(out=pt[:, 0:n * 128], in_=psf(pst)[:, 0:n * 128], func=AF.Exp),
                              reads=[PSB[pst]], writes=[ptb])
                        yield
                        pm, pmb = ptmr.next()
                        S.pool(lambda e, pm=pm, pt=pt, g=g, n=n: nc.gpsimd.tensor_tensor(out=pm[:, 0:n * 128], in0=pt[:, 0:n * 128],
                                                                in1=mT[:, 4 * g:4 * g + n, :].rearrange("p k t -> p (k t)"),
                                                                op=ALU.mult), reads=[ptb, mTb], writes=[pmb])
                        yield

                        def fo(e, kts=kts, pm=pm, ov=ov, h=h, j=j):
                            inst = None
                            for q, kt in enumerate(kts):
                                inst = nc.tensor.matmul(ov, lhsT=pm[:, q * 128:(q + 1) * 128], rhs=va[:, kt, h, 0:129],
                                                        start=(kt == 0), stop=(kt == j))
                            return inst
                        S.pe(fo, reads=[pmb, vab], writes=[PSB[ppv]])
                        yield
                    rz, rzb = rzr.next()
                    S.dve(lambda e, rz=rz, oc=oc: nc.vector.reciprocal(out=rz[:, 0:1], in_=psf(ppv)[:, oc + 128:oc + 129]),
                          reads=[PSB[ppv]], writes=[rzb])
                    yield
                    S.act(lambda e, oa=oa, h=h, oc=oc, rz=rz: nc.scalar.activation(out=oa[:, h * 128:(h + 1) * 128], in_=psf(ppv)[:, oc:oc + 128],
                                                         func=AF.Identity, scale=rz[:, 0:1]),
                          reads=[PSB[ppv], rzb], writes=[oab])
                    yield
                ptv = psh(pst)
                transposes([(ptv[:, h * 128:(h + 1) * 128], oa[:, h * 128:(h + 1) * 128]) for h in range(4)], ident,
                           [oab, constb], [PSB[pst]])
                yield
                S.act(lambda e, j=j, ptv=ptv: nc.scalar.copy(out=oT[:, 0:4, j * 128:(j + 1) * 128],
                                               in_=ptv[:, 0:512].rearrange("p (h t) -> p h t", h=4)),
                      reads=[PSB[pst]], writes=[oTb[j]])
                yield

        run_streams([dsa_stream(0), dsa_stream(1)])
        S.flush()
        A.release(m)

    def gla(s):
        m = A.mark()
        stage_state["pool"] = mk_stage()
        wq = A.alloc([128, 8, 256], BF16)
        wk = A.alloc([128, 8, 256], BF16)
        wv = A.alloc([128, 8, 512], BF16)
        wl = A.alloc([128, 8, 16], BF16)
        wo_ = A.alloc([128, 8, 512], BF16)
        wb_ = Buf()
        gc = gcol[:, 0, :]
        load_w(w_in_ab[:, 2120:2376], 8, 256, wq, wb_, gc)
        load_w(w_in_ab[:, 2376:2632], 8, 256, wk, wb_, gc)
        load_w(w_in_ab[:, 2632:3144], 8, 512, wv, wb_, gc)
        load_w(w_in_ab[:, 3144:3160], 8, 16, wl, wb_, gc)
        load_w(w_in_ab[:, 3160:3672], 8, 512, wo_, wb_, gc)
        glr = [(A.alloc([32, 64], F32), Buf()) for _ in range(2)]
        for gl_, glb in glr:
            S.dve(lambda e, gl_=gl_: nc.vector.memset(gl_, 1.0), writes=[glb])
        spr = Rot(A, 2, [64, 256], F32)
        ebr = Rot(A, 2, [128, 2, 2, 64], F32)
        ebvr = Rot(A, 2, [64, 256], F32)
        qer = Rot(A, 2, [128, 2, 2, 64], BF16)
        for q_, qb_ in qer.items:
            S.dve(lambda e, q_=q_: nc.vector.memset(q_, 0.0), writes=[qb_])
        kdr = Rot(A, 2, [128, 2, 64], BF16)
        klr = Rot(A, 2, [64, 256], BF16)
        vcr = Rot(A, 2, [64, 512], BF16)
        sgr = Rot(A, 2, [64, 512], F32)
        gvr = Rot(A, 2, [64, 512], F32)
        atr = Rot(A, 2, [64, 4, 64], BF16)
        sqr = Rot(A, 2, [64, 512], F32)
        ssr = Rot(A, 2, [64, 3, 4], F32)
        onr = Rot(A, 2, [64, 512], F32)
        obr = Rot(A, 2, [64, 512], BF16)
        Sf = A.alloc([128, 2, 128], F32)
        Sfb = [Buf(), Buf()]
        Sbf = [(A.alloc([128, 2, 128], BF16), [Buf(), Buf()]) for _ in range(2)]
        def gla_prep(c, F):
            t0 = c * 64
            ntb = nTb[t0 // 128]
            gl_, glb = glr[c % 2]
            mm_group(psf(0)[0:16, 0:64], [(wl[:, kc, 0:16], nT[:, kc, t0:t0 + 64]) for kc in range(8)], [ntb, wb_], [PSB[0]])
            yield
            S.act(lambda e, gl_=gl_: nc.scalar.copy(out=gl_[0:16, :], in_=psf(0)[0:16, 0:64]), reads=[PSB[0]], writes=[glb])
            yield
            mm_group(psf(0)[0:64, 256:512], [(gl_[0:17, 0:64], wg[0:17, :])], [glb, constb], [PSB[0]])
            yield
            sp, spb = spr.next()
            S.act(lambda e, sp=sp: nc.scalar.activation(out=sp, in_=psf(0)[0:64, 256:512], func=AF.Exp, scale=-1.0),
                  reads=[PSB[0]], writes=[spb])
            yield
            S.act(lambda e, sp=sp: nc.scalar.activation(out=sp, in_=sp, func=AF.Ln, bias=1.0), reads=[spb], writes=[spb])
            yield
            def fb(e, sp=sp):
                nc.tensor.matmul(psf(1)[:, 0:64], lhsT=sp[:, 0:128], rhs=tri_incl, start=True, stop=True)
                nc.tensor.matmul(psf(1)[:, 64:128], lhsT=sp[:, 128:256], rhs=tri_incl, start=True, stop=True)
                return nc.tensor.matmul(psf(1)[0:64, 128:384], lhsT=tri_rev, rhs=sp, start=True, stop=True)
            S.pe(fb, reads=[spb, constb], writes=[PSB[1]])
            yield
            eb, ebb = ebr.next()
            ebv, ebvb = ebvr.next()

            def fe(e, eb=eb, ebv=ebv):
                bv = psf(1)[:, 0:128].rearrange("p (g t) -> p g t", g=2)
                nc.scalar.activation(out=eb[:, 0], in_=bv, func=AF.Exp)
                nc.scalar.activation(out=eb[:, 1], in_=bv, func=AF.Exp, scale=-1.0)
                return nc.scalar.activation(out=ebv, in_=psf(1)[0:64, 128:384], func=AF.Exp)
            S.act(fe, reads=[PSB[1]], writes=[ebb, ebvb])
            yield
            def fqk(e, t0=t0):
                inst = None
                for g in range(2):
                    for kc in range(8):
                        nc.tensor.matmul(psf(2)[:, g * 64:(g + 1) * 64], lhsT=wq[:, kc, g * 128:(g + 1) * 128],
                                         rhs=nT[:, kc, t0:t0 + 64], start=(kc == 0), stop=(kc == 7))
                for g in range(2):
                    for kc in range(8):
                        nc.tensor.matmul(psf(2)[:, 128 + g * 64:128 + (g + 1) * 64], lhsT=wk[:, kc, g * 128:(g + 1) * 128],
                                         rhs=nT[:, kc, t0:t0 + 64], start=(kc == 0), stop=(kc == 7))
                for kc in range(8):
                    inst = nc.tensor.matmul(psf(2)[0:64, 256:512], lhsT=nT[:, kc, t0:t0 + 64], rhs=wk[:, kc, :],
                                            start=(kc == 0), stop=(kc == 7))
                return inst
            S.pe(fqk, reads=[ntb, wb_], writes=[PSB[2]])
            yield
            qe, qeb = qer.next()
            kd, kdb = kdr.next()
            kl, klb = klr.next()

            def fq(e, qe=qe, kd=kd, kl=kl, eb=eb, ebv=ebv):
                for hh in range(2):
                    p0 = hh * 64
                    nc.vector.scalar_tensor_tensor(out=qe[p0:p0 + 64, :, hh, :],
                                                   in0=psf(2)[p0:p0 + 64, 0:128].rearrange("p (g t) -> p g t", g=2),
                                                   scalar=0.125, in1=eb[p0:p0 + 64, 0], op0=ALU.mult, op1=ALU.mult)
                nc.vector.tensor_tensor(out=kd, in0=psf(2)[:, 128:256].rearrange("p (g t) -> p g t", g=2), in1=eb[:, 1],
                                        op=ALU.mult)
                return nc.vector.tensor_tensor(out=kl, in0=psf(2)[0:64, 256:512], in1=ebv, op=ALU.mult)
            S.dve(fq, reads=[PSB[2], ebb, ebvb], writes=[qeb, kdb, klb])
            yield
            mm_group(psf(3)[0:64, :], [(nT[:, kc, t0:t0 + 64], wv[:, kc, :]) for kc in range(8)], [ntb, wb_], [PSB[3]])
            yield
            vc, vcb = vcr.next()
            S.act(lambda e, vc=vc: nc.scalar.copy(out=vc, in_=psf(3)[0:64, :]), reads=[PSB[3]], writes=[vcb])
            yield
            mm_group(psf(4)[0:64, :], [(nT[:, kc, t0:t0 + 64], wo_[:, kc, :]) for kc in range(8)], [ntb, wb_], [PSB[4]])
            yield
            sg, sgb = sgr.next()

            def fsg(e, sg=sg):
                nc.scalar.activation(out=sg, in_=psf(4)[0:64, :], func=AF.Exp, scale=-1.0)
                nc.scalar.activation(out=sg, in_=sg, func=AF.Ln, bias=1.0)
                return nc.scalar.activation(out=sg, in_=sg, func=AF.Exp, scale=-1.0)
            S.act(fsg, reads=[PSB[4]], writes=[sgb])
            yield
            gv, gvb = gvr.next()
            S.dve(lambda e, gv=gv, sg=sg: nc.vector.tensor_tensor(out=gv, in0=psf(4)[0:64, :], in1=sg, op=ALU.mult),
                  reads=[PSB[4], sgb], writes=[gvb])
            yield
            F.update(dict(qe=qe, qeb=qeb, kd=kd, kdb=kdb, kl=kl, klb=klb, vc=vc, vcb=vcb, gv=gv, gvb=gvb, eb=eb, ebb=ebb))
            yield

        def gla_scan(c, F):
            t0 = c * 64
            qe, qeb, kd, kdb, kl, klb = F['qe'], F['qeb'], F['kd'], F['kdb'], F['kl'], F['klb']
            vc, vcb, gv, gvb, eb, ebb = F['vc'], F['vcb'], F['gv'], F['gvb'], F['eb'], F['ebb']
            def fat(e, kd=kd, qe=qe):
                inst = None
                for g in range(2):
                    for hh in range(2):
                        p0 = hh * 64
                        q = g * 2 + hh
                        inst = nc.tensor.matmul(psf(5)[0:64, q * 64:(q + 1) * 64], lhsT=kd[:, g, :],
                                                rhs=qe[:, g, hh, :], start=True, stop=True)
                return inst
            S.pe(fat, reads=[kdb, qeb], writes=[PSB[5]])
            yield
            at, atb = atr.next()
            S.dve(lambda e, at=at: nc.vector.tensor_tensor(
                out=at, in0=psf(5)[0:64, 0:256].rearrange("p (q t) -> p q t", q=4),
                in1=causT.unsqueeze(1).to_broadcast([64, 4, 64]), op=ALU.mult), reads=[PSB[5], constb], writes=[atb])
            yield
            sbf_prev, sbfb_prev = Sbf[(c + 1) % 2]
            sbf_cur, sbfb_cur = Sbf[c % 2]

            def fo(e, qe=qe, at=at, vc=vc, c=c, sbf_prev=sbf_prev):
                inst = None
                for g in range(2):
                    for hh in range(2):
                        p0 = hh * 64
                        q = g * 2 + hh
                        ov = psf(6)[0:64, q * 128:(q + 1) * 128]
                        if c > 0:
                            nc.tensor.matmul(ov, lhsT=qe[:, g, hh, :], rhs=sbf_prev[:, g, :], start=True, stop=False)
                        inst = nc.tensor.matmul(ov, lhsT=at[:, q, :], rhs=vc[:, q * 128:(q + 1) * 128], start=(c == 0), stop=True)
                return inst
            S.pe(fo, reads=[qeb, atb, vcb] + (sbfb_prev if c > 0 else []), writes=[PSB[6]])
            yield
            if c < NCH - 1:
                def fu(e, kl=kl, vc=vc):
                    nc.tensor.matmul(psf(7)[:, 0:256], lhsT=kl[:, 0:128], rhs=vc[:, 0:256], start=True, stop=True)
                    return nc.tensor.matmul(psf(7)[:, 256:512], lhsT=kl[:, 128:256], rhs=vc[:, 256:512], start=True, stop=True)
                S.pe(fu, reads=[klb, vcb], writes=[PSB[7]])
                for g in range(2):
                    def fs_(e, g=g, eb=eb, c=c):
                        inst = None
                        for hh in range(2):
                            p0 = hh * 64
                            uv = psf(7)[p0:p0 + 64, g * 256 + hh * 128:g * 256 + (hh + 1) * 128]
                            if c == 0:
                                inst = nc.vector.tensor_copy(out=Sf[p0:p0 + 64, g, :], in_=uv)
                            else:
                                inst = nc.vector.scalar_tensor_tensor(out=Sf[p0:p0 + 64, g, :], in0=Sf[p0:p0 + 64, g, :],
                                                                      scalar=eb[p0:p0 + 64, 0, g, 63:64], in1=uv,
                                                                      op0=ALU.mult, op1=ALU.add)
                        return inst
                    S.dve(fs_, reads=[PSB[7], ebb, Sfb[g]], writes=[Sfb[g]])
                    S.act(lambda e, g=g, sbf_cur=sbf_cur: nc.scalar.copy(out=sbf_cur[:, g, :], in_=Sf[:, g, :]),
                          reads=[Sfb[g]], writes=[sbfb_cur[g]])
            sq, sqb = sqr.next()
            ss, ssb = ssr.next()
            S.act(lambda e, sq=sq: nc.scalar.activation(out=sq, in_=psf(6)[0:64, :], func=AF.Square), reads=[PSB[6]], writes=[sqb])
            yield
            S.dve(lambda e, sq=sq, ss=ss: nc.vector.tensor_reduce(out=ss[:, 0, :], in_=sq.rearrange("p (h d) -> p h d", h=4),
                                                                  axis=AX.X, op=ALU.add), reads=[sqb], writes=[ssb])
            yield
            rstd_from_ss(ss, ssb, 4, 1.0 / 128, True)
            yield
            on, onb = onr.next()

            def fn_(e, on=on, ss=ss):
                inst = None
                for h in range(4):
                    inst = nc.vector.scalar_tensor_tensor(out=on[:, h * 128:(h + 1) * 128], in0=psf(6)[0:64, h * 128:(h + 1) * 128],
                                                          scalar=ss[:, 2, h:h + 1], in1=gains[0:64, 4, :], op0=ALU.mult,
                                                          op1=ALU.mult)
                return inst
            S.dve(fn_, reads=[PSB[6], ssb, constb], writes=[onb])
            yield
            ob, obb = obr.next()
            S.dve(lambda e, ob=ob, on=on, gv=gv: nc.vector.tensor_tensor(out=ob, in0=on, in1=gv, op=ALU.mult),
                  reads=[onb, gvb], writes=[obb])
            yield
            ptv = psh(5)
            transposes([(ptv[:, 512 + h * 64:512 + (h + 1) * 64], ob[:, h * 128:(h + 1) * 128]) for h in range(4)],
                       ident[0:64, 0:64], [obb, constb], [PSB[5]])
            yield
            S.act(lambda e, t0=t0, ptv=ptv: nc.scalar.copy(out=oT[:, 4:8, t0:t0 + 64],
                                                           in_=ptv[:, 512:768].rearrange("p (h t) -> p h t", h=4)),
                  reads=[PSB[5]], writes=[oTb[t0 // 128]])
            yield

        NCH = SEQ // 64
        Fs = [dict() for _ in range(NCH)]
        run_streams([gla_prep(0, Fs[0])])
        for c in range(NCH):
            gens = [gla_scan(c, Fs[c])]
            if c + 1 < NCH:
                gens.insert(0, gla_prep(c + 1, Fs[c + 1]))
            run_streams(gens)
        S.flush()
        A.release(m)

    def sb_attn(s):
        m = A.mark()
        qT = A.alloc([128, 4, SEQ], BF16)
        kT = A.alloc([128, 4, SEQ], BF16)
        v = A.alloc([128, NT, 512], BF16)
        qTb, kTb, vb_ = Buf(), Buf(), Buf()
        gc = gcol[:, 1, :]
        for hg in range(2):
            mp = A.mark()
            stage_state["pool"] = mk_stage()
            wbp = [(A.alloc([128, 8, 512], BF16), Buf()) for _ in range(2)]
            wss = [mk_qk_ws(5 + k) for k in range(NPS)]
            for bi, (nm, c0) in enumerate((("q", hg * 512), ("k", 1024 + hg * 512), ("v", 2048 + hg * 512))):
                wb, wbb = wbp[bi % 2]
                load_w(w_in_c[:, c0:c0 + 512], 8, 512, wb, wbb, gc)
                if nm == "q":
                    proj_streamed(wb, wbb, 512, lambda pi, i, k: qk_post(pi, i, wss[k], 2, qT, qTb, 128 ** -0.5, False))
                elif nm == "k":
                    proj_streamed(wb, wbb, 512, lambda pi, i, k: qk_post(pi, i, wss[k], 3, kT, kTb, 1.0, False))
                else:
                    def hv(pi, i, k):
                        S.act(lambda e: nc.scalar.copy(out=v[:, i, :], in_=psf(pi)), reads=[PSB[pi]], writes=[vb_])
                        yield
                    proj_streamed(wb, wbb, 512, hv)
            S.flush()
            A.release(mp)
            def sb_stream(st_, heads, hg=hg):
                pz, pc, po = 0 + st_, 2 + st_, 4 + st_
                espr = Rot(A, 2, [128, 512], F32)
                lor = Rot(A, 2, [128, 512], BF16)
                ar = Rot(A, 2, [128, 512], BF16)
                lbs = [(A.alloc([128, 512], BF16), Buf()) for _ in range(3)]
                gs = [0]

                def S1(f):
                    h, qb, kt, step, nsteps = f["h"], f["qb"], f["kt"], f["step"], f["nsteps"]
                    cc, ncol, diag = f["cc"], f["ncol"], f["diag"]
                    q0 = qb * 512 + cc
                    mm_group(psf(pz)[:, 0:ncol], [(kT[:, h, kt * 128:(kt + 1) * 128], qT[:, h, q0:q0 + ncol])],
                             [kTb, qTb], [PSB[pz]])
                    yield
                    es, esb = espr.next()
                    f["es"], f["esb"] = es, esb
                    S.act(lambda e: nc.scalar.activation(out=es[:, 0:ncol], in_=psf(pz)[:, 0:ncol], func=AF.Exp, scale=-1.0),
                          reads=[PSB[pz]], writes=[esb])
                    yield
                    S.act(lambda e: nc.scalar.activation(out=es[:, 0:ncol], in_=es[:, 0:ncol], func=AF.Ln, bias=1.0),
                          reads=[esb], writes=[esb])
                    yield
                    lo, lob = lor.next()
                    f["lo"], f["lob"] = lo, lob
                    S.dve(lambda e: nc.vector.scalar_tensor_tensor(out=lo[:, 0:ncol], in0=psf(pz)[:, 0:ncol], scalar=-1.0,
                                                                   in1=es[:, 0:ncol], op0=ALU.mult, op1=ALU.subtract),
                          reads=[PSB[pz], esb], writes=[lob])
                    yield
                    if diag:
                        S.pool(lambda e: nc.gpsimd.tensor_tensor(out=lo[:, 0:128], in0=lo[:, 0:128], in1=strictT, op=ALU.mult),
                               reads=[lob, constb], writes=[lob])
                        yield
                    g = gs[0]
                    gs[0] += 1
                    f["lbc"] = lbs[g % 3]
                    if step < nsteps - 1:
                        lbn, lbnb = lbs[(g + 1) % 3]
                        lbc, lbcb = lbs[g % 3]
                        ccn = f["cc_next"]
                        if ccn < cc:
                            S.pool(lambda e: nc.gpsimd.memset(lbn[:, ccn:cc], 0.0), writes=[lbnb])
                            yield
                        if step == 0:
                            S.dve(lambda e: nc.vector.tensor_copy(out=lbn[:, cc:512], in_=lo[:, 0:ncol]), reads=[lob], writes=[lbnb])
                        else:
                            S.dve(lambda e: nc.vector.tensor_tensor(out=lbn[:, cc:512], in0=lbc[:, cc:512], in1=lo[:, 0:ncol],
                                                                    op=ALU.add), reads=[lob, lbcb], writes=[lbnb])
                        yield

                def S2(f):
                    h, qb, kt, step, nsteps = f["h"], f["qb"], f["kt"], f["step"], f["nsteps"]
                    cc, ncol, diag = f["cc"], f["ncol"], f["diag"]
                    es, esb, lo, lob = f["es"], f["esb"], f["lo"], f["lob"]
                    lbc, lbcb = f["lbc"]
                    if step == 0:
                        mm_group(psf(pc)[:, 0:ncol], [(triU, lo[:, 0:ncol])], [lob, constb], [PSB[pc]])
                    else:
                        mm_group(psf(pc)[:, 0:ncol], [(triU, lo[:, 0:ncol]), (ones128, lbc[:, cc:512])],
                                 [lob, lbcb, constb], [PSB[pc]])
                    yield
                    S.dve(lambda e: nc.vector.tensor_tensor(out=es[:, 0:ncol], in0=psf(pc)[:, 0:ncol], in1=es[:, 0:ncol],
                                                            op=ALU.subtract), reads=[PSB[pc], esb], writes=[esb])
                    yield
                    a_, ab_ = ar.next()
                    S.act(lambda e: nc.scalar.activation(out=a_[:, 0:ncol], in_=es[:, 0:ncol], func=AF.Exp),
                          reads=[esb], writes=[ab_])
                    yield
                    if diag:
                        S.pool(lambda e: nc.gpsimd.tensor_tensor(out=a_[:, 0:128], in0=a_[:, 0:128], in1=strictT, op=ALU.mult),
                               reads=[ab_, constb], writes=[ab_])
                        yield
                    S.pe(lambda e: nc.tensor.matmul(psf(po)[:, cc:512], lhsT=v[:, kt, h * 128:(h + 1) * 128], rhs=a_[:, 0:ncol],
                                                    start=(step == 0), stop=(step == nsteps - 1)),
                         reads=[vb_, ab_], writes=[PSB[po]])
                    yield
                    if step == nsteps - 1:
                        hh = hg * 4 + h
                        S.act(lambda e: nc.scalar.copy(out=oT[:, hh, qb * 512:(qb + 1) * 512], in_=psf(po)),
                              reads=[PSB[po]], writes=oTb[4 * qb:4 * qb + 4])
                        yield

                pending = None
                for h in heads:
                    for qb in range(4):
                        kts = list(range(4 * qb + 3, -1, -1))
                        ccs = [max(0, kt - 4 * qb) * 128 for kt in kts]
                        for step, kt in enumerate(kts):
                            f = {"h": h, "qb": qb, "kt": kt, "step": step, "nsteps": len(kts), "cc": ccs[step],
                                 "ncol": 512 - ccs[step], "diag": kt >= 4 * qb,
                                 "cc_next": ccs[step + 1] if step + 1 < len(kts) else 0}
                            yield from S1(f)
                            if pending is not None:
                                yield from S2(pending)
                            pending = f
                yield from S2(pending)

            run_streams([sb_stream(0, [0, 1]), sb_stream(1, [2, 3])])
            S.flush()
            A.release(mp)
        A.release(m)

    def dump_h(s):
        m = A.mark()
        t = [(A.alloc([128, 1024], F32), Buf(), f"xin{k}") for k in range(2)]
        for i in range(NT):
            tt, tb, ch = t[i % 2]
            S.dma(lambda e, tt=tt, i=i: nc.sync.dma_start(out=tt, in_=h_scr[s, i * 128:(i + 1) * 128, :]), ch,
                  reads=[hb[s][i]], writes=[tb])
            S.dma(lambda e, tt=tt, i=i: nc.sync.dma_start(out=out[s, i * 128:(i + 1) * 128, :], in_=tt), f"st{i % 2}",
                  reads=[tb], writes=[])
        S.flush()
        A.release(m)

    for s in range(NSEQ):
        if stop != "const":
            phase_norm(s, x)
        if stop == "norm":
            continue
        if stop == "const":
            continue
        dsa(s)
        if stop == "dsa":
            continue
        phase_norm(s, x)
        gla(s)
        if stop == "gla":
            continue
        out_proj_and_norm(s, w_out_ab, x, h_scr)
        if stop == "mix0":
            dump_h(s)
            continue
        ffn(s, 0, False)
        if stop == "ffn0":
            dump_h(s)
            continue
        phase_norm(s, h_scr)
        sb_attn(s)
        out_proj_and_norm(s, w_out_c, h_scr, h_scr)
        if stop == "mix1":
            dump_h(s)
            continue
        ffn(s, 1, True)
    S.flush(final=True)
    return nc


def host_constants():
    c = {}
    c["c_ident"] = np.eye(128, dtype=np.float32)
    pos = np.arange(SEQ, dtype=np.float32)
    inv_a = np.power(np.float32(500000.0), -np.arange(16, dtype=np.float32) * 2.0 / 32).astype(np.float32)
    ang = pos[:, None] * inv_a[None, :]
    c["c_rope_a"] = np.concatenate([np.cos(ang), np.sin(ang)], axis=1).astype(np.float32)
    inv_i = np.power(np.float32(500000.0), -np.arange(8, dtype=np.float32) * 2.0 / 16).astype(np.float32)
    ang = pos[:, None] * inv_i[None, :]
    c["c_rope_i"] = np.concatenate([np.cos(ang), np.sin(ang)], axis=1).astype(np.float32)
    a = np.arange(64)
    m64 = np.zeros((64, 3, 64), np.float32)
    m64[:, 0, :] = (a[:, None] <= a[None, :]) * (-1.0 / 16.0)
    m64[:, 1, :] = (a[:, None] > a[None, :]) * (-1.0 / 16.0)
    m64[:, 2, :] = (a[:, None] <= a[None, :]) * 1.0
    c["c_m64"] = m64
    b = np.arange(128)
    m128 = np.zeros((128, 3, 128), np.float32)
    m128[:, 0, :] = (b[:, None] > b[None, :]) * 1.0
    m128[:, 1, :] = 1.0
    m128[:, 2, :] = (b[:, None] < b[None, :]) * 1.0
    c["c_m128"] = m128
    return c


_CACHE = {}


def kernel(x, g_mix, g_ffn, w_in_ab, gq_a, gk_a, w_gate_up, b_gate, g_gla, w_out_ab,
           w_in_c, gq_c, gk_c, w_out_c, w_up, w_down, _ncores=8, _stop=None):
    f = lambda a: np.ascontiguousarray(np.asarray(a, dtype=np.float32))
    x = f(x)
    nseq = x.shape[0] // _ncores
    shared = {
        "w_in_ab": f(w_in_ab)[0], "w_gate_up": f(w_gate_up)[0], "b_gate": f(b_gate), "w_out_ab": f(w_out_ab)[0],
        "w_in_c": f(w_in_c)[0], "w_out_c": f(w_out_c)[0], "w_up": f(w_up), "w_down": f(w_down),
    }
    gains = np.stack([np.broadcast_to(f(g).reshape(1, 128), (128, 128)) for g in (gq_a, gk_a, gq_c, gk_c, g_gla)], axis=1)
    shared["c_gains"] = np.ascontiguousarray(gains, dtype=np.float32)
    gcols = np.stack([f(g_mix)[0].reshape(8, 128).T, f(g_mix)[1].reshape(8, 128).T,
                      f(g_ffn)[0].reshape(8, 128).T, f(g_ffn)[1].reshape(8, 128).T], axis=1)
    shared["c_gcol"] = np.ascontiguousarray(gcols, dtype=np.float32)
    shared.update(host_constants())
    key = (nseq, _stop)
    if key not in _CACHE:
        _CACHE[key] = build_program(nseq, _stop)
    nc = _CACHE[key]
    in_maps = []
    for c in range(_ncores):
        d = dict(shared)
        d["x"] = np.ascontiguousarray(x[c * nseq:(c + 1) * nseq])
        in_maps.append(d)
    res = run_bass_kernel_spmd(nc, in_maps, core_ids=list(range(_ncores)))
    return np.concatenate([np.asarray(r["out"], dtype=np.float32) for r in res.results], axis=0)
```

```python
import numpy as np
import concourse.bass as bass
import concourse.mybir as mybir
from concourse.bass_utils import run_bass_kernel_spmd
from concourse.alu_op_type import AluOpType as ALU

F32 = mybir.dt.float32
BF16 = mybir.dt.bfloat16
AF = mybir.ActivationFunctionType
AX = mybir.AxisListType

SEQ = 2048
DM = 1024
NT = SEQ // 128
EPS = 1e-6
ABW = 3672
NEG = -1.0e30
BIS_ITERS = 12
ARENA_BYTES = 175 * 1024

_ENG_ATTR = {"pe": "tensor", "act": "scalar", "dve": "vector", "pool": "gpsimd", "sp": "sync"}


class Buf:
    __slots__ = ("lw", "rd")

    def __init__(self):
        self.lw = None
        self.rd = {}


class Sched:
    ENG = ("pe", "act", "dve", "pool", "sp")

    def __init__(self, nc):
        self.nc = nc
        self.sem = {e: nc.alloc_semaphore("sem_" + e) for e in self.ENG}
        self.cnt = {e: 0 for e in self.ENG}
        self.ops = {e: [] for e in self.ENG}
        self.waited = {e: {} for e in self.ENG}
        self.chan = {}

    def channel(self, name):
        if name not in self.chan:
            self.chan[name] = [self.nc.alloc_semaphore("ch_" + name), 0]
        return name

    def _deps(self, reads, writes):
        d = {}
        for b in reads:
            if b.lw is not None and d.get(b.lw[0], 0) < b.lw[1]:
                d[b.lw[0]] = b.lw[1]
        for b in writes:
            if b.lw is not None and d.get(b.lw[0], 0) < b.lw[1]:
                d[b.lw[0]] = b.lw[1]
            for k, v in b.rd.items():
                if d.get(k, 0) < v:
                    d[k] = v
        return d

    def op(self, eng, fn, reads=(), writes=()):
        d = self._deps(reads, writes)
        self.cnt[eng] += 1
        idx = self.cnt[eng]
        for b in reads:
            b.rd[eng] = idx
        for b in writes:
            b.lw = (eng, idx)
            b.rd = {}
        self.ops[eng].append((d, fn, None))

    def pe(self, fn, reads=(), writes=()):
        self.op("pe", fn, reads, writes)

    def act(self, fn, reads=(), writes=()):
        self.op("act", fn, reads, writes)

    def dve(self, fn, reads=(), writes=()):
        self.op("dve", fn, reads, writes)

    def pool(self, fn, reads=(), writes=()):
        self.op("pool", fn, reads, writes)

    def dma(self, fn, chan, reads=(), writes=(), queue="sp"):
        d = self._deps(reads, writes)
        c = self.chan[chan]
        c[1] += 16
        key = "ch:" + chan
        for b in reads:
            b.rd[key] = c[1]
        for b in writes:
            b.lw = (key, c[1])
            b.rd = {}
        self.ops[queue].append((d, fn, chan))

    def _semof(self, k):
        if k.startswith("ch:"):
            return self.chan[k[3:]][0]
        return self.sem[k]

    def flush(self, final=False):
        nc = self.nc
        if final:
            d = {}
            for name, c in self.chan.items():
                if c[1] > 0:
                    d["ch:" + name] = c[1]
            self.ops["sp"].append((d, None, None))
        with nc.Block() as blk:
            for eng in self.ENG:
                ops = self.ops[eng]

                def body(e, eng=eng, ops=ops):
                    w = self.waited[eng]
                    for d, fn, chan in ops:
                        for k, v in d.items():
                            if k == eng and eng == "pe":
                                continue
                            if w.get(k, 0) < v:
                                e.wait_ge(self._semof(k), v)
                                w[k] = v
                        if fn is None:
                            continue
                        inst = fn(e)
                        if chan is None:
                            inst.then_inc(self.sem[eng], 1)
                        else:
                            inst.then_inc(self.chan[chan][0], 16)

                getattr(blk, _ENG_ATTR[eng])(body)
        self.ops = {e: [] for e in self.ENG}


class Arena:
    def __init__(self, nc, nbytes, base=None):
        self.t = nc.alloc_sbuf_tensor("arena", [128, nbytes // 4], F32) if base is None else base
        self.top = 0
        self.nbytes = nbytes

    def mark(self):
        return self.top

    def release(self, m):
        self.top = m

    def alloc(self, shape, dtype):
        esz = 4 if dtype == F32 else 2
        n = int(np.prod(shape[1:]))
        nb = (n * esz + 63) // 64 * 64
        off = self.top
        self.top += nb
        assert self.top <= self.nbytes, ("arena overflow", self.top)
        ap = self.t[0:shape[0], off // 4:(off + nb) // 4]
        if dtype != F32:
            ap = ap.bitcast(dtype)
        ap = ap[:, 0:n]
        if len(shape) == 3:
            ap = ap.rearrange("p (a b) -> p a b", a=shape[1])
        elif len(shape) == 4:
            ap = ap.rearrange("p (a b c) -> p a b c", a=shape[1], b=shape[2])
        return ap


class Rot:
    def __init__(self, arena, n, shape, dtype):
        self.items = [(arena.alloc(shape, dtype), Buf()) for _ in range(n)]
        self.i = 0

    def next(self):
        it = self.items[self.i % len(self.items)]
        self.i += 1
        return it


def run_streams(gens):
    gens = list(gens)
    while gens:
        for g in list(gens):
            try:
                next(g)
            except StopIteration:
                gens.remove(g)


def build_program(NSEQ=2, stop=None):
    nc = bass.Bass("TRN2", target_bir_lowering=False)
    S = Sched(nc)

    def dram_in(name, shape):
        return nc.dram_tensor(name, shape, F32, kind="ExternalInput").ap()

    x = dram_in("x", [NSEQ, SEQ, DM])
    w_in_ab = dram_in("w_in_ab", [DM, ABW])
    w_gate_up = dram_in("w_gate_up", [16, 256])
    b_gate = dram_in("b_gate", [1, 256])
    w_out_ab = dram_in("w_out_ab", [DM, DM])
    w_in_c = dram_in("w_in_c", [DM, 3 * DM])
    w_out_c = dram_in("w_out_c", [DM, DM])
    w_up = dram_in("w_up", [2, DM, 4 * DM])
    w_down = dram_in("w_down", [2, 4 * DM, DM])
    c_ident = dram_in("c_ident", [128, 128])
    c_rope_a = dram_in("c_rope_a", [SEQ, 32])
    c_rope_i = dram_in("c_rope_i", [SEQ, 16])
    c_m64 = dram_in("c_m64", [64, 3, 64])
    c_m128 = dram_in("c_m128", [128, 3, 128])
    c_gains = dram_in("c_gains", [128, 5, 128])
    c_gcol = dram_in("c_gcol", [128, 4, 8])
    out = nc.dram_tensor("out", [NSEQ, SEQ, DM], F32, kind="ExternalOutput").ap()
    h_scr = nc.dram_tensor("h_scr", [NSEQ, SEQ, DM], F32).ap()
    hb = [[Buf() for _ in range(NT)] for _ in range(NSEQ)]

    A = Arena(nc, ARENA_BYTES)
    PS = [nc.alloc_psum_tensor(f"psb{i}", [128, 512], F32) for i in range(8)]
    PSB = [Buf() for _ in range(8)]

    def psf(i):
        return PS[i][:, :]

    def psh(i):
        return PS[i][:, :].bitcast(BF16)

    for nm in ("c0", "c1", "c2", "c3", "xin0", "xin1", "xin2", "xin3", "st0", "st1", "st2", "st3", "stg0", "stg1", "hres"):
        S.channel(nm)

    nT_raw = A.alloc([128, 8192], F32)
    nT = nT_raw.bitcast(BF16).rearrange("p (a b) -> p a b", a=8)
    nTb = [Buf() for _ in range(NT)]
    oT_raw = A.alloc([128, 8192], F32)
    oT = oT_raw.bitcast(BF16).rearrange("p (a b) -> p a b", a=8)
    oTb = [Buf() for _ in range(NT)]
    identf = A.alloc([128, 128], F32)
    ident = A.alloc([128, 128], BF16)
    ropeA = A.alloc([128, NT, 32], F32)
    ropeI = A.alloc([128, NT, 16], F32)
    m64 = A.alloc([64, 3, 64], F32)
    m128f = A.alloc([128, 3, 128], F32)
    m128 = A.alloc([128, 3, 128], BF16)
    gains = A.alloc([128, 5, 128], F32)
    gcol = A.alloc([128, 4, 8], F32)
    wg = A.alloc([32, 256], F32)
    thr_all = A.alloc([128, 1], F32)
    constb = Buf()

    def bc_row(ap):
        r = ap.partition_broadcast(128)
        if len(r.shape) == 3:
            r = r.rearrange("p a b -> p (a b)")
        return r

    def dma_simple(out_ap, in_ap, chan, writes, reads=(), ncdma=True):
        S.dma(lambda e: nc.sync.dma_start(out=out_ap, in_=in_ap), chan, reads=reads, writes=writes)

    dma_simple(identf, c_ident, "c0", [constb])
    dma_simple(ropeA, c_rope_a.rearrange("(i p) c -> p i c", p=128), "c0", [constb])
    dma_simple(ropeI, c_rope_i.rearrange("(i p) c -> p i c", p=128), "c0", [constb])
    dma_simple(m64, c_m64, "c0", [constb])
    dma_simple(m128f, c_m128, "c0", [constb])
    dma_simple(gains, c_gains, "c0", [constb])
    dma_simple(wg[0:16, :], w_gate_up, "c0", [constb])
    dma_simple(wg[16:17, :], b_gate, "c0", [constb])
    dma_simple(gcol, c_gcol, "c0", [constb])
    S.dve(lambda e: nc.vector.tensor_copy(out=ident, in_=identf), reads=[constb], writes=[constb])
    S.dve(lambda e: nc.vector.tensor_copy(out=m128, in_=m128f), reads=[constb], writes=[constb])
    S.dve(lambda e: nc.vector.memset(thr_all, -1.0e29), reads=[], writes=[constb])
    S.flush()
    base_mark = A.mark()

    triU = m128[:, 0, :]
    ones128 = m128[:, 1, :]
    strictT = m128[:, 2, :]
    tri_incl = m64[:, 0, :]
    tri_rev = m64[:, 1, :]
    causT = m64[:, 2, :]

    def mk_stage(ar=None):
        ar = A if ar is None else ar
        return [(ar.alloc([128, 2048], F32), Buf(), S.channel(f"stg{i}")) for i in range(2)]

    stage_state = {"pool": None, "i": 0}

    def load_w(src2d, KC, ncols, dst, dstb, gc=None):
        srcv = src2d.rearrange("(k p) c -> p k c", p=128)
        kstep = max(1, 2048 // ncols)
        for k0 in range(0, KC, kstep):
            kn = min(kstep, KC - k0)
            st, stb, ch = stage_state["pool"][stage_state["i"] % 2]
            stage_state["i"] += 1
            stv = st[:, 0:kn * ncols].rearrange("p (k c) -> p k c", k=kn)
            S.dma(lambda e, stv=stv, k0=k0, kn=kn: nc.sync.dma_start(out=stv, in_=srcv[:, k0:k0 + kn, :]),
                  ch, writes=[stb])
            if gc is None:
                S.pool(lambda e, stv=stv, k0=k0, kn=kn: nc.gpsimd.tensor_copy(out=dst[:, k0:k0 + kn, :], in_=stv),
                       reads=[stb], writes=[dstb])
            else:
                S.pool(lambda e, stv=stv, k0=k0, kn=kn: nc.gpsimd.tensor_tensor(
                    out=dst[:, k0:k0 + kn, :], in0=stv,
                    in1=gc[:, k0:k0 + kn].unsqueeze(2).to_broadcast([128, kn, ncols]), op=ALU.mult),
                    reads=[stb, constb], writes=[dstb])

    def mm_group(out_ap, pairs, rd, wr):
        def f(e):
            n = len(pairs)
            inst = None
            for q, (l, r) in enumerate(pairs):
                inst = nc.tensor.matmul(out_ap, lhsT=l, rhs=r, start=(q == 0), stop=(q == n - 1))
            return inst
        S.pe(f, reads=rd, writes=wr)

    def transposes(outs_ins, idn, rd, wr):
        def f(e):
            inst = None
            for o, i_ in outs_ins:
                inst = nc.tensor.transpose(o, i_, idn)
            return inst
        S.pe(f, reads=rd, writes=wr)

    def rstd_from_ss(ssv, ssb, n, inv_n, add_eps):
        P = ssv.shape[0]
        if add_eps:
            S.dve(lambda e: nc.vector.tensor_scalar(out=ssv[:, 0, :], in0=ssv[:, 0, :], scalar1=inv_n, scalar2=EPS,
                                                    op0=ALU.mult, op1=ALU.add), reads=[ssb], writes=[ssb])
            S.act(lambda e: nc.scalar.activation(out=ssv[:, 1, :], in_=ssv[:, 0, :], func=AF.Ln), reads=[ssb], writes=[ssb])
        else:
            S.act(lambda e: nc.scalar.activation(out=ssv[:, 1, :], in_=ssv[:, 0, :], func=AF.Ln, scale=inv_n),
                  reads=[ssb], writes=[ssb])
        S.act(lambda e: nc.scalar.activation(out=ssv[:, 2, :], in_=ssv[:, 1, :], func=AF.Exp, scale=-0.5),
              reads=[ssb], writes=[ssb])

    def rope(xv, xb, H, half, table, i, tmp, tmpb, P=128, prow=None):
        if prow is None:
            cs = table[0:P, i, :]
        else:
            cs = prow
        cos = cs[:, 0:half].unsqueeze(1).to_broadcast([P, H, half])
        sin = cs[:, half:2 * half].unsqueeze(1).to_broadcast([P, H, half])
        x1 = xv[:, :, 0:half]
        x2 = xv[:, :, half:2 * half]

        def f1(e):
            nc.gpsimd.tensor_tensor(out=tmp[:, 0], in0=x1, in1=cos, op=ALU.mult)
            nc.gpsimd.tensor_tensor(out=tmp[:, 1], in0=x2, in1=sin, op=ALU.mult)
            nc.gpsimd.tensor_tensor(out=tmp[:, 2], in0=x2, in1=cos, op=ALU.mult)
            return nc.gpsimd.tensor_tensor(out=tmp[:, 3], in0=x1, in1=sin, op=ALU.mult)
        S.pool(f1, reads=[xb, constb], writes=[tmpb])

        def f2(e):
            nc.gpsimd.tensor_tensor(out=x1, in0=tmp[:, 0], in1=tmp[:, 1], op=ALU.subtract)
            return nc.gpsimd.tensor_tensor(out=x2, in0=tmp[:, 2], in1=tmp[:, 3], op=ALU.add)
        S.pool(f2, reads=[tmpb], writes=[xb])

    def norm_transpose(xt, xtb, i, ws):
        junk, jb = ws["junk"].next()
        ss, sb = ws["ss"].next()
        nb, nbb = ws["nb"].next()
        S.act(lambda e: nc.scalar.activation(out=junk, in_=xt, func=AF.Square, accum_out=ss[:, 0, 0:1]),
              reads=[xtb], writes=[jb, sb])
        yield
        S.dve(lambda e: nc.vector.tensor_scalar(out=ss[:, 0, :], in0=ss[:, 0, :], scalar1=1.0 / DM, scalar2=EPS,
                                                op0=ALU.mult, op1=ALU.add), reads=[sb], writes=[sb])
        yield
        S.act(lambda e: nc.scalar.activation(out=ss[:, 1, :], in_=ss[:, 0, :], func=AF.Ln), reads=[sb], writes=[sb])
        yield
        S.act(lambda e: nc.scalar.activation(out=ss[:, 2, :], in_=ss[:, 1, :], func=AF.Exp, scale=-0.5), reads=[sb], writes=[sb])
        yield
        S.dve(lambda e: nc.vector.tensor_scalar(out=nb, in0=xt, scalar1=ss[:, 2, 0:1], scalar2=None, op0=ALU.mult),
              reads=[xtb, sb], writes=[nbb])
        yield
        pb = ws["psT"]
        pv = psh(pb)
        transposes([(pv[:, kc * 128:(kc + 1) * 128], nb[:, kc * 128:(kc + 1) * 128]) for kc in range(8)], ident,
                   [nbb, constb], [PSB[pb]])
        yield
        S.act(lambda e: nc.scalar.copy(out=nT[:, :, i * 128:(i + 1) * 128],
                                       in_=pv[:, 0:1024].rearrange("p (k t) -> p k t", k=8)),
              reads=[PSB[pb]], writes=[nTb[i]])
        yield

    def mk_norm_ws(psT):
        return {"junk": Rot(A, 1, [128, 1024], BF16), "ss": Rot(A, 2, [128, 3, 1], F32),
                "nb": Rot(A, 1, [128, 1024], BF16), "psT": psT}

    def phase_norm(s, src):
        m = A.mark()
        NS = 4
        wsl = [mk_norm_ws(4 + k) for k in range(NS)]
        xin = [(A.alloc([128, 1024], F32), Buf(), S.channel(f"xin{k}")) for k in range(NS)]

        def one(k, i):
            xt, xb, ch = xin[k]
            S.dma(lambda e: nc.sync.dma_start(out=xt, in_=src[s, i * 128:(i + 1) * 128, :]), ch,
                  reads=[hb[s][i]], writes=[xb])
            yield
            yield from norm_transpose(xt, xb, i, wsl[k])

        def stream(k):
            for i in range(k, NT, NS):
                yield from one(k, i)
        run_streams([stream(k) for k in range(NS)])
        S.flush()
        A.release(m)

    def qk_post(ps_i, i, ws, gidx, dstT, dst_b, scale, do_rope):
        sq, sqb = ws["sq"].next()
        ss, ssb = ws["ss4"].next()
        qn, qnb = ws["qn"].next()
        qh, qhb = ws["qh"].next()
        pv = psf(ps_i)
        S.act(lambda e: nc.scalar.activation(out=sq, in_=pv, func=AF.Square), reads=[PSB[ps_i]], writes=[sqb])
        yield
        S.dve(lambda e: nc.vector.tensor_reduce(out=ss[:, 0, :], in_=sq.rearrange("p (h d) -> p h d", h=4), axis=AX.X,
                                                op=ALU.add), reads=[sqb], writes=[ssb])
        yield
        S.dve(lambda e: nc.vector.tensor_scalar(out=ss[:, 0, :], in0=ss[:, 0, :], scalar1=1.0 / 128, scalar2=EPS,
                                                op0=ALU.mult, op1=ALU.add), reads=[ssb], writes=[ssb])
        yield
        S.act(lambda e: nc.scalar.activation(out=ss[:, 1, :], in_=ss[:, 0, :], func=AF.Ln), reads=[ssb], writes=[ssb])
        yield
        S.act(lambda e: nc.scalar.activation(out=ss[:, 2, :], in_=ss[:, 1, :], func=AF.Exp, scale=-0.5),
              reads=[ssb], writes=[ssb])
        yield

        def f(e):
            inst = None
            for h in range(4):
                inst = nc.vector.scalar_tensor_tensor(out=qn[:, h * 128:(h + 1) * 128], in0=pv[:, h * 128:(h + 1) * 128],
                                                      scalar=ss[:, 2, h:h + 1], in1=gains[:, gidx, :], op0=ALU.mult,
                                                      op1=ALU.mult)
            return inst
        S.dve(f, reads=[PSB[ps_i], ssb, constb], writes=[qnb])
        yield
        if do_rope:
            tmp, tmpb = ws["rtmp"].next()
            rope(qn.rearrange("p (h d) -> p h d", h=4), qnb, 4, 16, ropeA, i, tmp, tmpb)
            yield
        S.act(lambda e: nc.scalar.activation(out=qh, in_=qn, func=AF.Copy, scale=scale), reads=[qnb], writes=[qhb])
        yield
        pt = ws["psT"]
        ptv = psh(pt)
        transposes([(ptv[:, h * 128:(h + 1) * 128], qh[:, h * 128:(h + 1) * 128]) for h in range(4)], ident,
                   [qhb, constb], [PSB[pt]])
        yield
        S.act(lambda e: nc.scalar.copy(out=dstT[:, :, i * 128:(i + 1) * 128],
                                       in_=ptv[:, 0:512].rearrange("p (h t) -> p h t", h=4)),
              reads=[PSB[pt]], writes=[dst_b])
        yield

    NPS = 3

    def proj_streamed(wb, wbb, ncols, handler):
        def stream(k):
            for i in range(k, NT, NPS):
                proj_tok(wb, ncols, i, k, wbb)
                yield
                yield from handler(k, i, k)
        run_streams([stream(k) for k in range(NPS)])

    def proj_tok(wb, ncols, i, ps_i, wbb, M=128, t0=None):
        t0 = i * 128 if t0 is None else t0
        mm_group(psf(ps_i)[0:M, 0:ncols], [(nT[:, kc, t0:t0 + M], wb[:, kc, 0:ncols]) for kc in range(8)],
                 [nTb[t0 // 128], wbb], [PSB[ps_i]])

    def mk_qk_ws(psT):
        return {"sq": Rot(A, 1, [128, 512], F32), "ss4": Rot(A, 2, [128, 3, 4], F32), "qn": Rot(A, 1, [128, 512], F32),
                "qh": Rot(A, 1, [128, 512], BF16), "rtmp": Rot(A, 1, [128, 4, 4, 16], F32), "psT": psT}

    def out_proj_and_norm(s, wout_dram, src, dst):
        m = A.mark()
        stage_state["pool"] = mk_stage()
        wo = A.alloc([128, 8, DM], BF16)
        wob = Buf()
        for c0 in range(0, DM, 256):
            load_w(wout_dram[:, c0:c0 + 256], 8, 256, wo[:, :, c0:c0 + 256], wob)
        NS = 4
        wsl = [mk_norm_ws(2 * k) for k in range(NS)]
        hin = [(A.alloc([128, 1024], F32), Buf(), S.channel(f"xin{k}")) for k in range(NS)]
        hnew = [(A.alloc([128, 1024], F32), Buf(), S.channel(f"st{k}")) for k in range(NS)]

        def one(k, i):
            hi_, hib, ch = hin[k]
            hn, hnb, sch = hnew[k]
            S.dma(lambda e: nc.sync.dma_start(out=hi_, in_=src[s, i * 128:(i + 1) * 128, :]), ch,
                  reads=[hb[s][i]], writes=[hib])
            yield
            for half in range(2):
                pi = 2 * k + half
                mm_group(psf(pi), [(oT[:, c, i * 128:(i + 1) * 128], wo[:, c, half * 512:(half + 1) * 512])
                                   for c in range(8)], [oTb[i], wob], [PSB[pi]])
                yield
                S.dve(lambda e, pi=pi, half=half: nc.vector.tensor_tensor(
                    out=hn[:, half * 512:(half + 1) * 512], in0=psf(pi), in1=hi_[:, half * 512:(half + 1) * 512],
                    op=ALU.add), reads=[PSB[pi], hib], writes=[hnb])
                yield
            S.dma(lambda e: nc.sync.dma_start(out=dst[s, i * 128:(i + 1) * 128, :], in_=hn), sch,
                  reads=[hnb], writes=[hb[s][i]])
            yield
            yield from norm_transpose(hn, hnb, i, wsl[k])

        def stream(k):
            for i in range(k, NT, NS):
                yield from one(k, i)
        run_streams([stream(k) for k in range(NS)])
        S.flush()
        A.release(m)

    def ffn(s, layer, final):
        m = A.mark()
        stage_state["pool"] = mk_stage()
        hres = A.alloc([128, NT, DM], F32)
        hresb = [Buf() for _ in range(NT)]
        actT = A.alloc([128, 4, SEQ], BF16)
        actb = [Buf() for _ in range(4)]
        AO = Arena(nc, 32768, base=oT_raw)
        wub = [(AO.alloc([128, 8, 512], BF16), Buf()) for _ in range(2)]
        wdb = [(AO.alloc([128, 4, DM], BF16), Buf()) for _ in range(2)]
        rr = Rot(A, 2, [128, 512], F32)
        gc = gcol[:, 2 + layer, :]
        dst = out if final else h_scr
        for g in range(8):
            wu, wubb = wub[g % 2]
            wd, wdbb = wdb[g % 2]
            load_w(w_up[layer, :, g * 512:(g + 1) * 512], 8, 512, wu, wubb, gc)
            load_w(w_down[layer, g * 512:(g + 1) * 512, :], 4, DM, wd, wdbb)
            if g == 0:
                for i in range(NT):
                    S.dma(lambda e, i=i: nc.sync.dma_start(out=hres[:, i, :], in_=h_scr[s, i * 128:(i + 1) * 128, :]), "hres",
                          reads=[hb[s][i]], writes=[hresb[i]])
            for fc in range(4):
                for tb in range(4):
                    pi = (fc * 4 + tb) % 4
                    mm_group(psf(pi), [(wu[:, kc, fc * 128:(fc + 1) * 128], nT[:, kc, tb * 512:(tb + 1) * 512])
                                       for kc in range(8)], nTb[4 * tb:4 * tb + 4] + [wubb], [PSB[pi]])
                    r, rb = rr.next()
                    S.act(lambda e, r=r, pi=pi: nc.scalar.activation(out=r, in_=psf(pi), func=AF.Relu),
                          reads=[PSB[pi]], writes=[rb])
                    S.pool(lambda e, r=r, fc=fc, tb=tb: nc.gpsimd.tensor_tensor(
                        out=actT[:, fc, tb * 512:(tb + 1) * 512], in0=r, in1=r, op=ALU.mult),
                        reads=[rb], writes=[actb[tb]])
            for ti in range(NT):
                for half in range(2):
                    pi = 4 + (ti * 2 + half) % 4
                    mm_group(psf(pi), [(actT[:, fc, ti * 128:(ti + 1) * 128], wd[:, fc, half * 512:(half + 1) * 512])
                                       for fc in range(4)], [actb[ti // 4], wdbb], [PSB[pi]])
                    S.dve(lambda e, pi=pi, ti=ti, half=half: nc.vector.tensor_tensor(
                        out=hres[:, ti, half * 512:(half + 1) * 512], in0=psf(pi),
                        in1=hres[:, ti, half * 512:(half + 1) * 512], op=ALU.add),
                        reads=[PSB[pi], hresb[ti]], writes=[hresb[ti]])
                if g == 7:
                    S.dma(lambda e, ti=ti: nc.sync.dma_start(out=dst[s, ti * 128:(ti + 1) * 128, :], in_=hres[:, ti, :]),
                          f"st{ti % 4}", reads=[hresb[ti]], writes=[hb[s][ti]])
        S.flush()
        A.release(m)

    def dsa(s):
        m = A.mark()
        kaT = A.alloc([128, 4, SEQ], BF16)
        qaT = A.alloc([128, 4, SEQ], BF16)
        va = A.alloc([128, NT, 4, 132], BF16)
        iqT = A.alloc([128, 4, SEQ], BF16)
        ikT = A.alloc([128, SEQ], BF16)
        iws = A.alloc([128, NT, 8], F32)
        kaTb, qaTb, vab, iqTb, ikTb, iwsb = Buf(), Buf(), Buf(), Buf(), Buf(), Buf()
        m2 = A.mark()
        AO = Arena(nc, 32768, base=oT_raw)
        stage_state["pool"] = mk_stage(AO)
        wbp = [(AO.alloc([128, 8, 512], BF16), Buf()) for _ in range(2)]
        wss = [mk_qk_ws(5 + k) for k in range(NPS)]
        cp = [wss[k]["qn"] for k in range(NPS)]
        cpb = [wss[k]["qh"] for k in range(NPS)]
        itmp = [Rot(A, 1, [128, 4, 8, 8], F32) for _ in range(NPS)]
        ikf = [Rot(A, 1, [128, 72], F32) for _ in range(NPS)]
        ikd = [Rot(A, 1, [128, 128], BF16) for _ in range(NPS)]
        gc = gcol[:, 0, :]
        S.pool(lambda e: nc.gpsimd.memset(va[:, :, :, 128:129], 1.0), writes=[vab])

        def h_va(pi, i, k):
            S.act(lambda e: nc.scalar.copy(out=va[:, i, :, 0:128], in_=psf(pi).rearrange("p (h d) -> p h d", h=4)),
                  reads=[PSB[pi]], writes=[vab])
            yield

        def h_iq(pi, i, k):
            c, cb = cp[k].next()
            S.act(lambda e: nc.scalar.copy(out=c, in_=psf(pi)), reads=[PSB[pi]], writes=[cb])
            yield
            tmp, tmpb = itmp[k].next()
            rope(c.rearrange("p (h d) -> p h d", h=8), cb, 8, 8, ropeI, i, tmp, tmpb)
            yield
            ch_, chb = cpb[k].next()
            S.act(lambda e: nc.scalar.copy(out=ch_, in_=c), reads=[cb], writes=[chb])
            yield
            ptv = psh(5 + k)
            transposes([(ptv[:, p * 128:(p + 1) * 128], ch_[:, p * 128:(p + 1) * 128]) for p in range(4)], ident,
                       [chb, constb], [PSB[5 + k]])
            yield
            S.act(lambda e: nc.scalar.copy(out=iqT[:, :, i * 128:(i + 1) * 128],
                                           in_=ptv[:, 0:512].rearrange("p (h t) -> p h t", h=4)),
                  reads=[PSB[5 + k]], writes=[iqTb])
            yield

        def h_ik(pi, i, k):
            f_, fb = ikf[k].next()
            S.act(lambda e: nc.scalar.copy(out=f_, in_=psf(pi)[:, 0:72]), reads=[PSB[pi]], writes=[fb])
            yield
            tmp, tmpb = itmp[k].next()
            rope(f_[:, 0:64].rearrange("p (h d) -> p h d", h=1), fb, 1, 8, ropeI, i, tmp[:, :, 0:1, :], tmpb)
            yield
            d_, db = ikd[k].next()

            def fcp(e):
                nc.vector.tensor_copy(out=d_[:, 0:64], in_=f_[:, 0:64])
                nc.vector.tensor_copy(out=d_[:, 64:128], in_=f_[:, 0:64])
                return nc.vector.tensor_scalar(out=iws[:, i, :], in0=f_[:, 64:72], scalar1=1.0 / (8.0 * 8.0 ** 0.5),
                                               scalar2=None, op0=ALU.mult)
            S.dve(fcp, reads=[fb], writes=[db, iwsb])
            yield
            ptv = psh(5 + k)
            transposes([(ptv[:, 0:128], d_)], ident, [db, constb], [PSB[5 + k]])
            yield
            S.act(lambda e: nc.scalar.copy(out=ikT[:, i * 128:(i + 1) * 128], in_=ptv[:, 0:128]),
                  reads=[PSB[5 + k]], writes=[ikTb])
            yield

        blocks = [("qa", 0), ("ka", 512), ("va", 1024), ("iq", 1536), ("ik", 2048)]
        for bi, (nm, c0) in enumerate(blocks):
            ncols = 72 if nm == "ik" else 512
            wb, wbb = wbp[bi % 2]
            load_w(w_in_ab[:, c0:c0 + ncols], 8, ncols, wb[:, :, 0:ncols], wbb, gc)
            if nm == "qa":
                proj_streamed(wb, wbb, 512, lambda pi, i, k: qk_post(pi, i, wss[k], 0, qaT, qaTb, 128 ** -0.5, True))
            elif nm == "ka":
                proj_streamed(wb, wbb, 512, lambda pi, i, k: qk_post(pi, i, wss[k], 1, kaT, kaTb, 1.0, True))
            elif nm == "va":
                proj_streamed(wb, wbb, 512, h_va)
            elif nm == "iq":
                proj_streamed(wb, wbb, 512, h_iq)
            else:
                proj_streamed(wb, wbb, 72, h_ik)
        S.flush()
        A.release(m2)
        AN = Arena(nc, 32768, base=nT_raw)
        junk = AN.alloc([128, SEQ], BF16)
        junkb = Buf()

        def dsa_stream(k):
            sc = AN.alloc([128, SEQ], F32)
            scb = Buf()
            mk = AN.alloc([128, SEQ], BF16)
            mkb = Buf()
            mT = A.alloc([128, NT, 128], BF16)
            mTb = Buf()
            tmpf = Rot(A, 2, [128, 512], F32)
            ptr = Rot(A, 2, [128, 512], BF16)
            ptmr = Rot(A, 2, [128, 512], BF16)
            bsr = Rot(A, 2, [128, 8], F32)
            rzr = Rot(A, 2, [128, 4], F32)
            oar = Rot(A, 1, [128, 512], BF16)
            pidx, pst, ppv = k, 2 + k, 4 + k
            yield
            for j in range(k, NT, 2):
                W = (j + 1) * 128
                for h in range(8):
                    p0 = (h % 2) * 64
                    pair = h // 2
                    for kb in range((W + 511) // 512):
                        c0 = kb * 512
                        cw = min(512, W - c0)
                        mm_group(psf(pidx)[:, 0:cw], [(iqT[p0:p0 + 64, pair, j * 128:(j + 1) * 128], ikT[p0:p0 + 64, c0:c0 + cw])],
                                 [iqTb, ikTb], [PSB[pidx]])
                        yield
                        t_, tb_ = tmpf.next()
                        S.act(lambda e, t_=t_, cw=cw: nc.scalar.activation(out=t_[:, 0:cw], in_=psf(pidx)[:, 0:cw], func=AF.Relu),
                              reads=[PSB[pidx]], writes=[tb_])
                        yield
                        if h == 0:
                            S.dve(lambda e, t_=t_, c0=c0, cw=cw, j=j: nc.vector.tensor_scalar(out=sc[:, c0:c0 + cw], in0=t_[:, 0:cw], scalar1=iws[:, j, 0:1],
                                                                    scalar2=None, op0=ALU.mult), reads=[tb_, iwsb], writes=[scb])
                        else:
                            S.dve(lambda e, t_=t_, c0=c0, cw=cw, j=j, h=h: nc.vector.scalar_tensor_tensor(out=sc[:, c0:c0 + cw], in0=t_[:, 0:cw],
                                                                           scalar=iws[:, j, h:h + 1], in1=sc[:, c0:c0 + cw],
                                                                           op0=ALU.mult, op1=ALU.add),
                                  reads=[tb_, iwsb, scb], writes=[scb])
                        yield
                S.dve(lambda e, W=W: nc.vector.memset(sc[0:64, W - 64:W], NEG), reads=[scb], writes=[scb])
                yield
                if j >= 2:
                    bs, bsb = bsr.next()

                    def f0(e, bs=bs, W=W):
                        nc.vector.tensor_reduce(out=bs[:, 0:1], in_=sc[:, 0:W - 64], axis=AX.X, op=ALU.min)
                        return nc.vector.tensor_reduce(out=bs[:, 1:2], in_=sc[:, 0:W], axis=AX.X, op=ALU.max)
                    S.dve(f0, reads=[scb], writes=[bsb])
                    yield
                    S.dve(lambda e, bs=bs: nc.vector.tensor_tensor(out=bs[:, 1:2], in0=bs[:, 1:2], in1=bs[:, 0:1], op=ALU.subtract),
                          reads=[bsb], writes=[bsb])
                    yield
                    for it in range(1, BIS_ITERS + 1):
                        f = 2.0 ** (-it)
                        S.dve(lambda e, bs=bs, f=f: nc.vector.tensor_scalar(out=bs[:, 2:3], in0=bs[:, 1:2], scalar1=f, scalar2=bs[:, 0:1],
                                                                op0=ALU.mult, op1=ALU.add), reads=[bsb], writes=[bsb])
                        yield
                        S.dve(lambda e, bs=bs, W=W: nc.vector.tensor_scalar(out=junk[:, 0:W], in0=sc[:, 0:W], scalar1=bs[:, 2:3], scalar2=None,
                                                                op0=ALU.is_ge, op1=ALU.add, accum_out=bs[:, 3:4]),
                              reads=[bsb, scb], writes=[bsb, junkb])
                        yield
                        S.dve(lambda e, bs=bs: nc.vector.tensor_scalar(out=bs[:, 4:5], in0=bs[:, 3:4], scalar1=255.5, scalar2=bs[:, 1:2],
                                                                op0=ALU.is_ge, op1=ALU.mult), reads=[bsb], writes=[bsb])
                        yield
                        S.dve(lambda e, bs=bs, f=f: nc.vector.scalar_tensor_tensor(out=bs[:, 0:1], in0=bs[:, 4:5], scalar=f, in1=bs[:, 0:1],
                                                                       op0=ALU.mult, op1=ALU.add), reads=[bsb], writes=[bsb])
                        yield
                    thr, thrb = bs[:, 0:1], bsb
                else:
                    thr, thrb = thr_all[:, 0:1], constb
                S.dve(lambda e, W=W, thr=thr: nc.vector.tensor_scalar(out=mk[:, 0:W], in0=sc[:, 0:W], scalar1=thr, scalar2=None, op0=ALU.is_ge),
                      reads=[scb, thrb], writes=[mkb])
                yield
                for g in range((j + 4) // 4):
                    kts = list(range(4 * g, min(4 * g + 4, j + 1)))
                    n = len(kts)
                    pv = psh(pidx)
                    transposes([(pv[:, q * 128:(q + 1) * 128], mk[:, kt * 128:(kt + 1) * 128]) for q, kt in enumerate(kts)],
                               ident, [mkb, constb], [PSB[pidx]])
                    yield
                    S.act(lambda e, g=g, n=n, pv=pv: nc.scalar.copy(out=mT[:, 4 * g:4 * g + n, :],
                                                   in_=pv[:, 0:n * 128].rearrange("p (k t) -> p k t", k=n)),
                          reads=[PSB[pidx]], writes=[mTb])
                    yield
                oa, oab = oar.next()
                for h in range(4):
                    oc = (h % 2) * 256
                    ov = psf(ppv)[:, oc:oc + 129]
                    for g in range((j + 4) // 4):
                        kts = list(range(4 * g, min(4 * g + 4, j + 1)))
                        n = len(kts)

                        def fs(e, kts=kts, h=h, j=j):
                            inst = None
                            for q, kt in enumerate(kts):
                                inst = nc.tensor.matmul(psf(pst)[:, q * 128:(q + 1) * 128], lhsT=kaT[:, h, kt * 128:(kt + 1) * 128],
                                                        rhs=qaT[:, h, j * 128:(j + 1) * 128], start=True, stop=True)
                            return inst
                        S.pe(fs, reads=[kaTb, qaTb], writes=[PSB[pst]])
                        yield
                        pt, ptb = ptr.next()
                        S.act(lambda e, pt=pt, n=n: nc.scalar.activation(out=pt[:, 0:n * 128], in_=psf(pst)[:, 0:n * 128], func=AF.Exp),
                              reads=[PSB[pst]], writes=[ptb])
                        yield
                        pm, pmb = ptmr.next()
                        S.pool(lambda e, pm=pm, pt=pt, g=g, n=n: nc.gpsimd.tensor_tensor(out=pm[:, 0:n * 128], in0=pt[:, 0:n * 128],
                                                                in1=mT[:, 4 * g:4 * g + n, :].rearrange("p k t -> p (k t)"),
                                                                op=ALU.mult), reads=[ptb, mTb], writes=[pmb])
                        yield

                        def fo(e, kts=kts, pm=pm, ov=ov, h=h, j=j):
                            inst = None
                            for q, kt in enumerate(kts):
                                inst = nc.tensor.matmul(ov, lhsT=pm[:, q * 128:(q + 1) * 128], rhs=va[:, kt, h, 0:129],
                                                        start=(kt == 0), stop=(kt == j))
                            return inst
                        S.pe(fo, reads=[pmb, vab], writes=[PSB[ppv]])
                        yield
                    rz, rzb = rzr.next()
                    S.dve(lambda e, rz=rz, oc=oc: nc.vector.reciprocal(out=rz[:, 0:1], in_=psf(ppv)[:, oc + 128:oc + 129]),
                          reads=[PSB[ppv]], writes=[rzb])
                    yield
                    S.act(lambda e, oa=oa, h=h, oc=oc, rz=rz: nc.scalar.activation(out=oa[:, h * 128:(h + 1) * 128], in_=psf(ppv)[:, oc:oc + 128],
                                                         func=AF.Identity, scale=rz[:, 0:1]),
                          reads=[PSB[ppv], rzb], writes=[oab])
                    yield
                ptv = psh(pst)
                transposes([(ptv[:, h * 128:(h + 1) * 128], oa[:, h * 128:(h + 1) * 128]) for h in range(4)], ident,
                           [oab, constb], [PSB[pst]])
                yield
                S.act(lambda e, j=j, ptv=ptv: nc.scalar.copy(out=oT[:, 0:4, j * 128:(j + 1) * 128],
                                               in_=ptv[:, 0:512].rearrange("p (h t) -> p h t", h=4)),
                      reads=[PSB[pst]], writes=[oTb[j]])
                yield

        run_streams([dsa_stream(0), dsa_stream(1)])
        S.flush()
        A.release(m)

    def gla(s):
        m = A.mark()
        stage_state["pool"] = mk_stage()
        wq = A.alloc([128, 8, 256], BF16)
        wk = A.alloc([128, 8, 256], BF16)
        wv = A.alloc([128, 8, 512], BF16)
        wl = A.alloc([128, 8, 16], BF16)
        wo_ = A.alloc([128, 8, 512], BF16)
        wb_ = Buf()
        gc = gcol[:, 0, :]
        load_w(w_in_ab[:, 2120:2376], 8, 256, wq, wb_, gc)
        load_w(w_in_ab[:, 2376:2632], 8, 256, wk, wb_, gc)
        load_w(w_in_ab[:, 2632:3144], 8, 512, wv, wb_, gc)
        load_w(w_in_ab[:, 3144:3160], 8, 16, wl, wb_, gc)
        load_w(w_in_ab[:, 3160:3672], 8, 512, wo_, wb_, gc)
        glr = [(A.alloc([32, 64], F32), Buf()) for _ in range(2)]
        for gl_, glb in glr:
            S.dve(lambda e, gl_=gl_: nc.vector.memset(gl_, 1.0), writes=[glb])
        spr = Rot(A, 2, [64, 256], F32)
        ebr = Rot(A, 2, [128, 2, 2, 64], F32)
        ebvr = Rot(A, 2, [64, 256], F32)
        qer = Rot(A, 2, [128, 2, 2, 64], BF16)
        for q_, qb_ in qer.items:
            S.dve(lambda e, q_=q_: nc.vector.memset(q_, 0.0), writes=[qb_])
        kdr = Rot(A, 2, [128, 2, 64], BF16)
        klr = Rot(A, 2, [64, 256], BF16)
        vcr = Rot(A, 2, [64, 512], BF16)
        sgr = Rot(A, 2, [64, 512], F32)
        gvr = Rot(A, 2, [64, 512], F32)
        atr = Rot(A, 2, [64, 4, 64], BF16)
        sqr = Rot(A, 2, [64, 512], F32)
        ssr = Rot(A, 2, [64, 3, 4], F32)
        onr = Rot(A, 2, [64, 512], F32)
        obr = Rot(A, 2, [64, 512], BF16)
        Sf = A.alloc([128, 2, 128], F32)
        Sfb = [Buf(), Buf()]
        Sbf = [(A.alloc([128, 2, 128], BF16), [Buf(), Buf()]) for _ in range(2)]
        def gla_prep(c, F):
            t0 = c * 64
            ntb = nTb[t0 // 128]
            gl_, glb = glr[c % 2]
            mm_group(psf(0)[0:16, 0:64], [(wl[:, kc, 0:16], nT[:, kc, t0:t0 + 64]) for kc in range(8)], [ntb, wb_], [PSB[0]])
            yield
            S.act(lambda e, gl_=gl_: nc.scalar.copy(out=gl_[0:16, :], in_=psf(0)[0:16, 0:64]), reads=[PSB[0]], writes=[glb])
            yield
            mm_group(psf(0)[0:64, 256:512], [(gl_[0:17, 0:64], wg[0:17, :])], [glb, constb], [PSB[0]])
            yield
            sp, spb = spr.next()
            S.act(lambda e, sp=sp: nc.scalar.activation(out=sp, in_=psf(0)[0:64, 256:512], func=AF.Exp, scale=-1.0),
                  reads=[PSB[0]], writes=[spb])
            yield
            S.act(lambda e, sp=sp: nc.scalar.activation(out=sp, in_=sp, func=AF.Ln, bias=1.0), reads=[spb], writes=[spb])
            yield
            def fb(e, sp=sp):
                nc.tensor.matmul(psf(1)[:, 0:64], lhsT=sp[:, 0:128], rhs=tri_incl, start=True, stop=True)
                nc.tensor.matmul(psf(1)[:, 64:128], lhsT=sp[:, 128:256], rhs=tri_incl, start=True, stop=True)
                return nc.tensor.matmul(psf(1)[0:64, 128:384], lhsT=tri_rev, rhs=sp, start=True, stop=True)
            S.pe(fb, reads=[spb, constb], writes=[PSB[1]])
            yield
            eb, ebb = ebr.next()
            ebv, ebvb = ebvr.next()

            def fe(e, eb=eb, ebv=ebv):
                bv = psf(1)[:, 0:128].rearrange("p (g t) -> p g t", g=2)
                nc.scalar.activation(out=eb[:, 0], in_=bv, func=AF.Exp)
                nc.scalar.activation(out=eb[:, 1], in_=bv, func=AF.Exp, scale=-1.0)
                return nc.scalar.activation(out=ebv, in_=psf(1)[0:64, 128:384], func=AF.Exp)
            S.act(fe, reads=[PSB[1]], writes=[ebb, ebvb])
            yield
            def fqk(e, t0=t0):
                inst = None
                for g in range(2):
                    for kc in range(8):
                        nc.tensor.matmul(psf(2)[:, g * 64:(g + 1) * 64], lhsT=wq[:, kc, g * 128:(g + 1) * 128],
                                         rhs=nT[:, kc, t0:t0 + 64], start=(kc == 0), stop=(kc == 7))
                for g in range(2):
                    for kc in range(8):
                        nc.tensor.matmul(psf(2)[:, 128 + g * 64:128 + (g + 1) * 64], lhsT=wk[:, kc, g * 128:(g + 1) * 128],
                                         rhs=nT[:, kc, t0:t0 + 64], start=(kc == 0), stop=(kc == 7))
                for kc in range(8):
                    inst = nc.tensor.matmul(psf(2)[0:64, 256:512], lhsT=nT[:, kc, t0:t0 + 64], rhs=wk[:, kc, :],
                                            start=(kc == 0), stop=(kc == 7))
                return inst
            S.pe(fqk, reads=[ntb, wb_], writes=[PSB[2]])
            yield
            qe, qeb = qer.next()
            kd, kdb = kdr.next()
            kl, klb = klr.next()

            def fq(e, qe=qe, kd=kd, kl=kl, eb=eb, ebv=ebv):
                for hh in range(2):
                    p0 = hh * 64
                    nc.vector.scalar_tensor_tensor(out=qe[p0:p0 + 64, :, hh, :],
                                                   in0=psf(2)[p0:p0 + 64, 0:128].rearrange("p (g t) -> p g t", g=2),
                                                   scalar=0.125, in1=eb[p0:p0 + 64, 0], op0=ALU.mult, op1=ALU.mult)
                nc.vector.tensor_tensor(out=kd, in0=psf(2)[:, 128:256].rearrange("p (g t) -> p g t", g=2), in1=eb[:, 1],
                                        op=ALU.mult)
                return nc.vector.tensor_tensor(out=kl, in0=psf(2)[0:64, 256:512], in1=ebv, op=ALU.mult)
            S.dve(fq, reads=[PSB[2], ebb, ebvb], writes=[qeb, kdb, klb])
            yield
            mm_group(psf(3)[0:64, :], [(nT[:, kc, t0:t0 + 64], wv[:, kc, :]) for kc in range(8)], [ntb, wb_], [PSB[3]])
            yield
            vc, vcb = vcr.next()
            S.act(lambda e, vc=vc: nc.scalar.copy(out=vc, in_=psf(3)[0:64, :]), reads=[PSB[3]], writes=[vcb])
            yield
            mm_group(psf(4)[0:64, :], [(nT[:, kc, t0:t0 + 64], wo_[:, kc, :]) for kc in range(8)], [ntb, wb_], [PSB[4]])
            yield
            sg, sgb = sgr.next()

            def fsg(e, sg=sg):
                nc.scalar.activation(out=sg, in_=psf(4)[0:64, :], func=AF.Exp, scale=-1.0)
                nc.scalar.activation(out=sg, in_=sg, func=AF.Ln, bias=1.0)
                return nc.scalar.activation(out=sg, in_=sg, func=AF.Exp, scale=-1.0)
            S.act(fsg, reads=[PSB[4]], writes=[sgb])
            yield
            gv, gvb = gvr.next()
            S.dve(lambda e, gv=gv, sg=sg: nc.vector.tensor_tensor(out=gv, in0=psf(4)[0:64, :], in1=sg, op=ALU.mult),
                  reads=[PSB[4], sgb], writes=[gvb])
            yield
            F.update(dict(qe=qe, qeb=qeb, kd=kd, kdb=kdb, kl=kl, klb=klb, vc=vc, vcb=vcb, gv=gv, gvb=gvb, eb=eb, ebb=ebb))
            yield

        def gla_scan(c, F):
            t0 = c * 64
            qe, qeb, kd, kdb, kl, klb = F['qe'], F['qeb'], F['kd'], F['kdb'], F['kl'], F['klb']
            vc, vcb, gv, gvb, eb, ebb = F['vc'], F['vcb'], F['gv'], F['gvb'], F['eb'], F['ebb']
            def fat(e, kd=kd, qe=qe):
                inst = None
                for g in range(2):
                    for hh in range(2):
                        p0 = hh * 64
                        q = g * 2 + hh
                        inst = nc.tensor.matmul(psf(5)[0:64, q * 64:(q + 1) * 64], lhsT=kd[:, g, :],
                                                rhs=qe[:, g, hh, :], start=True, stop=True)
                return inst
            S.pe(fat, reads=[kdb, qeb], writes=[PSB[5]])
            yield
            at, atb = atr.next()
            S.dve(lambda e, at=at: nc.vector.tensor_tensor(
                out=at, in0=psf(5)[0:64, 0:256].rearrange("p (q t) -> p q t", q=4),
                in1=causT.unsqueeze(1).to_broadcast([64, 4, 64]), op=ALU.mult), reads=[PSB[5], constb], writes=[atb])
            yield
            sbf_prev, sbfb_prev = Sbf[(c + 1) % 2]
            sbf_cur, sbfb_cur = Sbf[c % 2]

            def fo(e, qe=qe, at=at, vc=vc, c=c, sbf_prev=sbf_prev):
                inst = None
                for g in range(2):
                    for hh in range(2):
                        p0 = hh * 64
                        q = g * 2 + hh
                        ov = psf(6)[0:64, q * 128:(q + 1) * 128]
                        if c > 0:
                            nc.tensor.matmul(ov, lhsT=qe[:, g, hh, :], rhs=sbf_prev[:, g, :], start=True, stop=False)
                        inst = nc.tensor.matmul(ov, lhsT=at[:, q, :], rhs=vc[:, q * 128:(q + 1) * 128], start=(c == 0), stop=True)
                return inst
            S.pe(fo, reads=[qeb, atb, vcb] + (sbfb_prev if c > 0 else []), writes=[PSB[6]])
            yield
            if c < NCH - 1:
                def fu(e, kl=kl, vc=vc):
                    nc.tensor.matmul(psf(7)[:, 0:256], lhsT=kl[:, 0:128], rhs=vc[:, 0:256], start=True, stop=True)
                    return nc.tensor.matmul(psf(7)[:, 256:512], lhsT=kl[:, 128:256], rhs=vc[:, 256:512], start=True, stop=True)
                S.pe(fu, reads=[klb, vcb], writes=[PSB[7]])
                for g in range(2):
                    def fs_(e, g=g, eb=eb, c=c):
                        inst = None
                        for hh in range(2):
                            p0 = hh * 64
                            uv = psf(7)[p0:p0 + 64, g * 256 + hh * 128:g * 256 + (hh + 1) * 128]
                            if c == 0:
                                inst = nc.vector.tensor_copy(out=Sf[p0:p0 + 64, g, :], in_=uv)
                            else:
                                inst = nc.vector.scalar_tensor_tensor(out=Sf[p0:p0 + 64, g, :], in0=Sf[p0:p0 + 64, g, :],
                                                                      scalar=eb[p0:p0 + 64, 0, g, 63:64], in1=uv,
                                                                      op0=ALU.mult, op1=ALU.add)
                        return inst
                    S.dve(fs_, reads=[PSB[7], ebb, Sfb[g]], writes=[Sfb[g]])
                    S.act(lambda e, g=g, sbf_cur=sbf_cur: nc.scalar.copy(out=sbf_cur[:, g, :], in_=Sf[:, g, :]),
                          reads=[Sfb[g]], writes=[sbfb_cur[g]])
            sq, sqb = sqr.next()
            ss, ssb = ssr.next()
            S.act(lambda e, sq=sq: nc.scalar.activation(out=sq, in_=psf(6)[0:64, :], func=AF.Square), reads=[PSB[6]], writes=[sqb])
            yield
            S.dve(lambda e, sq=sq, ss=ss: nc.vector.tensor_reduce(out=ss[:, 0, :], in_=sq.rearrange("p (h d) -> p h d", h=4),
                                                                  axis=AX.X, op=ALU.add), reads=[sqb], writes=[ssb])
            yield
            rstd_from_ss(ss, ssb, 4, 1.0 / 128, True)
            yield
            on, onb = onr.next()

            def fn_(e, on=on, ss=ss):
                inst = None
                for h in range(4):
                    inst = nc.vector.scalar_tensor_tensor(out=on[:, h * 128:(h + 1) * 128], in0=psf(6)[0:64, h * 128:(h + 1) * 128],
                                                          scalar=ss[:, 2, h:h + 1], in1=gains[0:64, 4, :], op0=ALU.mult,
                                                          op1=ALU.mult)
                return inst
            S.dve(fn_, reads=[PSB[6], ssb, constb], writes=[onb])
            yield
            ob, obb = obr.next()
            S.dve(lambda e, ob=ob, on=on, gv=gv: nc.vector.tensor_tensor(out=ob, in0=on, in1=gv, op=ALU.mult),
                  reads=[onb, gvb], writes=[obb])
            yield
            ptv = psh(5)
            transposes([(ptv[:, 512 + h * 64:512 + (h + 1) * 64], ob[:, h * 128:(h + 1) * 128]) for h in range(4)],
                       ident[0:64, 0:64], [obb, constb], [PSB[5]])
            yield
            S.act(lambda e, t0=t0, ptv=ptv: nc.scalar.copy(out=oT[:, 4:8, t0:t0 + 64],
                                                           in_=ptv[:, 512:768].rearrange("p (h t) -> p h t", h=4)),
                  reads=[PSB[5]], writes=[oTb[t0 // 128]])
            yield

        NCH = SEQ // 64
        Fs = [dict() for _ in range(NCH)]
        run_streams([gla_prep(0, Fs[0])])
        for c in range(NCH):
            gens = [gla_scan(c, Fs[c])]
            if c + 1 < NCH:
                gens.insert(0, gla_prep(c + 1, Fs[c + 1]))
            run_streams(gens)
        S.flush()
        A.release(m)

    def sb_attn(s):
        m = A.mark()
        qT = A.alloc([128, 4, SEQ], BF16)
        kT = A.alloc([128, 4, SEQ], BF16)
        v = A.alloc([128, NT, 512], BF16)
        qTb, kTb, vb_ = Buf(), Buf(), Buf()
        gc = gcol[:, 1, :]
        for hg in range(2):
            mp = A.mark()
            stage_state["pool"] = mk_stage()
            wbp = [(A.alloc([128, 8, 512], BF16), Buf()) for _ in range(2)]
            wss = [mk_qk_ws(5 + k) for k in range(NPS)]
            for bi, (nm, c0) in enumerate((("q", hg * 512), ("k", 1024 + hg * 512), ("v", 2048 + hg * 512))):
                wb, wbb = wbp[bi % 2]
                load_w(w_in_c[:, c0:c0 + 512], 8, 512, wb, wbb, gc)
                if nm == "q":
                    proj_streamed(wb, wbb, 512, lambda pi, i, k: qk_post(pi, i, wss[k], 2, qT, qTb, 128 ** -0.5, False))
                elif nm == "k":
                    proj_streamed(wb, wbb, 512, lambda pi, i, k: qk_post(pi, i, wss[k], 3, kT, kTb, 1.0, False))
                else:
                    def hv(pi, i, k):
                        S.act(lambda e: nc.scalar.copy(out=v[:, i, :], in_=psf(pi)), reads=[PSB[pi]], writes=[vb_])
                        yield
                    proj_streamed(wb, wbb, 512, hv)
            S.flush()
            A.release(mp)
            def sb_stream(st_, heads, hg=hg):
                pz, pc, po = 0 + st_, 2 + st_, 4 + st_
                espr = Rot(A, 2, [128, 512], F32)
                lor = Rot(A, 2, [128, 512], BF16)
                ar = Rot(A, 2, [128, 512], BF16)
                lbs = [(A.alloc([128, 512], BF16), Buf()) for _ in range(3)]
                gs = [0]

                def S1(f):
                    h, qb, kt, step, nsteps = f["h"], f["qb"], f["kt"], f["step"], f["nsteps"]
                    cc, ncol, diag = f["cc"], f["ncol"], f["diag"]
                    q0 = qb * 512 + cc
                    mm_group(psf(pz)[:, 0:ncol], [(kT[:, h, kt * 128:(kt + 1) * 128], qT[:, h, q0:q0 + ncol])],
                             [kTb, qTb], [PSB[pz]])
                    yield
                    es, esb = espr.next()
                    f["es"], f["esb"] = es, esb
                    S.act(lambda e: nc.scalar.activation(out=es[:, 0:ncol], in_=psf(pz)[:, 0:ncol], func=AF.Exp, scale=-1.0),
                          reads=[PSB[pz]], writes=[esb])
                    yield
                    S.act(lambda e: nc.scalar.activation(out=es[:, 0:ncol], in_=es[:, 0:ncol], func=AF.Ln, bias=1.0),
                          reads=[esb], writes=[esb])
                    yield
                    lo, lob = lor.next()
                    f["lo"], f["lob"] = lo, lob
                    S.dve(lambda e: nc.vector.scalar_tensor_tensor(out=lo[:, 0:ncol], in0=psf(pz)[:, 0:ncol], scalar=-1.0,
                                                                   in1=es[:, 0:ncol], op0=ALU.mult, op1=ALU.subtract),
                          reads=[PSB[pz], esb], writes=[lob])
                    yield
                    if diag:
                        S.pool(lambda e: nc.gpsimd.tensor_tensor(out=lo[:, 0:128], in0=lo[:, 0:128], in1=strictT, op=ALU.mult),
                               reads=[lob, constb], writes=[lob])
                        yield
                    g = gs[0]
                    gs[0] += 1
                    f["lbc"] = lbs[g % 3]
                    if step < nsteps - 1:
                        lbn, lbnb = lbs[(g + 1) % 3]
                        lbc, lbcb = lbs[g % 3]
                        ccn = f["cc_next"]
                        if ccn < cc:
                            S.pool(lambda e: nc.gpsimd.memset(lbn[:, ccn:cc], 0.0), writes=[lbnb])
                            yield
                        if step == 0:
                            S.dve(lambda e: nc.vector.tensor_copy(out=lbn[:, cc:512], in_=lo[:, 0:ncol]), reads=[lob], writes=[lbnb])
                        else:
                            S.dve(lambda e: nc.vector.tensor_tensor(out=lbn[:, cc:512], in0=lbc[:, cc:512], in1=lo[:, 0:ncol],
                                                                    op=ALU.add), reads=[lob, lbcb], writes=[lbnb])
                        yield

                def S2(f):
                    h, qb, kt, step, nsteps = f["h"], f["qb"], f["kt"], f["step"], f["nsteps"]
                    cc, ncol, diag = f["cc"], f["ncol"], f["diag"]
                    es, esb, lo, lob = f["es"], f["esb"], f["lo"], f["lob"]
                    lbc, lbcb = f["lbc"]
                    if step == 0:
                        mm_group(psf(pc)[:, 0:ncol], [(triU, lo[:, 0:ncol])], [lob, constb], [PSB[pc]])
                    else:
                        mm_group(psf(pc)[:, 0:ncol], [(triU, lo[:, 0:ncol]), (ones128, lbc[:, cc:512])],
                                 [lob, lbcb, constb], [PSB[pc]])
                    yield
                    S.dve(lambda e: nc.vector.tensor_tensor(out=es[:, 0:ncol], in0=psf(pc)[:, 0:ncol], in1=es[:, 0:ncol],
                                                            op=ALU.subtract), reads=[PSB[pc], esb], writes=[esb])
                    yield
                    a_, ab_ = ar.next()
                    S.act(lambda e: nc.scalar.activation(out=a_[:, 0:ncol], in_=es[:, 0:ncol], func=AF.Exp),
                          reads=[esb], writes=[ab_])
                    yield
                    if diag:
                        S.pool(lambda e: nc.gpsimd.tensor_tensor(out=a_[:, 0:128], in0=a_[:, 0:128], in1=strictT, op=ALU.mult),
                               reads=[ab_, constb], writes=[ab_])
                        yield
                    S.pe(lambda e: nc.tensor.matmul(psf(po)[:, cc:512], lhsT=v[:, kt, h * 128:(h + 1) * 128], rhs=a_[:, 0:ncol],
                                                    start=(step == 0), stop=(step == nsteps - 1)),
                         reads=[vb_, ab_], writes=[PSB[po]])
                    yield
                    if step == nsteps - 1:
                        hh = hg * 4 + h
                        S.act(lambda e: nc.scalar.copy(out=oT[:, hh, qb * 512:(qb + 1) * 512], in_=psf(po)),
                              reads=[PSB[po]], writes=oTb[4 * qb:4 * qb + 4])
                        yield

                pending = None
                for h in heads:
                    for qb in range(4):
                        kts = list(range(4 * qb + 3, -1, -1))
                        ccs = [max(0, kt - 4 * qb) * 128 for kt in kts]
                        for step, kt in enumerate(kts):
                            f = {"h": h, "qb": qb, "kt": kt, "step": step, "nsteps": len(kts), "cc": ccs[step],
                                 "ncol": 512 - ccs[step], "diag": kt >= 4 * qb,
                                 "cc_next": ccs[step + 1] if step + 1 < len(kts) else 0}
                            yield from S1(f)
                            if pending is not None:
                                yield from S2(pending)
                            pending = f
                yield from S2(pending)

            run_streams([sb_stream(0, [0, 1]), sb_stream(1, [2, 3])])
            S.flush()
            A.release(mp)
        A.release(m)

    def dump_h(s):
        m = A.mark()
        t = [(A.alloc([128, 1024], F32), Buf(), f"xin{k}") for k in range(2)]
        for i in range(NT):
            tt, tb, ch = t[i % 2]
            S.dma(lambda e, tt=tt, i=i: nc.sync.dma_start(out=tt, in_=h_scr[s, i * 128:(i + 1) * 128, :]), ch,
                  reads=[hb[s][i]], writes=[tb])
            S.dma(lambda e, tt=tt, i=i: nc.sync.dma_start(out=out[s, i * 128:(i + 1) * 128, :], in_=tt), f"st{i % 2}",
                  reads=[tb], writes=[])
        S.flush()
        A.release(m)

    for s in range(NSEQ):
        if stop != "const":
            phase_norm(s, x)
        if stop == "norm":
            continue
        if stop == "const":
            continue
        dsa(s)
        if stop == "dsa":
            continue
        phase_norm(s, x)
        gla(s)
        if stop == "gla":
            continue
        out_proj_and_norm(s, w_out_ab, x, h_scr)
        if stop == "mix0":
            dump_h(s)
            continue
        ffn(s, 0, False)
        if stop == "ffn0":
            dump_h(s)
            continue
        phase_norm(s, h_scr)
        sb_attn(s)
        out_proj_and_norm(s, w_out_c, h_scr, h_scr)
        if stop == "mix1":
            dump_h(s)
            continue
        ffn(s, 1, True)
    S.flush(final=True)
    return nc


def host_constants():
    c = {}
    c["c_ident"] = np.eye(128, dtype=np.float32)
    pos = np.arange(SEQ, dtype=np.float32)
    inv_a = np.power(np.float32(500000.0), -np.arange(16, dtype=np.float32) * 2.0 / 32).astype(np.float32)
    ang = pos[:, None] * inv_a[None, :]
    c["c_rope_a"] = np.concatenate([np.cos(ang), np.sin(ang)], axis=1).astype(np.float32)
    inv_i = np.power(np.float32(500000.0), -np.arange(8, dtype=np.float32) * 2.0 / 16).astype(np.float32)
    ang = pos[:, None] * inv_i[None, :]
    c["c_rope_i"] = np.concatenate([np.cos(ang), np.sin(ang)], axis=1).astype(np.float32)
    a = np.arange(64)
    m64 = np.zeros((64, 3, 64), np.float32)
    m64[:, 0, :] = (a[:, None] <= a[None, :]) * (-1.0 / 16.0)
    m64[:, 1, :] = (a[:, None] > a[None, :]) * (-1.0 / 16.0)
    m64[:, 2, :] = (a[:, None] <= a[None, :]) * 1.0
    c["c_m64"] = m64
    b = np.arange(128)
    m128 = np.zeros((128, 3, 128), np.float32)
    m128[:, 0, :] = (b[:, None] > b[None, :]) * 1.0
    m128[:, 1, :] = 1.0
    m128[:, 2, :] = (b[:, None] < b[None, :]) * 1.0
    c["c_m128"] = m128
    return c


_CACHE = {}


def kernel(x, g_mix, g_ffn, w_in_ab, gq_a, gk_a, w_gate_up, b_gate, g_gla, w_out_ab,
           w_in_c, gq_c, gk_c, w_out_c, w_up, w_down, _ncores=8, _stop=None):
    f = lambda a: np.ascontiguousarray(np.asarray(a, dtype=np.float32))
    x = f(x)
    nseq = x.shape[0] // _ncores
    shared = {
        "w_in_ab": f(w_in_ab)[0], "w_gate_up": f(w_gate_up)[0], "b_gate": f(b_gate), "w_out_ab": f(w_out_ab)[0],
        "w_in_c": f(w_in_c)[0], "w_out_c": f(w_out_c)[0], "w_up": f(w_up), "w_down": f(w_down),
    }
    gains = np.stack([np.broadcast_to(f(g).reshape(1, 128), (128, 128)) for g in (gq_a, gk_a, gq_c, gk_c, g_gla)], axis=1)
    shared["c_gains"] = np.ascontiguousarray(gains, dtype=np.float32)
    gcols = np.stack([f(g_mix)[0].reshape(8, 128).T, f(g_mix)[1].reshape(8, 128).T,
                      f(g_ffn)[0].reshape(8, 128).T, f(g_ffn)[1].reshape(8, 128).T], axis=1)
    shared["c_gcol"] = np.ascontiguousarray(gcols, dtype=np.float32)
    shared.update(host_constants())
    key = (nseq, _stop)
    if key not in _CACHE:
        _CACHE[key] = build_program(nseq, _stop)
    nc = _CACHE[key]
    in_maps = []
    for c in range(_ncores):
        d = dict(shared)
        d["x"] = np.ascontiguousarray(x[c * nseq:(c + 1) * nseq])
        in_maps.append(d)
    res = run_bass_kernel_spmd(nc, in_maps, core_ids=list(range(_ncores)))
    return np.concatenate([np.asarray(r["out"], dtype=np.float32) for r in res.results], axis=0)
```
